# Optimizing a Trainium2 kernel written in Bass

```python
import jax, jax.numpy as jnp
from jax import lax
import numpy as np

D_MODEL = 1024
BATCH = 16
SEQ = 2048
DEPTH = 2

D_MIX = D_MODEL
ATT_HEADS = 8
ATT_HEAD_DIM = 64
ATT_WIDTH = ATT_HEADS * ATT_HEAD_DIM
MLSTM_HEADS = 4
MLSTM_HEAD_DIM = 128
MLSTM_WIDTH = MLSTM_HEADS * MLSTM_HEAD_DIM
Q_RANK = 256
N_IDX_HEADS = 8
IDX_DIM = 64
INDEX_TOPK = 256
Q_BLOCK = 128
MLSTM_CHUNK = 64
CONV_WIDTH = 4
FFN_HIDDEN = -(-8 * D_MODEL // (3 * 256)) * 256
ROPE_THETA = 10000.0
EPS = 1e-6
IN_SPLITS = (Q_RANK, ATT_WIDTH, ATT_WIDTH, IDX_DIM, N_IDX_HEADS,
             MLSTM_WIDTH, MLSTM_WIDTH, MLSTM_WIDTH, MLSTM_WIDTH, MLSTM_HEADS, MLSTM_HEADS)
IN_COLS = sum(IN_SPLITS)

kernel_name = "hybrid_dsa_mlstm_sandwich_adaln_block"


def rms_norm(x, w):
    x32 = x.astype(jnp.float32)
    y = x32 * lax.rsqrt(jnp.mean(x32 * x32, axis=-1, keepdims=True) + EPS)
    return (y * w.astype(jnp.float32)).astype(x.dtype)


def rope(x, positions):
    d = x.shape[-1]
    inv_freq = ROPE_THETA ** (-jnp.arange(0, d, 2, dtype=jnp.float32) / d)
    ang = positions.astype(jnp.float32)[..., None] * inv_freq
    cos = jnp.cos(ang)[:, :, None, :]
    sin = jnp.sin(ang)[:, :, None, :]
    x1, x2 = jnp.split(x.astype(jnp.float32), 2, axis=-1)
    return jnp.concatenate([x1 * cos - x2 * sin, x2 * cos + x1 * sin], axis=-1).astype(x.dtype)


def causal_dwconv(x, w, b):
    ch = x.shape[-1]
    y = lax.conv_general_dilated(x, w[:, None, :].astype(x.dtype), window_strides=(1,),
                                 padding=[(CONV_WIDTH - 1, 0)],
                                 dimension_numbers=('NWC', 'WIO', 'NWC'),
                                 feature_group_count=ch)
    return y + b.astype(x.dtype)


def dsa_attention(q, k, v, q_idx, k_idx, w_idx):
    def one_seq(args):
        q_, k_, v_, qi_, ki_, wi_ = args
        seq = q_.shape[0]
        n_sel = min(INDEX_TOPK, seq // 4)
        key_pos = jnp.arange(seq)
        ki32 = ki_.astype(jnp.float32)
        scale = ATT_HEAD_DIM ** -0.5

        def block(i):
            start = i * Q_BLOCK
            t = start + jnp.arange(Q_BLOCK)
            qb = lax.dynamic_slice_in_dim(q_, start, Q_BLOCK, 0)
            qib = lax.dynamic_slice_in_dim(qi_, start, Q_BLOCK, 0).astype(jnp.float32)
            wib = lax.dynamic_slice_in_dim(wi_, start, Q_BLOCK, 0).astype(jnp.float32)
            idx_logits = jnp.einsum('thd,sd->ths', qib, ki32)
            idx_score = jnp.einsum('ths,th->ts', jax.nn.relu(idx_logits), wib)
            idx_score = jnp.where(key_pos[None, :] <= t[:, None], idx_score, -jnp.inf)
            _, sel = lax.top_k(idx_score, n_sel)
            valid = sel <= t[:, None]
            k_sel = k_[sel]
            v_sel = v_[sel]
            logits = jnp.einsum('thd,tkhd->thk', qb, k_sel).astype(jnp.float32) * scale
            logits = jnp.where(valid[:, None, :], logits, -jnp.inf)
            p = jax.nn.softmax(logits, axis=-1)
            return jnp.einsum('thk,tkhd->thd', p.astype(v_.dtype), v_sel)

        out = lax.map(block, jnp.arange(seq // Q_BLOCK))
        return out.reshape((seq,) + q_.shape[1:])

    return lax.map(one_seq, (q, k, v, q_idx, k_idx, w_idx))


def mlstm_chunkwise(q, k, v, ig, lf):
    b_, nh, seq, dk = q.shape
    dv = v.shape[-1]
    nc = seq // MLSTM_CHUNK

    def chunks(a):
        return jnp.moveaxis(a.reshape(a.shape[:2] + (nc, MLSTM_CHUNK) + a.shape[3:]), 2, 0)

    qc, kc, vc, igc, lfc = chunks(q), chunks(k), chunks(v), chunks(ig), chunks(lf)
    bc = jnp.cumsum(lfc, axis=-1)
    causal = jnp.tril(jnp.ones((MLSTM_CHUNK, MLSTM_CHUNK), dtype=bool))

    def step(carry, inp):
        c_s, n_s, m_s = carry
        q_, k_, v_, ig_, b_c = inp
        d_mat = b_c[..., :, None] - b_c[..., None, :] + ig_[..., None, :]
        d_mat = jnp.where(causal, d_mat, -jnp.inf)
        inter = b_c + m_s[..., None]
        m_row = jnp.maximum(inter, jnp.max(d_mat, axis=-1))
        w_intra = jnp.exp(d_mat - m_row[..., None])
        w_inter = jnp.exp(inter - m_row)
        s_mat = jnp.einsum('bhtd,bhsd->bhts', q_, k_) * w_intra
        num = (jnp.einsum('bhts,bhsv->bhtv', s_mat, v_)
               + w_inter[..., None] * jnp.einsum('bhtd,bhdv->bhtv', q_, c_s))
        den = jnp.sum(s_mat, axis=-1) + w_inter * jnp.einsum('bhtd,bhd->bht', q_, n_s)
        h = num / jnp.maximum(jnp.abs(den), jnp.exp(-m_row))[..., None]
        total = b_c[..., -1]
        w_log = total[..., None] - b_c + ig_
        m_new = jnp.maximum(total + m_s, jnp.max(w_log, axis=-1))
        decay = jnp.exp(total + m_s - m_new)
        w_state = jnp.exp(w_log - m_new[..., None])
        c_new = decay[..., None, None] * c_s + jnp.einsum('bhs,bhsd,bhsv->bhdv', w_state, k_, v_)
        n_new = decay[..., None] * n_s + jnp.einsum('bhs,bhsd->bhd', w_state, k_)
        return (c_new, n_new, m_new), h

    init = (jnp.zeros((b_, nh, dk, dv), jnp.float32), jnp.zeros((b_, nh, dk), jnp.float32),
            jnp.zeros((b_, nh), jnp.float32))
    _, hs = lax.scan(step, init, (qc, kc, vc, igc, bc))
    return jnp.moveaxis(hs, 0, 2).reshape(b_, nh, seq, dv)


def hybrid_mixer(h, positions, w_in, q_latent_norm, w_q_up, w_qidx_up, conv_w, conv_b,
                 b_igate, b_fgate, attn_out_norm, mlstm_out_norm, w_out):
    b_, seq, _ = h.shape
    split_pts = [int(s) for s in np.cumsum(IN_SPLITS)[:-1]]
    (cq, k_att, v_att, k_idx, w_idx,
     q_m, k_m, v_m, o_m, i_m, f_m) = jnp.split(h @ w_in, split_pts, axis=-1)

    cq = rms_norm(cq, q_latent_norm)
    q = rope((cq @ w_q_up).reshape(b_, seq, ATT_HEADS, ATT_HEAD_DIM), positions)
    q_idx = rope((cq @ w_qidx_up).reshape(b_, seq, N_IDX_HEADS, IDX_DIM), positions)
    k = rope(k_att.reshape(b_, seq, ATT_HEADS, ATT_HEAD_DIM), positions)
    v = v_att.reshape(b_, seq, ATT_HEADS, ATT_HEAD_DIM)
    k_idx = rope(k_idx[:, :, None, :], positions)[:, :, 0, :]
    w_idx = w_idx * (N_IDX_HEADS * IDX_DIM) ** -0.5
    att = dsa_attention(q, k, v, q_idx, k_idx, w_idx)
    att = rms_norm(att, attn_out_norm.reshape(ATT_HEADS, ATT_HEAD_DIM)).reshape(b_, seq, ATT_WIDTH)

    qk = jax.nn.silu(causal_dwconv(jnp.concatenate([q_m, k_m], axis=-1), conv_w, conv_b))
    q_m, k_m = jnp.split(qk, 2, axis=-1)

    def heads(a):
        return a.reshape(b_, seq, MLSTM_HEADS, MLSTM_HEAD_DIM).transpose(0, 2, 1, 3).astype(jnp.float32)

    ig = (i_m + b_igate).astype(jnp.float32).transpose(0, 2, 1)
    lf = jax.nn.log_sigmoid((f_m + b_fgate).astype(jnp.float32)).transpose(0, 2, 1)
    hm = mlstm_chunkwise(heads(q_m) * MLSTM_HEAD_DIM ** -0.5, heads(k_m), heads(v_m), ig, lf)
    hm = hm.transpose(0, 2, 1, 3).astype(h.dtype)
    hm = rms_norm(hm, mlstm_out_norm.reshape(MLSTM_HEADS, MLSTM_HEAD_DIM))
    hm = (jax.nn.sigmoid(o_m).reshape(b_, seq, MLSTM_HEADS, MLSTM_HEAD_DIM) * hm).reshape(b_, seq, MLSTM_WIDTH)

    return jnp.concatenate([att, hm], axis=-1) @ w_out


def swiglu(h, w_gate_up, w_down):
    g, u = jnp.split(h @ w_gate_up, 2, axis=-1)
    return (jax.nn.silu(g) * u) @ w_down


def setup_inputs(seed: int = 0) -> dict:
    key = jax.random.key(seed)
    ks = jax.random.split(key, 24)
    L = DEPTH

    def nrm(k, shape, scale):
        return jax.random.normal(k, shape, jnp.float32) * scale

    def gain(k, shape):
        return 1.0 + 0.05 * jax.random.normal(k, shape, jnp.float32)

    x = nrm(ks[0], (BATCH, SEQ, D_MODEL), 1.0)
    c = nrm(ks[1], (BATCH, D_MODEL), 1.0)
    offsets = jax.random.randint(ks[2], (BATCH, 1), 0, 1024, dtype=jnp.int32)
    positions = (offsets + jnp.arange(SEQ, dtype=jnp.int32)[None, :]).astype(jnp.int32)
    b_fgate = (jnp.linspace(3.0, 6.0, MLSTM_HEADS, dtype=jnp.float32)[None, :]
               + nrm(ks[14], (L, MLSTM_HEADS), 0.1))
    return {
        "x": x,
        "c": c,
        "positions": positions,
        "w_mod": nrm(ks[3], (L, D_MODEL, 6 * D_MODEL), 0.5 * D_MODEL ** -0.5),
        "b_mod": nrm(ks[4], (L, 6 * D_MODEL), 0.01),
        "mix_norm_pre": gain(ks[5], (L, D_MODEL)),
        "mix_norm_post": gain(ks[6], (L, D_MODEL)),
        "w_in": nrm(ks[7], (L, D_MODEL, IN_COLS), D_MODEL ** -0.5),
        "q_latent_norm": gain(ks[8], (L, Q_RANK)),
        "w_q_up": nrm(ks[9], (L, Q_RANK, ATT_WIDTH), Q_RANK ** -0.5),
        "w_qidx_up": nrm(ks[10], (L, Q_RANK, N_IDX_HEADS * IDX_DIM), Q_RANK ** -0.5),
        "conv_w": nrm(ks[11], (L, CONV_WIDTH, 2 * MLSTM_WIDTH), CONV_WIDTH ** -0.5),
        "conv_b": nrm(ks[12], (L, 2 * MLSTM_WIDTH), 0.01),
        "b_igate": nrm(ks[13], (L, MLSTM_HEADS), 0.1),
        "b_fgate": b_fgate,
        "attn_out_norm": gain(ks[15], (L, ATT_WIDTH)),
        "mlstm_out_norm": gain(ks[16], (L, MLSTM_WIDTH)),
        "w_out": nrm(ks[17], (L, D_MIX, D_MODEL), D_MIX ** -0.5),
        "ffn_norm_pre": gain(ks[18], (L, D_MODEL)),
        "ffn_norm_post": gain(ks[19], (L, D_MODEL)),
        "w_gate_up": nrm(ks[20], (L, D_MODEL, 2 * FFN_HIDDEN), D_MODEL ** -0.5),
        "w_down": nrm(ks[21], (L, FFN_HIDDEN, D_MODEL), FFN_HIDDEN ** -0.5),
    }


def reference(x, c, positions, w_mod, b_mod, mix_norm_pre, mix_norm_post, w_in, q_latent_norm,
              w_q_up, w_qidx_up, conv_w, conv_b, b_igate, b_fgate, attn_out_norm, mlstm_out_norm,
              w_out, ffn_norm_pre, ffn_norm_post, w_gate_up, w_down):
    c_act = jax.nn.silu(c)
    for l in range(DEPTH):
        mod = c_act @ w_mod[l] + b_mod[l]
        sh_m, sc_m, g_m, sh_f, sc_f, g_f = jnp.split(mod, 6, axis=-1)
        h = rms_norm(x, mix_norm_pre[l]) * (1.0 + sc_m[:, None, :]) + sh_m[:, None, :]
        y = hybrid_mixer(h, positions, w_in[l], q_latent_norm[l], w_q_up[l], w_qidx_up[l],
                         conv_w[l], conv_b[l], b_igate[l], b_fgate[l], attn_out_norm[l],
                         mlstm_out_norm[l], w_out[l])
        x = x + g_m[:, None, :] * rms_norm(y, mix_norm_post[l])
        h = rms_norm(x, ffn_norm_pre[l]) * (1.0 + sc_f[:, None, :]) + sh_f[:, None, :]
        y = swiglu(h, w_gate_up[l], w_down[l])
        x = x + g_f[:, None, :] * rms_norm(y, ffn_norm_post[l])
    return x
```

```python
from contextlib import ExitStack
import math
import numpy as np
import concourse.bass as bass
import concourse.mybir as mybir
from concourse.bass_utils import run_bass_kernel_spmd

F32 = mybir.dt.float32
BF16 = mybir.dt.bfloat16
I32 = mybir.dt.int32
AF = mybir.ActivationFunctionType
ALU = mybir.AluOpType
AX = mybir.AxisListType

D = 1024
S = 2048
NT = 16
NSEQ = 2
FH = 2816
NFC = 22
INC = 3408
EPS = 1e-6
NEG = -1.0e30
TOPK = 256


class Region:
    __slots__ = ("w", "r")

    def __init__(self):
        self.w = None
        self.r = {}


class EngState:
    def __init__(self, name, eng, sem):
        self.name = name
        self.eng = eng
        self.sem = sem
        self.count = 0
        self.waited = {}


class Ctx:
    def __init__(self, nc, stack):
        self.nc = nc
        self.engs = {}
        for name in ("tensor", "vector", "scalar", "gpsimd", "sync"):
            sem = stack.enter_context(nc.semaphore("s_" + name))
            self.engs[name] = EngState(name, getattr(nc, name), sem)
        self.dma_pools = {}
        for q, n in (("sync", 24), ("gpsimd", 16), ("scalar", 4)):
            pool = []
            for i in range(n):
                sem = stack.enter_context(nc.semaphore("s_dma_%s%d" % (q, i)))
                pool.append([sem, 0])
            self.dma_pools[q] = [pool, 0]
        self.n_inst = 0
        self.trace = {n: [] for n in self.engs}

    def _wait(self, es, ev):
        sem, val = ev
        k = id(sem)
        if es.waited.get(k, 0) >= val:
            return
        es.eng.wait_ge(sem, val)
        es.waited[k] = val
        self.trace[es.name].append(("w", k, val))

    def check_deadlock(self):
        vals = {}
        pos = {n: 0 for n in self.trace}
        progress = True
        while progress:
            progress = False
            for n, tr in self.trace.items():
                while pos[n] < len(tr):
                    kind, k, v = tr[pos[n]]
                    if kind == "w":
                        if vals.get(k, 0) < v:
                            break
                    else:
                        vals[k] = vals.get(k, 0) + v
                    pos[n] += 1
                    progress = True
        stuck = {n: (pos[n], len(tr)) for n, tr in self.trace.items() if pos[n] < len(tr)}
        return stuck

    def _deps(self, es, reads, writes, skip_same=False):
        best = {}

        def add(ev):
            if ev is None:
                return
            k = id(ev[0])
            if k not in best or best[k][1] < ev[1]:
                best[k] = ev
        for r in reads:
            add(r.w)
        for w in writes:
            add(w.w)
            for ev in w.r.values():
                add(ev)
        for ev in best.values():
            if skip_same and ev[0] is es.sem:
                continue
            self._wait(es, ev)

    def _commit(self, ev, reads, writes):
        k = id(ev[0])
        for r in reads:
            r.r[k] = ev
        for w in writes:
            w.w = ev
            w.r = {}

    def op(self, engname, reads, writes, fn):
        es = self.engs[engname]
        self._deps(es, reads, writes, skip_same=(engname == "tensor"))
        inst = fn(es.eng)
        es.count += 1
        inst.then_inc(es.sem, 1)
        self.trace[engname].append(("i", id(es.sem), 1))
        self._commit((es.sem, es.count), reads, writes)
        self.n_inst += 1

    def dma(self, qname, out, in_, reads, writes, **kw):
        es = self.engs[qname]
        pr = self.dma_pools[qname]
        slot = pr[0][pr[1]]
        pr[1] = (pr[1] + 1) % len(pr[0])
        sem, tot = slot
        if tot > 0:
            self._wait(es, (sem, tot))
        self._deps(es, reads, writes)
        es.eng.dma_start(out=out, in_=in_, **kw).then_inc(sem, 16)
        self.trace[qname].append(("i", id(sem), 16))
        slot[1] = tot + 16
        self._commit((sem, tot + 16), reads, writes)
        self.n_inst += 1

    def barrier(self):
        snap = [(e.sem, e.count) for e in self.engs.values() if e.count > 0]
        for pr in self.dma_pools.values():
            snap += [(s_, t_) for s_, t_ in pr[0] if t_ > 0]
        for name in ("sync", "gpsimd", "tensor", "vector", "scalar"):
            es = self.engs[name]
            for ev in snap:
                if ev[0] is es.sem:
                    continue
                self._wait(es, ev)

    def finish(self):
        self.barrier()


def bc_mid(ap, n):
    p, f = ap.shape
    return ap.unsqueeze(1).to_broadcast([p, n, f])


def bc_last(ap, n):
    p, a = ap.shape
    return ap.unsqueeze(2).to_broadcast([p, a, n])


def build_nc(n_layers=2, dbg=False):
    nc = bass.Bass("TRN2", target_bir_lowering=False)

    def din(name, shape, dt=F32):
        return nc.dram_tensor(name, shape, dt, kind="ExternalInput").ap()

    def dscr(name, shape, dt):
        return nc.dram_tensor(name, shape, dt, kind=("ExternalOutput" if dbg else "Internal")).ap()

    x_d = din("x", [NSEQ * S, D])
    cT_d = din("cT", [128, 8, NSEQ])
    pos_d = din("pos", [128, NSEQ, NT], I32)
    w_mod_d = din("w_mod", [2, D, 6 * D])
    b_mod_d = din("b_mod", [2, 6 * D])
    npre_d = din("mix_norm_pre", [2, D])
    npost_d = din("mix_norm_post", [2, D])
    w_in_d = din("w_in", [2, D, INC])
    qln_d = din("q_latent_norm", [2, 256])
    wq_d = din("w_q_up", [2, 256, 512])
    wqi_d = din("w_qidx_up", [2, 256, 512])
    convw_d = din("convw", [2, 128, 8, 4])
    convb_d = din("convb", [2, 128, 8])
    big_d = din("b_igate", [2, 4])
    bfg_d = din("b_fgate", [2, 4])
    aon_d = din("attn_out_norm", [2, 512])
    mon_d = din("mlstm_out_norm", [2, 512])
    w_out_d = din("w_out", [2, D, D])
    fpre_d = din("ffn_norm_pre", [2, D])
    fpost_d = din("ffn_norm_post", [2, D])
    w_gu_d = din("w_gate_up", [2, D, 2 * FH])
    w_dn_d = din("w_down", [2, FH, D])
    ident_d = din("c_ident", [128, 128])
    utri_d = din("c_utri", [128, 128])
    negtri_d = din("c_negtri", [128, 128])
    negm_d = din("c_negm", [128, 128])
    invf_d = din("c_invf", [1, 32])
    pw2_d = din("c_pw2", [128, 32])

    out_d = nc.dram_tensor("out", [NSEQ * S, D], F32, kind="ExternalOutput").ap()

    xres_d = dscr("xres", [NSEQ * S, D], F32)
    x1_d = dscr("x1s", [NSEQ * S, D], F32)
    modd = dscr("modd", [2, NSEQ, 6 * D], F32)
    qT_d = dscr("qT", [NSEQ, 128, 4, S], BF16)
    qiT_d = dscr("qiT", [NSEQ, 128, 4, S], BF16)
    kT_d = dscr("kT", [NSEQ, 128, 4, S], BF16)
    kiT_d = dscr("kiT", [NSEQ, 64, S], BF16)
    V_d = dscr("Vs", [NSEQ, S, 512], BF16)
    wi_d = dscr("wi", [NSEQ, S, 8], F32)
    qmT_d = dscr("qmT", [NSEQ, 128, 8, S], BF16)
    Vm_d = dscr("Vm", [NSEQ, S, 512], BF16)
    og_d = dscr("og", [NSEQ, S, 512], BF16)
    ig_d = dscr("ig", [NSEQ, S, 4], F32)
    lf_d = dscr("lf", [NSEQ, S, 4], F32)
    att_d = dscr("att", [NSEQ, S, 512], BF16)
    hm_d = dscr("hm", [NSEQ, S, 512], BF16)

    R_xres = [[Region() for _ in range(NT)] for _ in range(NSEQ)]
    R_x1 = [[Region() for _ in range(NT)] for _ in range(NSEQ)]
    R_modd = Region()
    R_p1 = [[] for _ in range(NSEQ)]
    R_att = [[Region() for _ in range(NT)] for _ in range(NSEQ)]
    R_hm = [[Region() for _ in range(NT)] for _ in range(NSEQ)]

    with ExitStack() as st0:
        cx = Ctx(nc, st0)

        def V(r, w, fn):
            cx.op("vector", r, w, fn)

        def A(r, w, fn):
            cx.op("scalar", r, w, fn)

        def G(r, w, fn):
            cx.op("gpsimd", r, w, fn)

        def T(r, w, fn):
            cx.op("tensor", r, w, fn)

        uniq = [0]

        def sbuf(stk, name, shape, dt):
            uniq[0] += 1
            return stk.enter_context(nc.sbuf_tensor("%s_%d" % (name, uniq[0]), shape, dt))

        def psum(stk, name, shape, dt):
            uniq[0] += 1
            return stk.enter_context(nc.psum_tensor("%s_%d" % (name, uniq[0]), shape, dt))

        identf = sbuf(st0, "identf", [128, 128], F32)
        identb = sbuf(st0, "identb", [128, 128], BF16)
        utri = sbuf(st0, "utri", [128, 128], F32)
        negtri = sbuf(st0, "negtri", [128, 128], F32)
        negm = sbuf(st0, "negm", [128, 128], F32)
        onesf = sbuf(st0, "onesf", [128, 128], F32)
        invf = sbuf(st0, "invf", [128, 32], F32)
        posi = sbuf(st0, "posi", [128, NSEQ, NT], I32)
        posf = sbuf(st0, "posf", [128, NSEQ * NT], F32)
        COS = sbuf(st0, "COS", [128, NSEQ * NT, 32], F32)
        SIN = sbuf(st0, "SIN", [128, NSEQ * NT, 32], F32)
        cact = sbuf(st0, "cact", [128, 8, NSEQ], F32)
        thr0 = sbuf(st0, "thr0", [128, 1], F32)
        pw2 = sbuf(st0, "pw2", [128, 32], F32)
        R_c = Region()
        cx.dma("sync", identf[:], ident_d, [], [R_c])
        cx.dma("sync", utri[:], utri_d, [], [R_c])
        cx.dma("sync", negtri[:], negtri_d, [], [R_c])
        cx.dma("sync", negm[:], negm_d, [], [R_c])
        cx.dma("sync", invf[:], invf_d.partition_broadcast(128), [], [R_c])
        cx.dma("sync", posi[:], pos_d, [], [R_c])
        cx.dma("sync", pw2[:], pw2_d, [], [R_c])
        cx.dma("sync", cact[:], cT_d, [], [R_c])
        R_c2 = Region()
        R_c4, R_c5, R_c6 = Region(), Region(), Region()
        V([R_c], [R_c4], lambda e: e.tensor_copy(out=identb[:], in_=identf[:]))
        V([], [R_c5], lambda e: e.memset(onesf[:], 1.0))
        V([], [R_c6], lambda e: e.memset(thr0[:], -1.0e29))
        V([R_c], [R_c2], lambda e: e.tensor_copy(out=posf[:], in_=posi[:].rearrange("p a b -> p (a b)")))
        R_cs = Region()
        R_c3 = Region()
        A([R_c], [R_c3], lambda e: e.activation(out=cact[:], in_=cact[:], func=AF.Silu))
        RC = [R_c, R_c2, R_c3, R_c4, R_c5, R_c6, R_cs]

        with ExitStack() as st:
            R_ang = Region()
            ANG = sbuf(st, "ANG", [128, NSEQ * NT, 32], F32)
            ang2 = sbuf(st, "ang2", [128, NSEQ * NT, 32], F32)
            angk = sbuf(st, "angk", [128, NSEQ * NT, 32], F32)
            angi = sbuf(st, "angi", [128, NSEQ * NT, 32], I32)
            V([R_c, R_c2], [R_ang], lambda e: e.tensor_tensor(out=ANG[:], in0=bc_last(posf[:], 32), in1=bc_mid(invf[:], NSEQ * NT), op=ALU.mult))
            C1 = 6.28125
            C2 = 2.0 * math.pi - 6.28125
            for dst, off in ((SIN, 0.0), (COS, 0.5 * math.pi)):
                V([R_ang], [R_ang], lambda e: e.tensor_scalar(out=ang2[:], in0=ANG[:], scalar1=off, scalar2=None, op0=ALU.add))
                V([R_ang], [R_ang], lambda e: e.tensor_scalar(out=angk[:], in0=ang2[:], scalar1=1.0 / (2.0 * math.pi), scalar2=None, op0=ALU.mult))
                V([R_ang], [R_ang], lambda e: e.tensor_copy(out=angi[:], in_=angk[:]))
                V([R_ang], [R_ang], lambda e: e.tensor_copy(out=angk[:], in_=angi[:]))
                V([R_ang], [R_ang], lambda e: e.scalar_tensor_tensor(out=ang2[:], in0=angk[:], scalar=-C1, in1=ang2[:], op0=ALU.mult, op1=ALU.add))
                V([R_ang], [R_ang], lambda e: e.scalar_tensor_tensor(out=ang2[:], in0=angk[:], scalar=-C2, in1=ang2[:], op0=ALU.mult, op1=ALU.add))
                V([R_ang], [R_ang], lambda e: e.tensor_scalar(out=angk[:], in0=ang2[:], scalar1=math.pi, scalar2=-2.0 * math.pi, op0=ALU.is_gt, op1=ALU.mult))
                V([R_ang], [R_ang], lambda e: e.tensor_tensor(out=ang2[:], in0=ang2[:], in1=angk[:], op=ALU.add))
                V([R_ang], [R_ang], lambda e: e.tensor_scalar(out=angk[:], in0=ang2[:], scalar1=-math.pi, scalar2=2.0 * math.pi, op0=ALU.is_lt, op1=ALU.mult))
                V([R_ang], [R_ang], lambda e: e.tensor_tensor(out=ang2[:], in0=ang2[:], in1=angk[:], op=ALU.add))
                V([R_ang], [R_ang], lambda e: e.tensor_scalar(out=ang2[:], in0=ang2[:], scalar1=-3.1415925, scalar2=3.1415925, op0=ALU.max, op1=ALU.min))
                A([R_ang], [R_cs, R_ang], lambda e: e.activation(out=dst[:], in_=ang2[:], func=AF.Sin))
            wst = [sbuf(st, "wmst%d" % i, [128, 3072], BF16) for i in range(3)]
            cactb = sbuf(st, "cactb", [128, 8, NSEQ], BF16)
            R_cb = Region()
            V(RC, [R_cb], lambda e: e.tensor_copy(out=cactb[:], in_=cact[:]))
            R_wst = [Region() for _ in range(3)]
            bmod = sbuf(st, "bmod", [NSEQ, 6 * D], F32)
            modsb = sbuf(st, "modsb", [NSEQ, 6 * D], F32)
            R_bm, R_ms = Region(), Region()
            pm = [psum(st, "pm%d" % i, [128, 512], F32) for i in range(6)]
            R_pm = [Region() for _ in range(6)]
            cnt = 0
            for l in range(n_layers):
                cx.dma("sync", bmod[:], b_mod_d[l:l + 1, :].partition_broadcast(NSEQ), [], [R_bm])
                for half in range(2):
                    for kc in range(8):
                        b = cnt % 3
                        cnt += 1
                        cx.dma("gpsimd", wst[b][:], w_mod_d[l, kc * 128:(kc + 1) * 128, half * 3072:(half + 1) * 3072], [], [R_wst[b]])
                        for n in range(6):
                            T([R_wst[b], R_cb], [R_pm[n]], lambda e: e.matmul(pm[n][0:NSEQ, :], lhsT=cactb[:, kc, :], rhs=wst[b][:, n * 512:(n + 1) * 512], start=(kc == 0), stop=(kc == 7)))
                    for n in range(6):
                        c0 = half * 3072 + n * 512
                        V([R_pm[n], R_bm], [R_ms], lambda e: e.tensor_tensor(out=modsb[:, c0:c0 + 512], in0=pm[n][0:NSEQ, :], in1=bmod[:, c0:c0 + 512], op=ALU.add))
                cx.dma("sync", modd[l], modsb[:], [R_ms], [R_modd])
        cx.barrier()

        for l in range(n_layers):
            xin_d = x_d if l == 0 else xres_d
            xout_d = xres_d if l < n_layers - 1 else out_d
            R_xin = None if l == 0 else R_xres

            with ExitStack() as st:
                w_in_sb = sbuf(st, "w_in_sb", [128, 8, INC], BF16)
                wq_sb = sbuf(st, "wq_sb", [128, 2, 1024], BF16)
                R_w = [Region() for _ in range(40)]
                for kc in range(8):
                    cx.dma("gpsimd", w_in_sb[:, kc, :], w_in_d[l, kc * 128:(kc + 1) * 128, :], [], [R_w[kc]])
                for kc in range(2):
                    cx.dma("gpsimd", wq_sb[:, kc, 0:512], wq_d[l, kc * 128:(kc + 1) * 128, :], [], [R_w[8 + kc]])
                    cx.dma("gpsimd", wq_sb[:, kc, 512:1024], wqi_d[l, kc * 128:(kc + 1) * 128, :], [], [R_w[10 + kc]])
                gpre = sbuf(st, "gpre", [128, D], F32)
                qln = sbuf(st, "qln", [128, 256], F32)
                cw = sbuf(st, "cw", [128, 8, 4], F32)
                cb = sbuf(st, "cb", [128, 8], F32)
                bigt = sbuf(st, "bigt", [128, 4], F32)
                bfgt = sbuf(st, "bfgt", [128, 4], F32)
                R_sp = Region()
                cx.dma("sync", gpre[:], npre_d[l:l + 1, :].partition_broadcast(128), [], [R_sp])
                cx.dma("sync", qln[:], qln_d[l:l + 1, :].partition_broadcast(128), [], [R_sp])
                cx.dma("sync", cw[:], convw_d[l], [], [R_sp])
                cx.dma("sync", cb[:], convb_d[l], [], [R_sp])
                cx.dma("sync", bigt[:], big_d[l:l + 1, :].partition_broadcast(128), [], [R_sp])
                cx.dma("sync", bfgt[:], bfg_d[l:l + 1, :].partition_broadcast(128), [], [R_sp])

                MODA = sbuf(st, "MODA", [128, 2, D], F32)
                R_moda = Region()
                x_st = [sbuf(st, "x_st0", [128, 4, D], F32)] * 2
                R_xst = [Region()] * 2
                junk = sbuf(st, "junk", [128, D], BF16)
                R_junk = Region()
                ss = sbuf(st, "ss", [128, 8], F32)
                R_ss = Region()
                tmpf = sbuf(st, "tmpf", [128, D], F32)
                R_tmpf = Region()
                hb = sbuf(st, "hb", [128, 4, D], BF16)
                R_hb = [Region() for _ in range(4)]
                hTs = [sbuf(st, "hT%d" % i, [128, 8, 512], BF16) for i in range(2)]
                R_hTs = [Region() for _ in range(2)]
                smlF = sbuf(st, "smlF", [128, 8], F32)
                R_smlF = Region()
                junk2 = sbuf(st, "junk2", [128, 256], BF16)
                R_junk2 = Region()
                qTs = sbuf(st, "qTs", [128, 4, 512], BF16)
                qiTs = sbuf(st, "qiTs", [128, 4, 512], BF16)
                kTs = sbuf(st, "kTs", [128, 4, 512], BF16)
                kiTs = sbuf(st, "kiTs", [64, 512], BF16)
                Vs = sbuf(st, "Vs_sb", [128, 4, 512], BF16)
                Vms = sbuf(st, "Vms", [128, 4, 512], BF16)
                ogs = sbuf(st, "ogs", [128, 4, 512], BF16)
                wis = sbuf(st, "wis", [128, 4, 8], F32)
                igs = sbuf(st, "igs", [128, 4, 4], F32)
                lfs = sbuf(st, "lfs", [128, 4, 4], F32)
                qmTs = sbuf(st, "qmTs", [128, 8, 512], BF16)
                R_out = {k: Region() for k in ("qT", "qiT", "kT", "kiT", "V", "Vm", "og", "wi", "ig", "lf", "qmT")}
                pre = sbuf(st, "pre", [128, 8, 515], F32)
                R_pre = Region()
                acc = sbuf(st, "acc", [128, 512], F32)
                R_acc = Region()
                cqn = [sbuf(st, "cqn%d" % i, [128, 256], BF16) for i in range(2)]
                R_cqn = [Region() for _ in range(2)]
                rOs = {k_: [sbuf(st, "rO_%s%d" % (k_, i), [128, 512 if k_ != "ki" else 64], BF16) for i in range(2)] for k_ in ("k", "q", "qi", "ki")}
                R_rOs = {k_: [Region() for _ in range(2)] for k_ in ("k", "q", "qi", "ki")}
                cqT = sbuf(st, "cqT", [128, 2, 128], BF16)
                R_cqT = Region()
                rAs = [sbuf(st, "rA%d" % i, [128, 512], F32) for i in range(2)]
                rBs = [sbuf(st, "rB%d" % i, [128, 512], F32) for i in range(2)]
                R_rAs = [Region() for _ in range(2)]
                R_rBs = [Region() for _ in range(2)]
                ropei = [0]
                sml = sbuf(st, "sml", [128, 16], F32)
                R_sml = Region()
                pf = [psum(st, "pf%d" % i, [128, 512], F32) for i in range(6)]
                R_pf = [Region() for _ in range(6)]
                ptb = [psum(st, "ptb%d" % i, [128, 1024], BF16) for i in range(2)]
                R_pt = [Region() for _ in range(2)]
                pfi = [0]
                pti = [0]

                def next_pf():
                    i = pfi[0] % 6
                    pfi[0] += 1
                    return pf[i], R_pf[i]

                def next_pt():
                    i = pti[0] % 2
                    pti[0] += 1
                    return ptb[i], R_pt[i]

                def rope(src, R_src, ncols, tix, dst, R_dst_w):
                    nh = ncols // 64
                    rb_ = ropei[0] % 2
                    ropei[0] += 1
                    rA, rB, R_rA, R_rB = rAs[rb_], rBs[rb_], R_rAs[rb_], R_rBs[rb_]
                    cos = COS[:, tix, :]
                    sin = SIN[:, tix, :]
                    s3 = src.rearrange("p (g f) -> p g f", f=32)
                    a3 = rA[:, 0:ncols].rearrange("p (g f) -> p g f", f=32)
                    V([R_src] + RC, [R_rA], lambda e: e.tensor_tensor(out=a3, in0=s3, in1=bc_mid(cos, 2 * nh), op=ALU.mult))
                    s4 = src.rearrange("p (h t f) -> p h t f", t=2, f=32)
                    b4 = rB[:, 0:ncols].rearrange("p (h t f) -> p h t f", t=2, f=32)
                    a4 = rA[:, 0:ncols].rearrange("p (h t f) -> p h t f", t=2, f=32)
                    d4 = dst.rearrange("p (h t f) -> p h t f", t=2, f=32)
                    V([R_src] + RC, [R_rB], lambda e: e.tensor_tensor(out=b4[:, :, 0, :], in0=s4[:, :, 1, :], in1=bc_mid(sin, nh), op=ALU.mult))
                    V([R_src] + RC, [R_rB], lambda e: e.tensor_tensor(out=b4[:, :, 1, :], in0=s4[:, :, 0, :], in1=bc_mid(sin, nh), op=ALU.mult))
                    G([R_rA, R_rB], R_dst_w, lambda e: e.tensor_tensor(out=d4[:, :, 0, :], in0=a4[:, :, 0, :], in1=b4[:, :, 0, :], op=ALU.subtract))
                    G([R_rA, R_rB], R_dst_w, lambda e: e.tensor_tensor(out=d4[:, :, 1, :], in0=a4[:, :, 1, :], in1=b4[:, :, 1, :], op=ALU.add))

                def rstd_from_ss(col0, n, inv_n, tl=None, Rt=None):
                    tl = sml if tl is None else tl
                    Rt = R_sml if Rt is None else Rt
                    sl = tl[:, col0:col0 + n]
                    V([Rt], [Rt], lambda e: e.tensor_scalar(out=sl, in0=sl, scalar1=inv_n, scalar2=EPS, op0=ALU.mult, op1=ALU.add))
                    A([Rt], [Rt], lambda e: e.activation(out=sl, in_=sl, func=AF.Ln))
                    A([Rt], [Rt], lambda e: e.activation(out=sl, in_=sl, func=AF.Exp, scale=-0.5))

                xcnt = 0

                def p1_front(sq, sti, hT, R_hT):
                    if sti == 0:
                        cx.dma("sync", MODA[:, 0, :], modd[l, sq:sq + 1, 0:D].partition_broadcast(128), [R_modd], [R_moda])
                        cx.dma("sync", MODA[:, 1, :], modd[l, sq:sq + 1, D:2 * D].partition_broadcast(128), [R_modd], [R_moda])
                        V([R_moda, R_sp], [R_moda], lambda e: e.scalar_tensor_tensor(out=MODA[:, 1, :], in0=MODA[:, 1, :], scalar=1.0, in1=gpre[:], op0=ALU.add, op1=ALU.mult))
                    T0 = sq * S + sti * 512
                    xb = 0
                    xs = x_st[xb]
                    rd = [] if R_xin is None else [R_xin[sq][sti * 4 + j] for j in range(4)]
                    cx.dma("sync", xs[:], xin_d[T0:T0 + 512, :].rearrange("(j p) d -> p j d", p=128), rd, [R_xst[xb]])
                    V([], [R_smlF], lambda e: e.memset(smlF[:, 0:4], 0.0))
                    for j in range(4):
                        A([R_xst[xb], R_smlF], [R_junk, R_smlF], lambda e: e.activation(out=junk[:], in_=xs[:, j, :], func=AF.Square, accum_out=smlF[:, j:j + 1]))
                    rstd_from_ss(0, 4, 1.0 / D, smlF, R_smlF)
                    for j in range(4):
                        V([R_xst[xb], R_smlF, R_moda], [R_tmpf], lambda e: e.scalar_tensor_tensor(out=tmpf[:], in0=xs[:, j, :], scalar=smlF[:, j:j + 1], in1=MODA[:, 1, :], op0=ALU.mult, op1=ALU.mult))
                        G([R_tmpf, R_moda], [R_hb[j]], lambda e: e.tensor_tensor(out=hb[:, j, :], in0=tmpf[:], in1=MODA[:, 0, :], op=ALU.add))
                    for j in range(4):
                        pt, Rp = next_pt()
                        for kc in range(8):
                            T([R_hb[j]] + RC, [Rp], lambda e: e.transpose(out=pt[:, kc * 128:(kc + 1) * 128], in_=hb[:, j, kc * 128:(kc + 1) * 128], identity=identb[:]))
                        A([Rp], [R_hT], lambda e: e.activation(out=hT[:, :, j * 128:(j + 1) * 128], in_=pt[:].rearrange("p (k t) -> p k t", t=128), func=AF.Copy))


                def p1_back(sq, sti, hT, R_hT):
                    T0 = sq * S + sti * 512
                    if sti == 0:
                        V([], [R_pre], lambda e: e.memset(pre[:, :, 0:3], 0.0))
                    def proj(tsl, pb, Rpb, c0, c1, o0):
                        for kc in range(8):
                            T([R_hT, *R_w], [Rpb], lambda e: e.matmul(pb[:, o0:o0 + (c1 - c0)], lhsT=hT[:, kc, tsl], rhs=w_in_sb[:, kc, c0:c1], start=(kc == 0), stop=(kc == 7)))

                    def stage_P(j):
                        tix = sq * NT + sti * 4 + j
                        tsl = slice(j * 128, (j + 1) * 128)
                        jb = j % 2
                        pa, Rpa = next_pf()
                        proj(tsl, pa, Rpa, 0, 256, 0)
                        proj(tsl, pa, Rpa, 1280, 1352, 256)
                        proj(tsl, pa, Rpa, 3400, 3408, 328)
                        pk, Rpk = next_pf()
                        proj(tsl, pk, Rpk, 256, 768, 0)
                        V([], [R_sml], lambda e: e.memset(sml[:, 4:5], 0.0))
                        A([Rpa, R_sml], [R_junk2, R_sml], lambda e: e.activation(out=junk2[:], in_=pa[:, 0:256], func=AF.Square, accum_out=sml[:, 4:5]))
                        rstd_from_ss(4, 1, 1.0 / 256)
                        V([Rpa, R_sml, R_sp], [R_cqn[jb]], lambda e: e.scalar_tensor_tensor(out=cqn[jb][:], in0=pa[:, 0:256], scalar=sml[:, 4:5], in1=qln[:], op0=ALU.mult, op1=ALU.mult))
                        rope(pa[:, 256:320], Rpa, 64, tix, rOs["ki"][jb][:, 0:64], [R_rOs["ki"][jb]])
                        A([Rpa], [R_out["wi"]], lambda e: e.activation(out=wis[:, j, :], in_=pa[:, 320:328], func=AF.Identity, scale=512.0 ** -0.5))
                        V([Rpa, R_sp], [R_out["ig"]], lambda e: e.tensor_tensor(out=igs[:, j, :], in0=pa[:, 328:332], in1=bigt[:], op=ALU.add))
                        V([Rpa, R_sp], [R_sml], lambda e: e.tensor_tensor(out=sml[:, 8:12], in0=pa[:, 332:336], in1=bfgt[:], op=ALU.add))
                        A([R_sml], [R_sml], lambda e: e.activation(out=sml[:, 8:12], in_=sml[:, 8:12], func=AF.Exp, scale=-1.0))
                        A([R_sml], [R_sml], lambda e: e.activation(out=sml[:, 8:12], in_=sml[:, 8:12], func=AF.Ln, bias=1.0))
                        V([R_sml], [R_out["lf"]], lambda e: e.tensor_scalar(out=lfs[:, j, :], in0=sml[:, 8:12], scalar1=-1.0, scalar2=None, op0=ALU.mult))
                        rope(pk[:], Rpk, 512, tix, rOs["k"][jb][:], [R_rOs["k"][jb]])
                        pv, Rpv = next_pf()
                        proj(tsl, pv, Rpv, 768, 1280, 0)
                        A([Rpv], [R_out["V"]], lambda e: e.activation(out=Vs[:, j, :], in_=pv[:], func=AF.Copy))
                        pv, Rpv = next_pf()
                        proj(tsl, pv, Rpv, 2376, 2888, 0)
                        A([Rpv], [R_out["Vm"]], lambda e: e.activation(out=Vms[:, j, :], in_=pv[:], func=AF.Copy))
                        pv, Rpv = next_pf()
                        proj(tsl, pv, Rpv, 2888, 3400, 0)
                        A([Rpv], [R_out["og"]], lambda e: e.activation(out=ogs[:, j, :], in_=pv[:], func=AF.Sigmoid))

                    def stage_Q(j):
                        tix = sq * NT + sti * 4 + j
                        jb = j % 2
                        pt, Rp = next_pt()
                        for kc in range(2):
                            T([R_cqn[jb]] + RC, [Rp], lambda e: e.transpose(out=pt[:, kc * 128:(kc + 1) * 128], in_=cqn[jb][:, kc * 128:(kc + 1) * 128], identity=identb[:]))
                        A([Rp], [R_cqT], lambda e: e.activation(out=cqT[:], in_=pt[:, 0:256].rearrange("p (k t) -> p k t", t=128), func=AF.Copy))
                        for which, key in ((0, "q"), (1, "qi")):
                            pq, Rpq = next_pf()
                            for kc in range(2):
                                T([R_cqT, *R_w], [Rpq], lambda e: e.matmul(pq[:], lhsT=cqT[:, kc, :], rhs=wq_sb[:, kc, which * 512:(which + 1) * 512], start=(kc == 0), stop=(kc == 1)))
                            rope(pq[:], Rpq, 512, tix, rOs[key][jb][:], [R_rOs[key][jb]])

                    def stage_R(j):
                        tsl = slice(j * 128, (j + 1) * 128)
                        jb = j % 2
                        pt, Rp = next_pt()
                        T([R_rOs["ki"][jb]] + RC, [Rp], lambda e: e.transpose(out=pt[0:64, 0:128], in_=rOs["ki"][jb][:, 0:64], identity=identb[:]))
                        A([Rp], [R_out["kiT"]], lambda e: e.activation(out=kiTs[:, tsl], in_=pt[0:64, 0:128], func=AF.Copy))
                        for key, dstT, okey in (("k", kTs, "kT"), ("q", qTs, "qT"), ("qi", qiTs, "qiT")):
                            pt, Rp = next_pt()
                            for pr in range(4):
                                T([R_rOs[key][jb]] + RC, [Rp], lambda e: e.transpose(out=pt[:, pr * 128:(pr + 1) * 128], in_=rOs[key][jb][:, pr * 128:(pr + 1) * 128], identity=identb[:]))
                            A([Rp], [R_out[okey]], lambda e: e.activation(out=dstT[:, :, tsl], in_=pt[:, 0:512].rearrange("p (k t) -> p k t", t=128), func=AF.Copy))

                    def stage_FM(ch):
                        pc, Rpc = next_pf()
                        c0 = 1352 + ch * 128
                        for kc in range(8):
                            T([R_hT, *R_w], [Rpc], lambda e: e.matmul(pc[:], lhsT=w_in_sb[:, kc, c0:c0 + 128], rhs=hT[:, kc, :], start=(kc == 0), stop=(kc == 7)))
                        A([Rpc], [R_pre], lambda e: e.activation(out=pre[:, ch, 3:515], in_=pc[:], func=AF.Copy))
                        V([R_pre, R_sp], [R_acc], lambda e: e.tensor_scalar(out=acc[:], in0=pre[:, ch, 0:512], scalar1=cw[:, ch, 0:1], scalar2=None, op0=ALU.mult))
                        for jj in range(1, 4):
                            V([R_pre, R_sp, R_acc], [R_acc], lambda e: e.scalar_tensor_tensor(out=acc[:], in0=pre[:, ch, jj:jj + 512], scalar=cw[:, ch, jj:jj + 1], in1=acc[:], op0=ALU.mult, op1=ALU.add))
                        A([R_acc, R_sp], [R_out["qmT"]], lambda e: e.activation(out=qmTs[:, ch, :], in_=acc[:], func=AF.Silu, bias=cb[:, ch:ch + 1]))

                    for j in range(4):
                        stage_P(j)
                        stage_FM(2 * j)
                        stage_FM(2 * j + 1)
                        stage_Q(j)
                        if j >= 1:
                            stage_R(j - 1)
                    stage_R(3)
                    V([R_pre], [R_pre], lambda e: e.tensor_copy(out=pre[:, :, 0:3], in_=pre[:, :, 512:515]))

                    tsl2 = slice(sti * 512, (sti + 1) * 512)
                    if sti == 0:
                        R_p1[sq] = []

                    def Rp1_new():
                        r_ = Region()
                        R_p1[sq].append(r_)
                        return [r_]
                    cx.dma("gpsimd", qT_d[sq, :, :, tsl2], qTs[:], [R_out["qT"]], Rp1_new())
                    cx.dma("gpsimd", qiT_d[sq, :, :, tsl2], qiTs[:], [R_out["qiT"]], Rp1_new())
                    cx.dma("gpsimd", kT_d[sq, :, :, tsl2], kTs[:], [R_out["kT"]], Rp1_new())
                    cx.dma("gpsimd", kiT_d[sq, :, tsl2], kiTs[:], [R_out["kiT"]], Rp1_new())
                    cx.dma("gpsimd", qmT_d[sq, :, :, tsl2], qmTs[:], [R_out["qmT"]], Rp1_new())
                    for (dd, sb_, key) in ((V_d, Vs, "V"), (Vm_d, Vms, "Vm"), (og_d, ogs, "og"), (wi_d, wis, "wi"), (ig_d, igs, "ig"), (lf_d, lfs, "lf")):
                        cx.dma("gpsimd", dd[sq, tsl2, :].rearrange("(j p) c -> p j c", p=128), sb_[:], [R_out[key]], Rp1_new())

                items = [(sq_, sti_) for sq_ in range(NSEQ) for sti_ in range(4)]
                p1_front(items[0][0], items[0][1], hTs[0], R_hTs[0])
                for k_, (sq_, sti_) in enumerate(items):
                    if k_ + 1 < len(items):
                        p1_front(items[k_ + 1][0], items[k_ + 1][1], hTs[(k_ + 1) % 2], R_hTs[(k_ + 1) % 2])
                    p1_back(sq_, sti_, hTs[k_ % 2], R_hTs[k_ % 2])
            cx.barrier()

            with ExitStack() as st:
                qT = sbuf(st, "qT_sb", [128, 4, S], BF16)
                qiT = sbuf(st, "qiT_sb", [128, 4, S], BF16)
                kT = sbuf(st, "kT_sb", [128, 4, S], BF16)
                kiT = sbuf(st, "kiT_sb", [128, S], BF16)
                Vx = sbuf(st, "Vx", [128, NT, 8, 65], BF16)
                wi = sbuf(st, "wi_sb", [128, NT, 8], F32)
                aon = sbuf(st, "aon", [128, 512], F32)
                R_in = Region()
                R_sp = Region()
                cx.dma("sync", aon[:], aon_d[l:l + 1, :].partition_broadcast(128), [], [R_sp])
                dg = [sbuf(st, "dg%d" % i, [128, 8, 128], BF16) for i in range(2)]
                R_dg = [Region() for _ in range(2)]
                NRB = 8
                rl = [sbuf(st, "rl%d" % i, [128, 512], BF16) for i in range(NRB)]
                R_rl = [Region() for _ in range(NRB)]
                sc = [sbuf(st, "sc%d" % i, [128, S], F32) for i in range(4)]
                R_sc = [Region() for _ in range(4)]
                junkA = sbuf(st, "junkA", [128, S], BF16)
                R_junkA = Region()
                bis = [dict(lo=sbuf(st, "b_lo%d" % i, [128, 4], F32), dk=sbuf(st, "b_dk%d" % i, [128, 32], F32),
                            ndk=sbuf(st, "b_ndk%d" % i, [128, 32], F32), S=sbuf(st, "b_S%d" % i, [128, 32], F32),
                            nmid=sbuf(st, "b_nm%d" % i, [128, 1], F32), g=sbuf(st, "b_g%d" % i, [128, 1], F32),
                            inc=sbuf(st, "b_inc%d" % i, [128, 1], F32)) for i in range(2)]
                R_bis = [dict(lo=Region(), dk=Region(), S=Region(), nmid=Region(), g=Region(), inc=Region()) for _ in range(2)]
                NRND = 26
                wk = sbuf(st, "wk", [128, S], F32)
                R_wk = Region()
                m8 = [sbuf(st, "m8_%d" % i, [128, 8], F32) for i in range(2)]
                R_m8 = [Region() for _ in range(2)]
                mb = [sbuf(st, "mb%d" % i, [128, S], BF16) for i in range(4)]
                R_mb = [Region() for _ in range(4)]
                PT = [sbuf(st, "PT%d" % i, [128, NT, 128], BF16) for i in range(2)]
                R_PT = [Region() for _ in range(2)]
                atf = [sbuf(st, "atf%d" % i, [128, 8, 64], F32) for i in range(2)]
                R_atf = [Region() for _ in range(2)]
                lnd = [sbuf(st, "lnd%d" % i, [128, 16], F32) for i in range(2)]
                R_ev = [Region() for _ in range(2)]
                rs = [sbuf(st, "rs%d" % i, [128, 8], F32) for i in range(2)]
                R_rs = [Region() for _ in range(2)]
                sqf = sbuf(st, "sqf", [128, 8, 64], F32)
                R_sqf = Region()
                att_st = [sbuf(st, "att_st%d" % i, [128, 512], BF16) for i in range(2)]
                R_ast = [Region() for _ in range(2)]
                sml = sbuf(st, "sml2", [128, 24], F32)
                R_sml = Region()
                pl = [psum(st, "pl%d" % i, [128, 512], F32) for i in range(3)]
                R_pl = [Region() for _ in range(3)]
                psc = psum(st, "psc", [128, 512], F32)
                R_psc = Region()
                plt = [psum(st, "plt%d" % i, [128, 512], F32) for i in range(2)]
                R_plt = [Region() for _ in range(2)]
                pav = [psum(st, "pav%d" % i, [128, 512], F32) for i in range(2)]
                R_pav = [Region() for _ in range(2)]
                cnts = {"pl": 0, "rl": 0, "lt": 0, "pt": 0}

                def emit_scores(sq, i):
                    ns = (i + 1) * 128
                    tq = slice(i * 128, (i + 1) * 128)
                    dgb, Rdg = dg[i % 2], R_dg[i % 2]
                    scb, Rsc = sc[i % 4], R_sc[i % 4]
                    for h in range(8):
                        V([R_in] + RC, [Rdg], lambda e: e.tensor_scalar(out=dgb[:, h, :], in0=identf[:], scalar1=wi[:, i, h:h + 1], scalar2=None, op0=ALU.mult))
                    for c0 in range(0, ns, 512):
                        cols = min(512, ns - c0)
                        rbuf = []
                        for h in range(8):
                            po = (h % 2) * 64
                            pb, Rpb = pl[cnts["pl"] % 3], R_pl[cnts["pl"] % 3]
                            cnts["pl"] += 1
                            T([R_in], [Rpb], lambda e: e.matmul(pb[:, 0:cols], lhsT=qiT[po:po + 64, h // 2, tq], rhs=kiT[po:po + 64, c0:c0 + cols], start=True, stop=True))
                            rb, Rrb = rl[cnts["rl"] % NRB], R_rl[cnts["rl"] % NRB]
                            cnts["rl"] += 1
                            if h % 2 == 0:
                                A([Rpb], [Rrb], lambda e: e.activation(out=rb[:, 0:cols], in_=pb[:, 0:cols], func=AF.Relu))
                            else:
                                V([Rpb], [Rrb], lambda e: e.tensor_scalar(out=rb[:, 0:cols], in0=pb[:, 0:cols], scalar1=0.0, scalar2=None, op0=ALU.max))
                            rbuf.append((rb, Rrb))
                        for h in range(8):
                            rb, Rrb = rbuf[h]
                            T([Rrb, Rdg], [R_psc], lambda e: e.matmul(psc[:, 0:cols], lhsT=dgb[:, h, :], rhs=rb[:, 0:cols], start=(h == 0), stop=(h == 7)))
                        V([R_psc], [Rsc], lambda e: e.tensor_copy(out=scb[:, c0:c0 + cols], in_=psc[:, 0:cols]))
                    V([Rsc] + RC, [Rsc], lambda e: e.tensor_tensor(out=scb[:, tq], in0=scb[:, tq], in1=negtri[:], op=ALU.add))

                def emit_mask(i, thr, Rthr):
                    ns = (i + 1) * 128
                    scb, Rsc = sc[i % 4], R_sc[i % 4]
                    V([Rsc, Rthr], [R_mb[i % 4]], lambda e: e.tensor_scalar(out=mb[i % 4][:, 0:ns], in0=scb[:, 0:ns], scalar1=thr, scalar2=-30000.0, op0=ALU.is_lt, op1=ALU.mult))

                def emit_topk_dve(i):
                    ns = (i + 1) * 128
                    bb = i % 2
                    scb, Rsc = sc[i % 4], R_sc[i % 4]
                    if i >= 2:
                        nr = TOPK // 8
                        for r in range(nr):
                            src = scb if r == 0 else wk
                            Rs = Rsc if r == 0 else R_wk
                            V([Rs], [R_m8[bb]], lambda e: e.max(out=m8[bb][:], in_=src[:, 0:ns]))
                            if r < nr - 1:
                                V([Rs, R_m8[bb]], [R_wk], lambda e: e.match_replace(out=wk[:, 0:ns], in_to_replace=m8[bb][:], in_values=src[:, 0:ns], imm_value=NEG))
                        emit_mask(i, m8[bb][:, 7:8], R_m8[bb])
                    else:
                        emit_mask(i, thr0[:, 0:1], R_c6)

                def emit_topk_act(i, steps=()):
                    ns = (i + 1) * 128
                    scb, Rsc = sc[i % 4], R_sc[i % 4]
                    bt, Rb = bis[(i // 2) % 2], R_bis[(i // 2) % 2]
                    lo, dk, ndk, Sb, nmid, g, inc = bt["lo"], bt["dk"], bt["ndk"], bt["S"], bt["nmid"], bt["g"], bt["inc"]
                    V([Rsc], [Rb["lo"]], lambda e: e.tensor_reduce(out=lo[:, 0:1], in_=scb[:, 0:256], axis=AX.X, op=ALU.min))
                    V([Rsc], [Rb["lo"]], lambda e: e.tensor_reduce(out=lo[:, 1:2], in_=scb[:, 0:ns], axis=AX.X, op=ALU.max))
                    V([Rb["lo"]], [Rb["lo"]], lambda e: e.tensor_tensor(out=lo[:, 2:3], in0=lo[:, 1:2], in1=lo[:, 0:1], op=ALU.subtract))
                    V([Rb["lo"]] + RC, [Rb["dk"]], lambda e: e.tensor_scalar(out=dk[:], in0=pw2[:], scalar1=lo[:, 2:3], scalar2=None, op0=ALU.mult))
                    V([Rb["dk"]], [Rb["dk"]], lambda e: e.tensor_scalar(out=ndk[:], in0=dk[:], scalar1=-1.0, scalar2=None, op0=ALU.mult))
                    V([], [Rb["S"]], lambda e: e.memset(Sb[:], 0.0))
                    for k in range(NRND):
                        A([Rb["lo"], Rb["dk"]], [Rb["nmid"]], lambda e: e.activation(out=nmid[:], in_=lo[:, 0:1], func=AF.Identity, scale=-1.0, bias=ndk[:, k:k + 1]))
                        A([Rsc, Rb["nmid"], Rb["S"]], [R_junkA, Rb["S"]], lambda e: e.activation(out=junkA[:, 0:ns], in_=scb[:, 0:ns], func=AF.Sign, bias=nmid[:, 0:1], accum_out=Sb[:, k:k + 1]))
                        A([Rb["S"]], [Rb["g"]], lambda e: e.activation(out=g[:], in_=Sb[:, k:k + 1], func=AF.Sign, bias=float(ns - 511)))
                        A([Rb["g"], Rb["dk"]], [Rb["inc"]], lambda e: e.activation(out=inc[:], in_=g[:], func=AF.Relu, scale=dk[:, k:k + 1]))
                        A([Rb["inc"], Rb["lo"]], [Rb["lo"]], lambda e: e.activation(out=lo[:, 0:1], in_=inc[:], func=AF.Identity, bias=lo[:, 0:1]))
                        if k >= 2 and steps:
                            steps.pop(0)()
                    while steps:
                        steps.pop(0)()

                def emit_mask_act(i):
                    bt, Rb = bis[(i // 2) % 2], R_bis[(i // 2) % 2]
                    emit_mask(i, bt["lo"][:, 0:1], Rb["lo"])

                def attn_steps(sq, i):
                    tq = slice(i * 128, (i + 1) * 128)
                    bb = i % 2
                    mbb, Rmb = mb[i % 4], R_mb[i % 4]

                    def logits(h):
                        po = (h % 2) * 64
                        pr = h // 2
                        ptb_, Rptb = PT[h % 2], R_PT[h % 2]
                        for g0 in range(0, i + 1, 4):
                            g1 = min(g0 + 4, i + 1)
                            lt, Rlt = plt[cnts["lt"] % 2], R_plt[cnts["lt"] % 2]
                            cnts["lt"] += 1
                            for j in range(g0, g1):
                                o = (j - g0) * 128
                                ks = slice(j * 128, (j + 1) * 128)
                                T([R_in], [Rlt], lambda e: e.matmul(lt[:, o:o + 128], lhsT=kT[po:po + 64, pr, ks], rhs=qT[po:po + 64, pr, tq], start=True, stop=False))
                                T([Rmb] + RC, [Rlt], lambda e: e.matmul(lt[:, o:o + 128], lhsT=mbb[:, ks], rhs=identb[:], start=False, stop=True))
                            ncol = (g1 - g0) * 128
                            A([Rlt], [Rptb], lambda e: e.activation(out=ptb_[:, g0:g1, :], in_=lt[:, 0:ncol].rearrange("p (j t) -> p j t", t=128), func=AF.Exp, scale=0.125))

                    def pv(h):
                        ptb_, Rptb = PT[h % 2], R_PT[h % 2]
                        av = pav[h // 4]
                        Rav = R_pav[h // 4]
                        o = (h % 4) * 65
                        for j in range(i + 1):
                            T([Rptb, R_in], [Rav], lambda e: e.matmul(av[:, o:o + 65], lhsT=ptb_[:, j, :], rhs=Vx[:, j, h, :], start=(j == 0), stop=(j == i)))

                    def evac():
                        for hh in range(2):
                            av3 = pav[hh][:, 0:260].rearrange("p (h c) -> p h c", c=65)
                            A([R_pav[hh]], [R_ev[bb]], lambda e: e.activation(out=lnd[bb][:, hh * 4:hh * 4 + 4].unsqueeze(2), in_=av3[:, :, 64:65], func=AF.Ln))
                        A([R_ev[bb]], [R_ev[bb]], lambda e: e.activation(out=lnd[bb][:, 8:16], in_=lnd[bb][:, 0:8], func=AF.Exp, scale=-1.0))
                        for h in range(8):
                            av3 = pav[h // 4][:, 0:260].rearrange("p (h c) -> p h c", c=65)
                            A([R_pav[h // 4], R_ev[bb]], [R_atf[bb]], lambda e: e.activation(out=atf[bb][:, h, :], in_=av3[:, h % 4, 0:64], func=AF.Identity, scale=lnd[bb][:, 8 + h:9 + h]))

                    steps = [lambda: logits(0)]
                    for h in range(8):
                        def st_(h=h):
                            if h + 1 < 8:
                                logits(h + 1)
                            pv(h)
                        steps.append(st_)
                    steps.append(evac)
                    return steps

                def attn_norm(sq, i):
                    bb = i % 2
                    G([R_atf[bb]], [R_sqf], lambda e: e.tensor_tensor(out=sqf[:], in0=atf[bb][:], in1=atf[bb][:], op=ALU.mult))
                    V([R_sqf], [R_rs[bb]], lambda e: e.tensor_reduce(out=rs[bb][:], in_=sqf[:], axis=AX.X, op=ALU.add))
                    V([R_rs[bb]], [R_rs[bb]], lambda e: e.tensor_scalar(out=rs[bb][:], in0=rs[bb][:], scalar1=1.0 / 64, scalar2=EPS, op0=ALU.mult, op1=ALU.add))

                def emit_attn_fin(sq, i):
                    tq = slice(i * 128, (i + 1) * 128)
                    bb = i % 2
                    A([R_rs[bb]], [R_rs[bb]], lambda e: e.activation(out=rs[bb][:], in_=rs[bb][:], func=AF.Ln))
                    A([R_rs[bb]], [R_rs[bb]], lambda e: e.activation(out=rs[bb][:], in_=rs[bb][:], func=AF.Exp, scale=-0.5))
                    G([R_atf[bb], R_rs[bb]], [R_sqf], lambda e: e.tensor_tensor(out=sqf[:], in0=atf[bb][:], in1=bc_last(rs[bb][:], 64), op=ALU.mult))
                    G([R_sqf, R_sp], [R_ast[bb]], lambda e: e.tensor_tensor(out=att_st[bb][:], in0=sqf[:].rearrange("p h d -> p (h d)"), in1=aon[:], op=ALU.mult))
                    cx.dma("gpsimd", att_d[sq, tq, :], att_st[bb][:], [R_ast[bb]], [R_att[sq][i]])

                dummy = sbuf(st, "dummy2", [128, 1], F32)
                for sq in range(NSEQ):
                    if sq > 0:
                        cx.barrier()
                    R_ms = Region()
                    V([], [R_ms], lambda e: e.memset(Vx[:], 1.0))
                    rd = list(R_p1[sq]) + [R_ms]
                    Ls = []

                    def ld(out_, in__):
                        r_ = Region()
                        Ls.append(r_)
                        cx.dma("sync", out_, in__, rd, [r_])
                    ld(qT[:], qT_d[sq])
                    ld(qiT[:], qiT_d[sq])
                    ld(kT[:], kT_d[sq])
                    ld(kiT[0:64, :], kiT_d[sq])
                    ld(kiT[64:128, :], kiT_d[sq])
                    ld(wi[:], wi_d[sq].rearrange("(j p) c -> p j c", p=128))
                    for jt in range(NT):
                        ld(Vx[:, jt, :, 0:64], V_d[sq, jt * 128:(jt + 1) * 128, :].rearrange("p (h d) -> p h d", d=64))
                    V(Ls, [R_in], lambda e: e.memset(dummy[:], 0.0))
                    emit_scores(sq, 0)
                    emit_scores(sq, 1)
                    emit_topk_dve(0)
                    emit_topk_dve(1)
                    for p in range(NT // 2):
                        if p >= 1:
                            emit_attn_fin(sq, 2 * p - 2)
                            emit_attn_fin(sq, 2 * p - 1)
                        steps = attn_steps(sq, 2 * p) + attn_steps(sq, 2 * p + 1)
                        if p + 1 < NT // 2:
                            ia, ib = 2 * p + 2, 2 * p + 3
                            emit_scores(sq, ia)
                            emit_scores(sq, ib)
                            if ia >= 4:
                                emit_topk_act(ib, steps)
                                emit_topk_dve(ia)
                                emit_mask_act(ib)
                            else:
                                emit_topk_dve(ia)
                                emit_topk_dve(ib)
                        while steps:
                            steps.pop(0)()
                        attn_norm(sq, 2 * p)
                        attn_norm(sq, 2 * p + 1)
                    emit_attn_fin(sq, NT - 2)
                    emit_attn_fin(sq, NT - 1)
            cx.barrier()

            st_w = ExitStack()
            w_dn_sb = sbuf(st_w, "w_dn_sb", [128, NFC, D], BF16)
            R_wo = [Region() for _ in range(8)]
            R_wd = [Region() for _ in range(NFC)]
            for fc in range(NFC):
                cx.dma("gpsimd", w_dn_sb[:, fc, :], w_dn_d[l, fc * 128:(fc + 1) * 128, :], [], [R_wd[fc]])

            with ExitStack() as st:
                qmT = sbuf(st, "qmT_sb", [128, 8, S], BF16)
                Vmx = sbuf(st, "Vmx", [128, NT, 4, 129], BF16)
                og = sbuf(st, "og_sb", [128, NT, 512], BF16)
                ig = sbuf(st, "ig_sb", [128, NT, 4], F32)
                lf = sbuf(st, "lf_sb", [128, NT, 4], F32)
                mon = sbuf(st, "mon", [128, 512], F32)
                R_in = Region()
                R_sp = Region()
                cx.dma("sync", mon[:], mon_d[l:l + 1, :].partition_broadcast(128), [], [R_sp])
                Cf = sbuf(st, "Cf", [128, 4, 129], F32)
                Cb = sbuf(st, "Cb", [128, 4, 129], BF16)
                R_Cf = [Region() for _ in range(4)]
                R_Cb = [Region() for _ in range(4)]
                LU = sbuf(st, "LU", [128, 4, 128], F32)
                R_LU = [Region() for _ in range(4)]
                ebt = sbuf(st, "ebt", [128, 4, 128], F32)
                R_ebt = Region()
                DT = sbuf(st, "DT", [128, 4, 128], F32)
                R_DT = Region()
                STm = sbuf(st, "STm", [128, 4, 128], BF16)
                R_ST = [Region() for _ in range(4)]
                qtl = sbuf(st, "qtl", [128, 4, 128], BF16)
                R_qtl = [Region() for _ in range(4)]
                kw = sbuf(st, "kw", [128, 4, 128], BF16)
                R_kw = [Region() for _ in range(4)]
                sm = sbuf(st, "sm3", [128, 40], F32)
                R_sm = Region()
                hmf = sbuf(st, "hmf", [128, 4, 128], F32)
                hmq = sbuf(st, "hmq", [128, 4, 128], F32)
                R_hmf, R_hmq = Region(), Region()
                hm_st = sbuf(st, "hm_st", [128, 512], BF16)
                R_hst = Region()
                lnscale = math.log(128.0 ** -0.5)
                lnsc = sbuf(st, "lnsc", [128, 1], F32)
                V([], [R_sp], lambda e: e.memset(lnsc[:], lnscale))
                pB = psum(st, "pB", [128, 512], F32)
                pB2 = psum(st, "pB2", [128, 512], F32)
                pS = psum(st, "pS", [128, 512], F32)
                pH = [psum(st, "pH%d" % i, [128, 512], F32) for i in range(2)]
                pC = [psum(st, "pC%d" % i, [128, 512], F32) for i in range(2)]
                pK = psum(st, "pK", [128, 1024], BF16)
                R_pB, R_pB2, R_pS, R_pK = Region(), Region(), Region(), Region()
                R_pH = [Region() for _ in range(2)]
                R_pC = [Region() for _ in range(2)]
                R_bcol = Region()
                pB3 = pB[:].rearrange("p (h t) -> p h t", t=128)
                pB23 = pB2[:].rearrange("p (h t) -> p h t", t=128)
                pS3 = pS[:].rearrange("p (h t) -> p h t", t=128)
                dummy = sbuf(st, "dummy3", [128, 1], F32)
                for sq in range(NSEQ):
                    if sq > 0:
                        cx.barrier()
                    R_ms = Region()
                    V([], [R_ms], lambda e: e.memset(Vmx[:], 1.0))
                    rd = list(R_p1[sq]) + [R_ms]
                    Ls = []

                    def ld(out_, in__):
                        r_ = Region()
                        Ls.append(r_)
                        cx.dma("sync", out_, in__, rd, [r_])
                    ld(qmT[:], qmT_d[sq])
                    ld(ig[:], ig_d[sq].rearrange("(j p) c -> p j c", p=128))
                    ld(lf[:], lf_d[sq].rearrange("(j p) c -> p j c", p=128))
                    for jt in range(NT):
                        ld(Vmx[:, jt, :, 0:128], Vm_d[sq, jt * 128:(jt + 1) * 128, :].rearrange("p (h d) -> p h d", d=128))
                    ld(og[:], og_d[sq].rearrange("(j p) c -> p j c", p=128))
                    V(Ls, [R_in], lambda e: e.memset(dummy[:], 0.0))
                    for h in range(4):
                        V([], [R_Cf[h]], lambda e: e.memset(Cf[:, h, :], 0.0))
                        V([], [R_Cb[h]], lambda e: e.memset(Cb[:, h, :], 0.0))
                    for c in range(NT):
                        tq = slice(c * 128, (c + 1) * 128)
                        for h in range(4):
                            V([R_in] + RC, [R_LU[h]], lambda e: e.tensor_scalar(out=LU[:, h, :], in0=utri[:], scalar1=lf[:, c, h:h + 1], scalar2=None, op0=ALU.mult))
                            T([R_LU[h]] + RC, [R_pB], lambda e: e.matmul(pB3[:, h, :], lhsT=onesf[:], rhs=LU[:, h, :], start=True, stop=True))
                            T([R_LU[h]] + RC, [R_pB2], lambda e: e.matmul(pB23[:, h, :], lhsT=onesf[:], rhs=LU[:, h, :], start=True, stop=False))
                            T(RC, [R_pB2], lambda e: e.matmul(pB23[:, h, :], lhsT=identf[:], rhs=negm[:], start=False, stop=True))
                        T([R_in] + RC, [R_bcol], lambda e: e.matmul(pH[0][:, 300:304], lhsT=utri[:], rhs=lf[:, c, :], start=True, stop=True))
                        V([R_bcol, R_in], [R_sm], lambda e: e.tensor_tensor(out=sm[:, 0:4], in0=ig[:, c, :], in1=pH[0][:, 300:304], op=ALU.subtract))
                        V([R_sm], [R_sm], lambda e: e.tensor_scalar(out=sm[:, 4:8], in0=sm[:, 0:4], scalar1=lnscale, scalar2=None, op0=ALU.add))
                        V([R_sm, R_pB], [R_sm], lambda e: e.tensor_tensor(out=sm[:, 8:12].unsqueeze(2), in0=sm[:, 0:4].unsqueeze(2), in1=pB3[:, :, 127:128], op=ALU.add))
                        A([R_sm], [R_sm], lambda e: e.activation(out=sm[:, 12:16], in_=sm[:, 8:12], func=AF.Exp))
                        A([R_pB], [R_sm], lambda e: e.activation(out=sm[:, 16:20].unsqueeze(2), in_=pB3[:, :, 127:128], func=AF.Exp))
                        A([R_pB, R_sp], [R_ebt], lambda e: e.activation(out=ebt[:].rearrange("p h t -> p (h t)"), in_=pB[:], func=AF.Exp, bias=lnsc[:, 0:1]))
                        for h in range(4):
                            A([R_pB2, R_sm], [R_DT], lambda e: e.activation(out=DT[:, h, :], in_=pB23[:, h, :], func=AF.Exp, bias=sm[:, 4 + h:5 + h]))
                        for h in range(4):
                            T([R_in], [R_pS], lambda e: e.matmul(pS3[:, h, :], lhsT=qmT[:, 4 + h, tq], rhs=qmT[:, h, tq], start=True, stop=True))
                            T([R_in] + RC, [R_pK], lambda e: e.transpose(out=pK[:, h * 128:(h + 1) * 128], in_=qmT[:, 4 + h, tq], identity=identb[:]))
                        for h in range(4):
                            V([R_pS, R_DT], [R_ST[h]], lambda e: e.tensor_tensor(out=STm[:, h, :], in0=pS3[:, h, :], in1=DT[:, h, :], op=ALU.mult))
                            V([R_in, R_ebt], [R_qtl[h]], lambda e: e.tensor_tensor(out=qtl[:, h, :], in0=qmT[:, h, tq], in1=ebt[:, h, :], op=ALU.mult))
                            V([R_pK, R_sm], [R_kw[h]], lambda e: e.tensor_scalar(out=kw[:, h, :], in0=pK[:, h * 128:(h + 1) * 128], scalar1=sm[:, 12 + h:13 + h], scalar2=None, op0=ALU.mult))
                        for h in range(4):
                            ph = pH[h // 2]
                            o = (h % 2) * 129
                            T([R_ST[h], R_in], [R_pH[h // 2]], lambda e: e.matmul(ph[:, o:o + 129], lhsT=STm[:, h, :], rhs=Vmx[:, c, h, :], start=True, stop=False))
                            T([R_qtl[h], R_Cb[h]], [R_pH[h // 2]], lambda e: e.matmul(ph[:, o:o + 129], lhsT=qtl[:, h, :], rhs=Cb[:, h, :], start=False, stop=True))
                        for h in range(4):
                            pc = pC[h // 2]
                            o = (h % 2) * 129
                            T([R_kw[h], R_in], [R_pC[h // 2]], lambda e: e.matmul(pc[:, o:o + 129], lhsT=kw[:, h, :], rhs=Vmx[:, c, h, :], start=True, stop=True))
                            V([R_pC[h // 2], R_sm, R_Cf[h]], [R_Cf[h]], lambda e: e.scalar_tensor_tensor(out=Cf[:, h, :], in0=Cf[:, h, :], scalar=sm[:, 16 + h:17 + h], in1=pc[:, o:o + 129], op0=ALU.mult, op1=ALU.add))
                            A([R_Cf[h]], [R_Cb[h]], lambda e: e.activation(out=Cb[:, h, :], in_=Cf[:, h, :], func=AF.Copy))
                        for hh in range(2):
                            ph3 = pH[hh][:, 0:258].rearrange("p (h c) -> p h c", c=129)
                            V([R_pH[hh]], [R_sm], lambda e: e.tensor_copy(out=sm[:, 32 + hh * 2:34 + hh * 2].unsqueeze(2), in_=ph3[:, :, 128:129]))
                            V([R_sm], [R_sm], lambda e: e.scalar_tensor_tensor(out=sm[:, 20 + hh * 2:22 + hh * 2], in0=sm[:, 32 + hh * 2:34 + hh * 2], scalar=-1.0, in1=sm[:, 32 + hh * 2:34 + hh * 2], op0=ALU.mult, op1=ALU.max))
                            V([R_sm], [R_sm], lambda e: e.tensor_scalar(out=sm[:, 20 + hh * 2:22 + hh * 2], in0=sm[:, 20 + hh * 2:22 + hh * 2], scalar1=1.0, scalar2=None, op0=ALU.max))
                            V([R_sm], [R_sm], lambda e: e.reciprocal(out=sm[:, 24 + hh * 2:26 + hh * 2], in_=sm[:, 20 + hh * 2:22 + hh * 2]))
                            V([R_pH[hh], R_sm], [R_hmf], lambda e: e.tensor_tensor(out=hmf[:, hh * 2:hh * 2 + 2, :], in0=ph3[:, :, 0:128], in1=bc_last(sm[:, 24 + hh * 2:26 + hh * 2], 128), op=ALU.mult))
                        V([R_hmf], [R_hmq], lambda e: e.tensor_tensor(out=hmq[:], in0=hmf[:], in1=hmf[:], op=ALU.mult))
                        V([R_hmq], [R_sm], lambda e: e.tensor_reduce(out=sm[:, 28:32], in_=hmq[:], axis=AX.X, op=ALU.add))
                        V([R_sm], [R_sm], lambda e: e.tensor_scalar(out=sm[:, 28:32], in0=sm[:, 28:32], scalar1=1.0 / 128, scalar2=EPS, op0=ALU.mult, op1=ALU.add))
                        A([R_sm], [R_sm], lambda e: e.activation(out=sm[:, 28:32], in_=sm[:, 28:32], func=AF.Ln))
                        A([R_sm], [R_sm], lambda e: e.activation(out=sm[:, 28:32], in_=sm[:, 28:32], func=AF.Exp, scale=-0.5))
                        V([R_hmf, R_sm], [R_hmq], lambda e: e.tensor_tensor(out=hmq[:], in0=hmf[:], in1=bc_last(sm[:, 28:32], 128), op=ALU.mult))
                        V([R_hmq, R_sp], [R_hmq], lambda e: e.tensor_tensor(out=hmq[:].rearrange("p h d -> p (h d)"), in0=hmq[:].rearrange("p h d -> p (h d)"), in1=mon[:], op=ALU.mult))
                        V([R_hmq, R_in], [R_hst], lambda e: e.tensor_tensor(out=hm_st[:], in0=hmq[:].rearrange("p h d -> p (h d)"), in1=og[:, c, :], op=ALU.mult))
                        cx.dma("gpsimd", hm_d[sq, tq, :], hm_st[:], [R_hst], [R_hm[sq][c]])
            cx.barrier()

            w_gu_sb = sbuf(st_w, "w_gu_sb", [128, 8, 2 * FH], BF16)
            R_wg = [Region() for _ in range(16)]
            with ExitStack() as st:
                R_w = R_wo
                w_out_sb = sbuf(st, "w_out_sb", [128, 8, D], BF16)
                for kc in range(8):
                    cx.dma("gpsimd", w_out_sb[:, kc, :], w_out_d[l, kc * 128:(kc + 1) * 128, :], [], [R_wo[kc]])
                def load_wgu(ix):
                    kc, half = ix // 2, ix % 2
                    cx.dma("gpsimd", w_gu_sb[:, kc, half * FH:(half + 1) * FH], w_gu_d[l, kc * 128:(kc + 1) * 128, half * FH:(half + 1) * FH], [], [R_wg[ix]])
                gpost = sbuf(st, "gpost", [128, D], F32)
                R_sp = Region()
                cx.dma("sync", gpost[:], npost_d[l:l + 1, :].partition_broadcast(128), [], [R_sp])
                GM = sbuf(st, "GM", [128, D], F32)
                R_gm = Region()
                mx = [sbuf(st, "mx%d" % i, [128, D], BF16) for i in range(2)]
                R_mx = [Region() for _ in range(2)]
                mTs = [sbuf(st, "mT%d" % i, [128, 8, 128], BF16) for i in range(2)]
                R_mTs = [Region() for _ in range(2)]
                xt = [sbuf(st, "xt4_%d" % i, [128, D], F32) for i in range(2)]
                R_xt = [Region() for _ in range(2)]
                tmpf = sbuf(st, "tmp4", [128, D], F32)
                R_tmpf = Region()
                xo = [sbuf(st, "xo4_%d" % i, [128, D], F32) for i in range(2)]
                R_xo = [Region() for _ in range(2)]
                junk = sbuf(st, "junk4", [128, 512], BF16)
                R_junk = Region()
                sml = sbuf(st, "sml4", [128, 8], F32)
                R_sml = Region()
                py = [psum(st, "py%d" % i, [128, 512], F32) for i in range(4)]
                R_py = [Region() for _ in range(4)]
                ptb = [psum(st, "pt4_%d" % i, [128, 1024], BF16) for i in range(2)]
                R_pt = [Region() for _ in range(2)]
                def f4a_front(sq, i, b):
                    tq = slice(i * 128, (i + 1) * 128)
                    tg = slice(sq * S + i * 128, sq * S + (i + 1) * 128)
                    cx.dma("sync", mx[b][:, 0:512], att_d[sq, tq, :], [R_att[sq][i]], [R_mx[b]])
                    cx.dma("sync", mx[b][:, 512:1024], hm_d[sq, tq, :], [R_hm[sq][i]], [R_mx[b]])
                    rd = [] if R_xin is None else [R_xin[sq][i]]
                    cx.dma("sync", xt[b][:], xin_d[tg, :], rd, [R_xt[b]])
                    pt, Rp = ptb[b], R_pt[b]
                    for kc in range(8):
                        T([R_mx[b]] + RC, [Rp], lambda e: e.transpose(out=pt[:, kc * 128:(kc + 1) * 128], in_=mx[b][:, kc * 128:(kc + 1) * 128], identity=identb[:]))
                    A([Rp], [R_mTs[b]], lambda e: e.activation(out=mTs[b][:], in_=pt[:].rearrange("p (k t) -> p k t", t=128), func=AF.Copy))
                    for n in range(2):
                        pyb, Rpy = py[b * 2 + n], R_py[b * 2 + n]
                        for kc in range(8):
                            T([R_mTs[b], *R_w], [Rpy], lambda e: e.matmul(pyb[:], lhsT=mTs[b][:, kc, :], rhs=w_out_sb[:, kc, n * 512:(n + 1) * 512], start=(kc == 0), stop=(kc == 7)))

                def f4a_back(sq, i, b):
                    tg = slice(sq * S + i * 128, sq * S + (i + 1) * 128)
                    if i == 0:
                        cx.dma("sync", GM[:], modd[l, sq:sq + 1, 2 * D:3 * D].partition_broadcast(128), [R_modd], [R_gm])
                        V([R_gm, R_sp], [R_gm], lambda e: e.tensor_tensor(out=GM[:], in0=GM[:], in1=gpost[:], op=ALU.mult))
                    V([], [R_sml], lambda e: e.memset(sml[:, 0:2], 0.0))
                    for n in range(2):
                        pyb, Rpy = py[b * 2 + n], R_py[b * 2 + n]
                        A([Rpy, R_sml], [R_junk, R_sml], lambda e: e.activation(out=junk[:], in_=pyb[:], func=AF.Square, accum_out=sml[:, n:n + 1]))
                    V([R_sml], [R_sml], lambda e: e.tensor_tensor(out=sml[:, 2:3], in0=sml[:, 0:1], in1=sml[:, 1:2], op=ALU.add))
                    V([R_sml], [R_sml], lambda e: e.tensor_scalar(out=sml[:, 2:3], in0=sml[:, 2:3], scalar1=1.0 / D, scalar2=EPS, op0=ALU.mult, op1=ALU.add))
                    A([R_sml], [R_sml], lambda e: e.activation(out=sml[:, 2:3], in_=sml[:, 2:3], func=AF.Ln))
                    A([R_sml], [R_sml], lambda e: e.activation(out=sml[:, 2:3], in_=sml[:, 2:3], func=AF.Exp, scale=-0.5))
                    for n in range(2):
                        pyb, Rpy = py[b * 2 + n], R_py[b * 2 + n]
                        cs = slice(n * 512, (n + 1) * 512)
                        V([Rpy, R_sml, R_gm], [R_tmpf], lambda e: e.scalar_tensor_tensor(out=tmpf[:, cs], in0=pyb[:], scalar=sml[:, 2:3], in1=GM[:, cs], op0=ALU.mult, op1=ALU.mult))
                    V([R_tmpf, R_xt[b]], [R_xo[b]], lambda e: e.tensor_tensor(out=xo[b][:], in0=tmpf[:], in1=xt[b][:], op=ALU.add))
                    cx.dma("gpsimd", x1_d[tg, :], xo[b][:], [R_xo[b]], [R_x1[sq][i]])

                items4 = [(sq_, i_) for sq_ in range(NSEQ) for i_ in range(NT)]
                f4a_front(items4[0][0], items4[0][1], 0)
                for k_, (sq_, i_) in enumerate(items4):
                    if k_ + 1 < len(items4):
                        f4a_front(items4[k_ + 1][0], items4[k_ + 1][1], (k_ + 1) % 2)
                    if k_ % 2 == 0 and k_ // 2 < 16:
                        load_wgu(k_ // 2)
                    f4a_back(sq_, i_, k_ % 2)
            cx.barrier()

            with ExitStack() as st:
                R_w = R_wg + R_wd
                MODB = sbuf(st, "MODB", [128, 3, D], F32)
                R_modb = Region()
                x_st = [sbuf(st, "x5_%d" % i, [128, 2, D], F32) for i in range(2)]
                R_xst = [Region() for _ in range(2)]
                tmpf = sbuf(st, "tmp5", [128, D], F32)
                R_tmpf = Region()
                junk = tmpf
                R_junk = R_tmpf
                tmpF = sbuf(st, "tmpF5", [128, D], F32)
                R_tmpF = Region()
                junkF = tmpF
                R_junkF = R_tmpF
                smlF = sbuf(st, "smlF5", [128, 4], F32)
                R_smlF = Region()
                hb = sbuf(st, "hb5", [128, 2, D], BF16)
                R_hb = [Region() for _ in range(2)]
                hTs = [sbuf(st, "hT5_%d" % i, [128, 8, 256], BF16) for i in range(2)]
                R_hTs = [Region() for _ in range(2)]
                gua = sbuf(st, "gua", [128, NFC, 256], BF16)
                R_gua = Region()
                sg = [sbuf(st, "sg0", [128, 256], F32)] * 2
                R_sg = [Region()] * 2
                xo = [sbuf(st, "xo5_0", [128, D], F32)] * 2
                R_xo = [Region()] * 2
                gq = xo[0]
                R_gq = R_xo[0]
                sml = sbuf(st, "sml5", [128, 8], F32)
                R_sml = Region()
                pg = [psum(st, "pg%d" % i, [128, 512], F32) for i in range(2)]
                R_pg = [Region() for _ in range(2)]
                pyb_ = [psum(st, "py5_%d" % i, [128, 512], F32) for i in range(4)]
                R_pyb = [Region() for _ in range(4)]
                ptb = [psum(st, "pt5_%d" % i, [128, 1024], BF16) for i in range(2)]
                R_pt = [Region() for _ in range(2)]
                pgi = [0]
                pti = [0]
                xoi = [0]

                def load_modb(sq):
                    cx.dma("sync", MODB[:, 0, :], modd[l, sq:sq + 1, 3 * D:4 * D].partition_broadcast(128), [R_modd], [R_modb])
                    cx.dma("sync", MODB[:, 1, :], modd[l, sq:sq + 1, 4 * D:5 * D].partition_broadcast(128), [R_modd], [R_modb])
                    cx.dma("sync", MODB[:, 2, :], modd[l, sq:sq + 1, 5 * D:6 * D].partition_broadcast(128), [R_modd], [R_modb])
                    cx.dma("sync", gq[:], fpre_d[l:l + 1, :].partition_broadcast(128), [], [R_gq])
                    V([R_modb, R_gq], [R_modb], lambda e: e.scalar_tensor_tensor(out=MODB[:, 1, :], in0=MODB[:, 1, :], scalar=1.0, in1=gq[:], op0=ALU.add, op1=ALU.mult))
                    cx.dma("sync", gq[:], fpost_d[l:l + 1, :].partition_broadcast(128), [], [R_gq])
                    V([R_modb, R_gq], [R_modb], lambda e: e.tensor_tensor(out=MODB[:, 2, :], in0=MODB[:, 2, :], in1=gq[:], op=ALU.mult))

                def frontA(sq, ti, kb):
                    T0 = sq * S + ti * 256
                    xs = x_st[kb]
                    cx.dma("sync", xs[:], x1_d[T0:T0 + 256, :].rearrange("(j p) d -> p j d", p=128), [R_x1[sq][ti * 2], R_x1[sq][ti * 2 + 1]], [R_xst[kb]])
                    V([], [R_smlF], lambda e: e.memset(smlF[:, 0:2], 0.0))
                    for j in range(2):
                        A([R_xst[kb], R_smlF], [R_junkF, R_smlF], lambda e: e.activation(out=junkF[:], in_=xs[:, j, :], func=AF.Square, accum_out=smlF[:, j:j + 1]))
                    V([R_smlF], [R_smlF], lambda e: e.tensor_scalar(out=smlF[:, 0:2], in0=smlF[:, 0:2], scalar1=1.0 / D, scalar2=EPS, op0=ALU.mult, op1=ALU.add))
                    A([R_smlF], [R_smlF], lambda e: e.activation(out=smlF[:, 0:2], in_=smlF[:, 0:2], func=AF.Ln))
                    A([R_smlF], [R_smlF], lambda e: e.activation(out=smlF[:, 0:2], in_=smlF[:, 0:2], func=AF.Exp, scale=-0.5))
                    for j in range(2):
                        V([R_xst[kb], R_smlF, R_modb], [R_tmpF], lambda e: e.scalar_tensor_tensor(out=tmpF[:], in0=xs[:, j, :], scalar=smlF[:, j:j + 1], in1=MODB[:, 1, :], op0=ALU.mult, op1=ALU.mult))
                        V([R_tmpF, R_modb], [R_hb[j]], lambda e: e.tensor_tensor(out=hb[:, j, :], in0=tmpF[:], in1=MODB[:, 0, :], op=ALU.add))

                def frontB(kb):
                    for j in range(2):
                        pt, Rp = ptb[pti[0] % 2], R_pt[pti[0] % 2]
                        pti[0] += 1
                        for kc in range(8):
                            T([R_hb[j]] + RC, [Rp], lambda e: e.transpose(out=pt[:, kc * 128:(kc + 1) * 128], in_=hb[:, j, kc * 128:(kc + 1) * 128], identity=identb[:]))
                        A([Rp], [R_hTs[kb]], lambda e: e.activation(out=hTs[kb][:, :, j * 128:(j + 1) * 128], in_=pt[:].rearrange("p (k t) -> p k t", t=128), func=AF.Copy))

                def gate_up(kb):
                    hT, R_hT = hTs[kb], R_hTs[kb]
                    for fc in range(NFC):
                        pgb, Rpg = pg[pgi[0] % 2], R_pg[pgi[0] % 2]
                        pgi[0] += 1
                        for kc in range(8):
                            T([R_hT, *R_w], [Rpg], lambda e: e.matmul(pgb[:, 0:256], lhsT=w_gu_sb[:, kc, fc * 128:(fc + 1) * 128], rhs=hT[:, kc, :], start=(kc == 0), stop=(kc == 7)))
                        for kc in range(8):
                            T([R_hT, *R_w], [Rpg], lambda e: e.matmul(pgb[:, 256:512], lhsT=w_gu_sb[:, kc, FH + fc * 128:FH + (fc + 1) * 128], rhs=hT[:, kc, :], start=(kc == 0), stop=(kc == 7)))
                        sgb, Rsg = sg[fc % 2], R_sg[fc % 2]
                        A([Rpg], [Rsg], lambda e: e.activation(out=sgb[:], in_=pgb[:, 0:256], func=AF.Silu))
                        V([Rsg, Rpg], [R_gua], lambda e: e.tensor_tensor(out=gua[:, fc, :], in0=sgb[:], in1=pgb[:, 256:512], op=ALU.mult))

                def down_tail(sq, ti, kb):
                    T0 = sq * S + ti * 256
                    xs = x_st[kb]
                    for j in range(2):
                        V([], [R_sml], lambda e: e.memset(sml[:, 4:6], 0.0))
                        for n in range(2):
                            pyq, Rpyq = pyb_[j * 2 + n], R_pyb[j * 2 + n]
                            for fc in range(NFC):
                                T([R_gua, *R_w], [Rpyq], lambda e: e.matmul(pyq[:], lhsT=gua[:, fc, j * 128:(j + 1) * 128], rhs=w_dn_sb[:, fc, n * 512:(n + 1) * 512], start=(fc == 0), stop=(fc == NFC - 1)))
                            A([Rpyq, R_sml], [R_junk, R_sml], lambda e: e.activation(out=junk[:, 0:512], in_=pyq[:], func=AF.Square, accum_out=sml[:, 4 + n:5 + n]))
                        V([R_sml], [R_sml], lambda e: e.tensor_tensor(out=sml[:, 6:7], in0=sml[:, 4:5], in1=sml[:, 5:6], op=ALU.add))
                        V([R_sml], [R_sml], lambda e: e.tensor_scalar(out=sml[:, 6:7], in0=sml[:, 6:7], scalar1=1.0 / D, scalar2=EPS, op0=ALU.mult, op1=ALU.add))
                        A([R_sml], [R_sml], lambda e: e.activation(out=sml[:, 6:7], in_=sml[:, 6:7], func=AF.Ln))
                        A([R_sml], [R_sml], lambda e: e.activation(out=sml[:, 6:7], in_=sml[:, 6:7], func=AF.Exp, scale=-0.5))
                        for n in range(2):
                            pyq, Rpyq = pyb_[j * 2 + n], R_pyb[j * 2 + n]
                            cs = slice(n * 512, (n + 1) * 512)
                            V([Rpyq, R_sml, R_modb], [R_tmpf], lambda e: e.scalar_tensor_tensor(out=tmpf[:, cs], in0=pyq[:], scalar=sml[:, 6:7], in1=MODB[:, 2, cs], op0=ALU.mult, op1=ALU.mult))
                        ob = xoi[0] % 2
                        xoi[0] += 1
                        V([R_tmpf, R_xst[kb]], [R_xo[ob]], lambda e: e.tensor_tensor(out=xo[ob][:], in0=tmpf[:], in1=xs[:, j, :], op=ALU.add))
                        tg = slice(T0 + j * 128, T0 + (j + 1) * 128)
                        cx.dma("gpsimd", xout_d[tg, :], xo[ob][:], [R_xo[ob]], [R_xres[sq][ti * 2 + j]])

                items5 = [(sq_, ti_) for sq_ in range(NSEQ) for ti_ in range(8)]
                load_modb(0)
                frontA(0, 0, 0)
                frontB(0)
                for k_, (sq_, ti_) in enumerate(items5):
                    kb = k_ % 2
                    nxt = items5[k_ + 1] if k_ + 1 < len(items5) else None
                    if nxt is not None and nxt[0] == sq_:
                        frontA(nxt[0], nxt[1], 1 - kb)
                    gate_up(kb)
                    if nxt is not None and nxt[0] == sq_:
                        frontB(1 - kb)
                    down_tail(sq_, ti_, kb)
                    if nxt is not None and nxt[0] != sq_:
                        load_modb(nxt[0])
                        frontA(nxt[0], nxt[1], 1 - kb)
                        frontB(1 - kb)
            cx.barrier()
            st_w.close()
        cx.finish()
        stuck = cx.check_deadlock()
        if stuck:
            raise RuntimeError("static deadlock check failed: %r" % (stuck,))
    return nc


def _consts():
    k = np.arange(128)
    ident = np.eye(128, dtype=np.float32)
    utri = (k[:, None] <= k[None, :]).astype(np.float32)
    negtri = np.where(k[None, :] <= k[:, None], 0.0, NEG).astype(np.float32)
    negm = np.where(k[:, None] <= k[None, :], 0.0, -30000.0).astype(np.float32)
    invf = (10000.0 ** (-np.arange(0, 64, 2, dtype=np.float32) / 64)).astype(np.float32)[None, :]
    pw2 = np.ascontiguousarray(np.broadcast_to((2.0 ** -(np.arange(32, dtype=np.float32) + 1.0)).astype(np.float32)[None, :], (128, 32)))
    return dict(c_ident=ident, c_utri=utri, c_negtri=negtri, c_negm=negm, c_invf=invf, c_pw2=pw2)


def make_in_maps(inputs, n_cores=8):
    f32 = lambda a: np.ascontiguousarray(np.asarray(a, dtype=np.float32))
    shared = {}
    for k in ("w_mod", "b_mod", "mix_norm_pre", "mix_norm_post", "w_in", "q_latent_norm", "w_q_up", "w_qidx_up",
              "b_igate", "b_fgate", "attn_out_norm", "mlstm_out_norm", "w_out", "ffn_norm_pre", "ffn_norm_post",
              "w_gate_up", "w_down"):
        shared[k] = f32(inputs[k])
    cw = f32(inputs["conv_w"])
    shared["convw"] = np.ascontiguousarray(cw.reshape(2, 4, 8, 128).transpose(0, 3, 2, 1))
    shared["convb"] = np.ascontiguousarray(f32(inputs["conv_b"]).reshape(2, 8, 128).transpose(0, 2, 1))
    shared.update(_consts())
    x = f32(inputs["x"])
    c = f32(inputs["c"])
    pos = np.asarray(inputs["positions"]).astype(np.int32)
    maps = []
    for i in range(n_cores):
        m = dict(shared)
        m["x"] = np.ascontiguousarray(x[2 * i:2 * i + 2].reshape(NSEQ * S, D))
        m["cT"] = np.ascontiguousarray(c[2 * i:2 * i + 2].reshape(NSEQ, 8, 128).transpose(2, 1, 0))
        m["pos"] = np.ascontiguousarray(pos[2 * i:2 * i + 2].reshape(NSEQ, NT, 128).transpose(2, 0, 1))
        maps.append(m)
    return maps


_NC_CACHE = {}


def kernel(**inputs):
    if "nc" not in _NC_CACHE:
        _NC_CACHE["nc"] = build_nc()
    nc = _NC_CACHE["nc"]
    maps = make_in_maps(inputs)
    res = run_bass_kernel_spmd(nc, maps, core_ids=list(range(8)))
    outs = [np.asarray(r["out"]).reshape(NSEQ, S, D) for r in res.results]
    return np.concatenate(outs, axis=0).astype(np.float32)
```

```python
from contextlib import ExitStack
import math
import numpy as np
import concourse.bass as bass
import concourse.mybir as mybir
from concourse.bass_utils import run_bass_kernel_spmd

F32 = mybir.dt.float32
BF16 = mybir.dt.bfloat16
I32 = mybir.dt.int32
AF = mybir.ActivationFunctionType
ALU = mybir.AluOpType
AX = mybir.AxisListType

D = 1024
S = 2048
NT = 16
NSEQ = 2
FH = 2816
NFC = 22
INC = 3408
EPS = 1e-6
NEG = -1.0e30
TOPK = 256


class Region:
    __slots__ = ("w", "r")

    def __init__(self):
        self.w = None
        self.r = {}


class EngState:
    def __init__(self, name, eng, sem):
        self.name = name
        self.eng = eng
        self.sem = sem
        self.count = 0
        self.waited = {}


class Ctx:
    def __init__(self, nc, stack):
        self.nc = nc
        self.engs = {}
        for name in ("tensor", "vector", "scalar", "gpsimd", "sync"):
            sem = stack.enter_context(nc.semaphore("s_" + name))
            self.engs[name] = EngState(name, getattr(nc, name), sem)
        self.dma_pools = {}
        for q, n in (("sync", 24), ("gpsimd", 16), ("scalar", 4)):
            pool = []
            for i in range(n):
                sem = stack.enter_context(nc.semaphore("s_dma_%s%d" % (q, i)))
                pool.append([sem, 0])
            self.dma_pools[q] = [pool, 0]
        self.n_inst = 0
        self.trace = {n: [] for n in self.engs}

    def _wait(self, es, ev):
        sem, val = ev
        k = id(sem)
        if es.waited.get(k, 0) >= val:
            return
        es.eng.wait_ge(sem, val)
        es.waited[k] = val
        self.trace[es.name].append(("w", k, val))

    def check_deadlock(self):
        vals = {}
        pos = {n: 0 for n in self.trace}
        progress = True
        while progress:
            progress = False
            for n, tr in self.trace.items():
                while pos[n] < len(tr):
                    kind, k, v = tr[pos[n]]
                    if kind == "w":
                        if vals.get(k, 0) < v:
                            break
                    else:
                        vals[k] = vals.get(k, 0) + v
                    pos[n] += 1
                    progress = True
        stuck = {n: (pos[n], len(tr)) for n, tr in self.trace.items() if pos[n] < len(tr)}
        return stuck

    def _deps(self, es, reads, writes, skip_same=False):
        best = {}

        def add(ev):
            if ev is None:
                return
            k = id(ev[0])
            if k not in best or best[k][1] < ev[1]:
                best[k] = ev
        for r in reads:
            add(r.w)
        for w in writes:
            add(w.w)
            for ev in w.r.values():
                add(ev)
        for ev in best.values():
            if skip_same and ev[0] is es.sem:
                continue
            self._wait(es, ev)

    def _commit(self, ev, reads, writes):
        k = id(ev[0])
        for r in reads:
            r.r[k] = ev
        for w in writes:
            w.w = ev
            w.r = {}

    def op(self, engname, reads, writes, fn):
        es = self.engs[engname]
        self._deps(es, reads, writes, skip_same=(engname == "tensor"))
        inst = fn(es.eng)
        es.count += 1
        inst.then_inc(es.sem, 1)
        self.trace[engname].append(("i", id(es.sem), 1))
        self._commit((es.sem, es.count), reads, writes)
        self.n_inst += 1

    def dma(self, qname, out, in_, reads, writes, **kw):
        es = self.engs[qname]
        pr = self.dma_pools[qname]
        slot = pr[0][pr[1]]
        pr[1] = (pr[1] + 1) % len(pr[0])
        sem, tot = slot
        if tot > 0:
            self._wait(es, (sem, tot))
        self._deps(es, reads, writes)
        es.eng.dma_start(out=out, in_=in_, **kw).then_inc(sem, 16)
        self.trace[qname].append(("i", id(sem), 16))
        slot[1] = tot + 16
        self._commit((sem, tot + 16), reads, writes)
        self.n_inst += 1

    def barrier(self):
        snap = [(e.sem, e.count) for e in self.engs.values() if e.count > 0]
        for pr in self.dma_pools.values():
            snap += [(s_, t_) for s_, t_ in pr[0] if t_ > 0]
        for name in ("sync", "gpsimd", "tensor", "vector", "scalar"):
            es = self.engs[name]
            for ev in snap:
                if ev[0] is es.sem:
                    continue
                self._wait(es, ev)

    def finish(self):
        self.barrier()


def bc_mid(ap, n):
    p, f = ap.shape
    return ap.unsqueeze(1).to_broadcast([p, n, f])


def bc_last(ap, n):
    p, a = ap.shape
    return ap.unsqueeze(2).to_broadcast([p, a, n])


def build_nc(n_layers=2, dbg=False):
    nc = bass.Bass("TRN2", target_bir_lowering=False)

    def din(name, shape, dt=F32):
        return nc.dram_tensor(name, shape, dt, kind="ExternalInput").ap()

    def dscr(name, shape, dt):
        return nc.dram_tensor(name, shape, dt, kind=("ExternalOutput" if dbg else "Internal")).ap()

    x_d = din("x", [NSEQ * S, D])
    cT_d = din("cT", [128, 8, NSEQ])
    pos_d = din("pos", [128, NSEQ, NT], I32)
    w_mod_d = din("w_mod", [2, D, 6 * D])
    b_mod_d = din("b_mod", [2, 6 * D])
    npre_d = din("mix_norm_pre", [2, D])
    npost_d = din("mix_norm_post", [2, D])
    w_in_d = din("w_in", [2, D, INC])
    qln_d = din("q_latent_norm", [2, 256])
    wq_d = din("w_q_up", [2, 256, 512])
    wqi_d = din("w_qidx_up", [2, 256, 512])
    convw_d = din("convw", [2, 128, 8, 4])
    convb_d = din("convb", [2, 128, 8])
    big_d = din("b_igate", [2, 4])
    bfg_d = din("b_fgate", [2, 4])
    aon_d = din("attn_out_norm", [2, 512])
    mon_d = din("mlstm_out_norm", [2, 512])
    w_out_d = din("w_out", [2, D, D])
    fpre_d = din("ffn_norm_pre", [2, D])
    fpost_d = din("ffn_norm_post", [2, D])
    w_gu_d = din("w_gate_up", [2, D, 2 * FH])
    w_dn_d = din("w_down", [2, FH, D])
    ident_d = din("c_ident", [128, 128])
    utri_d = din("c_utri", [128, 128])
    negtri_d = din("c_negtri", [128, 128])
    negm_d = din("c_negm", [128, 128])
    invf_d = din("c_invf", [1, 32])
    pw2_d = din("c_pw2", [128, 32])

    out_d = nc.dram_tensor("out", [NSEQ * S, D], F32, kind="ExternalOutput").ap()

    xres_d = dscr("xres", [NSEQ * S, D], F32)
    x1_d = dscr("x1s", [NSEQ * S, D], F32)
    modd = dscr("modd", [2, NSEQ, 6 * D], F32)
    qT_d = dscr("qT", [NSEQ, 128, 4, S], BF16)
    qiT_d = dscr("qiT", [NSEQ, 128, 4, S], BF16)
    kT_d = dscr("kT", [NSEQ, 128, 4, S], BF16)
    kiT_d = dscr("kiT", [NSEQ, 64, S], BF16)
    V_d = dscr("Vs", [NSEQ, S, 512], BF16)
    wi_d = dscr("wi", [NSEQ, S, 8], F32)
    qmT_d = dscr("qmT", [NSEQ, 128, 8, S], BF16)
    Vm_d = dscr("Vm", [NSEQ, S, 512], BF16)
    og_d = dscr("og", [NSEQ, S, 512], BF16)
    ig_d = dscr("ig", [NSEQ, S, 4], F32)
    lf_d = dscr("lf", [NSEQ, S, 4], F32)
    att_d = dscr("att", [NSEQ, S, 512], BF16)
    hm_d = dscr("hm", [NSEQ, S, 512], BF16)

    R_xres = [[Region() for _ in range(NT)] for _ in range(NSEQ)]
    R_x1 = [[Region() for _ in range(NT)] for _ in range(NSEQ)]
    R_modd = Region()
    R_p1 = [[] for _ in range(NSEQ)]
    R_att = [[Region() for _ in range(NT)] for _ in range(NSEQ)]
    R_hm = [[Region() for _ in range(NT)] for _ in range(NSEQ)]

    with ExitStack() as st0:
        cx = Ctx(nc, st0)

        def V(r, w, fn):
            cx.op("vector", r, w, fn)

        def A(r, w, fn):
            cx.op("scalar", r, w, fn)

        def G(r, w, fn):
            cx.op("gpsimd", r, w, fn)

        def T(r, w, fn):
            cx.op("tensor", r, w, fn)

        uniq = [0]

        def sbuf(stk, name, shape, dt):
            uniq[0] += 1
            return stk.enter_context(nc.sbuf_tensor("%s_%d" % (name, uniq[0]), shape, dt))

        def psum(stk, name, shape, dt):
            uniq[0] += 1
            return stk.enter_context(nc.psum_tensor("%s_%d" % (name, uniq[0]), shape, dt))

        identf = sbuf(st0, "identf", [128, 128], F32)
        identb = sbuf(st0, "identb", [128, 128], BF16)
        utri = sbuf(st0, "utri", [128, 128], F32)
        negtri = sbuf(st0, "negtri", [128, 128], F32)
        negm = sbuf(st0, "negm", [128, 128], F32)
        onesf = sbuf(st0, "onesf", [128, 128], F32)
        invf = sbuf(st0, "invf", [128, 32], F32)
        posi = sbuf(st0, "posi", [128, NSEQ, NT], I32)
        posf = sbuf(st0, "posf", [128, NSEQ * NT], F32)
        COS = sbuf(st0, "COS", [128, NSEQ * NT, 32], F32)
        SIN = sbuf(st0, "SIN", [128, NSEQ * NT, 32], F32)
        cact = sbuf(st0, "cact", [128, 8, NSEQ], F32)
        thr0 = sbuf(st0, "thr0", [128, 1], F32)
        pw2 = sbuf(st0, "pw2", [128, 32], F32)
        R_c = Region()
        cx.dma("sync", identf[:], ident_d, [], [R_c])
        cx.dma("sync", utri[:], utri_d, [], [R_c])
        cx.dma("sync", negtri[:], negtri_d, [], [R_c])
        cx.dma("sync", negm[:], negm_d, [], [R_c])
        cx.dma("sync", invf[:], invf_d.partition_broadcast(128), [], [R_c])
        cx.dma("sync", posi[:], pos_d, [], [R_c])
        cx.dma("sync", pw2[:], pw2_d, [], [R_c])
        cx.dma("sync", cact[:], cT_d, [], [R_c])
        R_c2 = Region()
        R_c4, R_c5, R_c6 = Region(), Region(), Region()
        V([R_c], [R_c4], lambda e: e.tensor_copy(out=identb[:], in_=identf[:]))
        V([], [R_c5], lambda e: e.memset(onesf[:], 1.0))
        V([], [R_c6], lambda e: e.memset(thr0[:], -1.0e29))
        V([R_c], [R_c2], lambda e: e.tensor_copy(out=posf[:], in_=posi[:].rearrange("p a b -> p (a b)")))
        R_cs = Region()
        R_c3 = Region()
        A([R_c], [R_c3], lambda e: e.activation(out=cact[:], in_=cact[:], func=AF.Silu))
        RC = [R_c, R_c2, R_c3, R_c4, R_c5, R_c6, R_cs]

        with ExitStack() as st:
            R_ang = Region()
            ANG = sbuf(st, "ANG", [128, NSEQ * NT, 32], F32)
            ang2 = sbuf(st, "ang2", [128, NSEQ * NT, 32], F32)
            angk = sbuf(st, "angk", [128, NSEQ * NT, 32], F32)
            angi = sbuf(st, "angi", [128, NSEQ * NT, 32], I32)
            V([R_c, R_c2], [R_ang], lambda e: e.tensor_tensor(out=ANG[:], in0=bc_last(posf[:], 32), in1=bc_mid(invf[:], NSEQ * NT), op=ALU.mult))
            C1 = 6.28125
            C2 = 2.0 * math.pi - 6.28125
            for dst, off in ((SIN, 0.0), (COS, 0.5 * math.pi)):
                V([R_ang], [R_ang], lambda e: e.tensor_scalar(out=ang2[:], in0=ANG[:], scalar1=off, scalar2=None, op0=ALU.add))
                V([R_ang], [R_ang], lambda e: e.tensor_scalar(out=angk[:], in0=ang2[:], scalar1=1.0 / (2.0 * math.pi), scalar2=None, op0=ALU.mult))
                V([R_ang], [R_ang], lambda e: e.tensor_copy(out=angi[:], in_=angk[:]))
                V([R_ang], [R_ang], lambda e: e.tensor_copy(out=angk[:], in_=angi[:]))
                V([R_ang], [R_ang], lambda e: e.scalar_tensor_tensor(out=ang2[:], in0=angk[:], scalar=-C1, in1=ang2[:], op0=ALU.mult, op1=ALU.add))
                V([R_ang], [R_ang], lambda e: e.scalar_tensor_tensor(out=ang2[:], in0=angk[:], scalar=-C2, in1=ang2[:], op0=ALU.mult, op1=ALU.add))
                V([R_ang], [R_ang], lambda e: e.tensor_scalar(out=angk[:], in0=ang2[:], scalar1=math.pi, scalar2=-2.0 * math.pi, op0=ALU.is_gt, op1=ALU.mult))
                V([R_ang], [R_ang], lambda e: e.tensor_tensor(out=ang2[:], in0=ang2[:], in1=angk[:], op=ALU.add))
                V([R_ang], [R_ang], lambda e: e.tensor_scalar(out=angk[:], in0=ang2[:], scalar1=-math.pi, scalar2=2.0 * math.pi, op0=ALU.is_lt, op1=ALU.mult))
                V([R_ang], [R_ang], lambda e: e.tensor_tensor(out=ang2[:], in0=ang2[:], in1=angk[:], op=ALU.add))
                V([R_ang], [R_ang], lambda e: e.tensor_scalar(out=ang2[:], in0=ang2[:], scalar1=-3.1415925, scalar2=3.1415925, op0=ALU.max, op1=ALU.min))
                A([R_ang], [R_cs, R_ang], lambda e: e.activation(out=dst[:], in_=ang2[:], func=AF.Sin))
            wst = [sbuf(st, "wmst%d" % i, [128, 3072], BF16) for i in range(3)]
            cactb = sbuf(st, "cactb", [128, 8, NSEQ], BF16)
            R_cb = Region()
            V(RC, [R_cb], lambda e: e.tensor_copy(out=cactb[:], in_=cact[:]))
            R_wst = [Region() for _ in range(3)]
            bmod = sbuf(st, "bmod", [NSEQ, 6 * D], F32)
            modsb = sbuf(st, "modsb", [NSEQ, 6 * D], F32)
            R_bm, R_ms = Region(), Region()
            pm = [psum(st, "pm%d" % i, [128, 512], F32) for i in range(6)]
            R_pm = [Region() for _ in range(6)]
            cnt = 0
            for l in range(n_layers):
                cx.dma("sync", bmod[:], b_mod_d[l:l + 1, :].partition_broadcast(NSEQ), [], [R_bm])
                for half in range(2):
                    for kc in range(8):
                        b = cnt % 3
                        cnt += 1
                        cx.dma("gpsimd", wst[b][:], w_mod_d[l, kc * 128:(kc + 1) * 128, half * 3072:(half + 1) * 3072], [], [R_wst[b]])
                        for n in range(6):
                            T([R_wst[b], R_cb], [R_pm[n]], lambda e: e.matmul(pm[n][0:NSEQ, :], lhsT=cactb[:, kc, :], rhs=wst[b][:, n * 512:(n + 1) * 512], start=(kc == 0), stop=(kc == 7)))
                    for n in range(6):
                        c0 = half * 3072 + n * 512
                        V([R_pm[n], R_bm], [R_ms], lambda e: e.tensor_tensor(out=modsb[:, c0:c0 + 512], in0=pm[n][0:NSEQ, :], in1=bmod[:, c0:c0 + 512], op=ALU.add))
                cx.dma("sync", modd[l], modsb[:], [R_ms], [R_modd])
        cx.barrier()

        for l in range(n_layers):
            xin_d = x_d if l == 0 else xres_d
            xout_d = xres_d if l < n_layers - 1 else out_d
            R_xin = None if l == 0 else R_xres

            with ExitStack() as st:
                w_in_sb = sbuf(st, "w_in_sb", [128, 8, INC], BF16)
                wq_sb = sbuf(st, "wq_sb", [128, 2, 1024], BF16)
                R_w = [Region() for _ in range(40)]
                for kc in range(8):
                    cx.dma("gpsimd", w_in_sb[:, kc, :], w_in_d[l, kc * 128:(kc + 1) * 128, :], [], [R_w[kc]])
                for kc in range(2):
                    cx.dma("gpsimd", wq_sb[:, kc, 0:512], wq_d[l, kc * 128:(kc + 1) * 128, :], [], [R_w[8 + kc]])
                    cx.dma("gpsimd", wq_sb[:, kc, 512:1024], wqi_d[l, kc * 128:(kc + 1) * 128, :], [], [R_w[10 + kc]])
                gpre = sbuf(st, "gpre", [128, D], F32)
                qln = sbuf(st, "qln", [128, 256], F32)
                cw = sbuf(st, "cw", [128, 8, 4], F32)
                cb = sbuf(st, "cb", [128, 8], F32)
                bigt = sbuf(st, "bigt", [128, 4], F32)
                bfgt = sbuf(st, "bfgt", [128, 4], F32)
                R_sp = Region()
                cx.dma("sync", gpre[:], npre_d[l:l + 1, :].partition_broadcast(128), [], [R_sp])
                cx.dma("sync", qln[:], qln_d[l:l + 1, :].partition_broadcast(128), [], [R_sp])
                cx.dma("sync", cw[:], convw_d[l], [], [R_sp])
                cx.dma("sync", cb[:], convb_d[l], [], [R_sp])
                cx.dma("sync", bigt[:], big_d[l:l + 1, :].partition_broadcast(128), [], [R_sp])
                cx.dma("sync", bfgt[:], bfg_d[l:l + 1, :].partition_broadcast(128), [], [R_sp])

                MODA = sbuf(st, "MODA", [128, 2, D], F32)
                R_moda = Region()
                x_st = [sbuf(st, "x_st0", [128, 4, D], F32)] * 2
                R_xst = [Region()] * 2
                junk = sbuf(st, "junk", [128, D], BF16)
                R_junk = Region()
                ss = sbuf(st, "ss", [128, 8], F32)
                R_ss = Region()
                tmpf = sbuf(st, "tmpf", [128, D], F32)
                R_tmpf = Region()
                hb = sbuf(st, "hb", [128, 4, D], BF16)
                R_hb = [Region() for _ in range(4)]
                hTs = [sbuf(st, "hT%d" % i, [128, 8, 512], BF16) for i in range(2)]
                R_hTs = [Region() for _ in range(2)]
                smlF = sbuf(st, "smlF", [128, 8], F32)
                R_smlF = Region()
                junk2 = sbuf(st, "junk2", [128, 256], BF16)
                R_junk2 = Region()
                qTs = sbuf(st, "qTs", [128, 4, 512], BF16)
                qiTs = sbuf(st, "qiTs", [128, 4, 512], BF16)
                kTs = sbuf(st, "kTs", [128, 4, 512], BF16)
                kiTs = sbuf(st, "kiTs", [64, 512], BF16)
                Vs = sbuf(st, "Vs_sb", [128, 4, 512], BF16)
                Vms = sbuf(st, "Vms", [128, 4, 512], BF16)
                ogs = sbuf(st, "ogs", [128, 4, 512], BF16)
                wis = sbuf(st, "wis", [128, 4, 8], F32)
                igs = sbuf(st, "igs", [128, 4, 4], F32)
                lfs = sbuf(st, "lfs", [128, 4, 4], F32)
                qmTs = sbuf(st, "qmTs", [128, 8, 512], BF16)
                R_out = {k: Region() for k in ("qT", "qiT", "kT", "kiT", "V", "Vm", "og", "wi", "ig", "lf", "qmT")}
                pre = sbuf(st, "pre", [128, 8, 515], F32)
                R_pre = Region()
                acc = sbuf(st, "acc", [128, 512], F32)
                R_acc = Region()
                cqn = [sbuf(st, "cqn%d" % i, [128, 256], BF16) for i in range(2)]
                R_cqn = [Region() for _ in range(2)]
                rOs = {k_: [sbuf(st, "rO_%s%d" % (k_, i), [128, 512 if k_ != "ki" else 64], BF16) for i in range(2)] for k_ in ("k", "q", "qi", "ki")}
                R_rOs = {k_: [Region() for _ in range(2)] for k_ in ("k", "q", "qi", "ki")}
                cqT = sbuf(st, "cqT", [128, 2, 128], BF16)
                R_cqT = Region()
                rAs = [sbuf(st, "rA%d" % i, [128, 512], F32) for i in range(2)]
                rBs = [sbuf(st, "rB%d" % i, [128, 512], F32) for i in range(2)]
                R_rAs = [Region() for _ in range(2)]
                R_rBs = [Region() for _ in range(2)]
                ropei = [0]
                sml = sbuf(st, "sml", [128, 16], F32)
                R_sml = Region()
                pf = [psum(st, "pf%d" % i, [128, 512], F32) for i in range(6)]
                R_pf = [Region() for _ in range(6)]
                ptb = [psum(st, "ptb%d" % i, [128, 1024], BF16) for i in range(2)]
                R_pt = [Region() for _ in range(2)]
                pfi = [0]
                pti = [0]

                def next_pf():
                    i = pfi[0] % 6
                    pfi[0] += 1
                    return pf[i], R_pf[i]

                def next_pt():
                    i = pti[0] % 2
                    pti[0] += 1
                    return ptb[i], R_pt[i]

                def rope(src, R_src, ncols, tix, dst, R_dst_w):
                    nh = ncols // 64
                    rb_ = ropei[0] % 2
                    ropei[0] += 1
                    rA, rB, R_rA, R_rB = rAs[rb_], rBs[rb_], R_rAs[rb_], R_rBs[rb_]
                    cos = COS[:, tix, :]
                    sin = SIN[:, tix, :]
                    s3 = src.rearrange("p (g f) -> p g f", f=32)
                    a3 = rA[:, 0:ncols].rearrange("p (g f) -> p g f", f=32)
                    V([R_src] + RC, [R_rA], lambda e: e.tensor_tensor(out=a3, in0=s3, in1=bc_mid(cos, 2 * nh), op=ALU.mult))
                    s4 = src.rearrange("p (h t f) -> p h t f", t=2, f=32)
                    b4 = rB[:, 0:ncols].rearrange("p (h t f) -> p h t f", t=2, f=32)
                    a4 = rA[:, 0:ncols].rearrange("p (h t f) -> p h t f", t=2, f=32)
                    d4 = dst.rearrange("p (h t f) -> p h t f", t=2, f=32)
                    V([R_src] + RC, [R_rB], lambda e: e.tensor_tensor(out=b4[:, :, 0, :], in0=s4[:, :, 1, :], in1=bc_mid(sin, nh), op=ALU.mult))
                    V([R_src] + RC, [R_rB], lambda e: e.tensor_tensor(out=b4[:, :, 1, :], in0=s4[:, :, 0, :], in1=bc_mid(sin, nh), op=ALU.mult))
                    G([R_rA, R_rB], R_dst_w, lambda e: e.tensor_tensor(out=d4[:, :, 0, :], in0=a4[:, :, 0, :], in1=b4[:, :, 0, :], op=ALU.subtract))
                    G([R_rA, R_rB], R_dst_w, lambda e: e.tensor_tensor(out=d4[:, :, 1, :], in0=a4[:, :, 1, :], in1=b4[:, :, 1, :], op=ALU.add))

                def rstd_from_ss(col0, n, inv_n, tl=None, Rt=None):
                    tl = sml if tl is None else tl
                    Rt = R_sml if Rt is None else Rt
                    sl = tl[:, col0:col0 + n]
                    V([Rt], [Rt], lambda e: e.tensor_scalar(out=sl, in0=sl, scalar1=inv_n, scalar2=EPS, op0=ALU.mult, op1=ALU.add))
                    A([Rt], [Rt], lambda e: e.activation(out=sl, in_=sl, func=AF.Ln))
                    A([Rt], [Rt], lambda e: e.activation(out=sl, in_=sl, func=AF.Exp, scale=-0.5))

                xcnt = 0

                def p1_front(sq, sti, hT, R_hT):
                    if sti == 0:
                        cx.dma("sync", MODA[:, 0, :], modd[l, sq:sq + 1, 0:D].partition_broadcast(128), [R_modd], [R_moda])
                        cx.dma("sync", MODA[:, 1, :], modd[l, sq:sq + 1, D:2 * D].partition_broadcast(128), [R_modd], [R_moda])
                        V([R_moda, R_sp], [R_moda], lambda e: e.scalar_tensor_tensor(out=MODA[:, 1, :], in0=MODA[:, 1, :], scalar=1.0, in1=gpre[:], op0=ALU.add, op1=ALU.mult))
                    T0 = sq * S + sti * 512
                    xb = 0
                    xs = x_st[xb]
                    rd = [] if R_xin is None else [R_xin[sq][sti * 4 + j] for j in range(4)]
                    cx.dma("sync", xs[:], xin_d[T0:T0 + 512, :].rearrange("(j p) d -> p j d", p=128), rd, [R_xst[xb]])
                    V([], [R_smlF], lambda e: e.memset(smlF[:, 0:4], 0.0))
                    for j in range(4):
                        A([R_xst[xb], R_smlF], [R_junk, R_smlF], lambda e: e.activation(out=junk[:], in_=xs[:, j, :], func=AF.Square, accum_out=smlF[:, j:j + 1]))
                    rstd_from_ss(0, 4, 1.0 / D, smlF, R_smlF)
                    for j in range(4):
                        V([R_xst[xb], R_smlF, R_moda], [R_tmpf], lambda e: e.scalar_tensor_tensor(out=tmpf[:], in0=xs[:, j, :], scalar=smlF[:, j:j + 1], in1=MODA[:, 1, :], op0=ALU.mult, op1=ALU.mult))
                        G([R_tmpf, R_moda], [R_hb[j]], lambda e: e.tensor_tensor(out=hb[:, j, :], in0=tmpf[:], in1=MODA[:, 0, :], op=ALU.add))
                    for j in range(4):
                        pt, Rp = next_pt()
                        for kc in range(8):
                            T([R_hb[j]] + RC, [Rp], lambda e: e.transpose(out=pt[:, kc * 128:(kc + 1) * 128], in_=hb[:, j, kc * 128:(kc + 1) * 128], identity=identb[:]))
                        A([Rp], [R_hT], lambda e: e.activation(out=hT[:, :, j * 128:(j + 1) * 128], in_=pt[:].rearrange("p (k t) -> p k t", t=128), func=AF.Copy))


                def p1_back(sq, sti, hT, R_hT):
                    T0 = sq * S + sti * 512
                    if sti == 0:
                        V([], [R_pre], lambda e: e.memset(pre[:, :, 0:3], 0.0))
                    def proj(tsl, pb, Rpb, c0, c1, o0):
                        for kc in range(8):
                            T([R_hT, *R_w], [Rpb], lambda e: e.matmul(pb[:, o0:o0 + (c1 - c0)], lhsT=hT[:, kc, tsl], rhs=w_in_sb[:, kc, c0:c1], start=(kc == 0), stop=(kc == 7)))

                    def stage_P(j):
                        tix = sq * NT + sti * 4 + j
                        tsl = slice(j * 128, (j + 1) * 128)
                        jb = j % 2
                        pa, Rpa = next_pf()
                        proj(tsl, pa, Rpa, 0, 256, 0)
                        proj(tsl, pa, Rpa, 1280, 1352, 256)
                        proj(tsl, pa, Rpa, 3400, 3408, 328)
                        pk, Rpk = next_pf()
                        proj(tsl, pk, Rpk, 256, 768, 0)
                        V([], [R_sml], lambda e: e.memset(sml[:, 4:5], 0.0))
                        A([Rpa, R_sml], [R_junk2, R_sml], lambda e: e.activation(out=junk2[:], in_=pa[:, 0:256], func=AF.Square, accum_out=sml[:, 4:5]))
                        rstd_from_ss(4, 1, 1.0 / 256)
                        V([Rpa, R_sml, R_sp], [R_cqn[jb]], lambda e: e.scalar_tensor_tensor(out=cqn[jb][:], in0=pa[:, 0:256], scalar=sml[:, 4:5], in1=qln[:], op0=ALU.mult, op1=ALU.mult))
                        rope(pa[:, 256:320], Rpa, 64, tix, rOs["ki"][jb][:, 0:64], [R_rOs["ki"][jb]])
                        A([Rpa], [R_out["wi"]], lambda e: e.activation(out=wis[:, j, :], in_=pa[:, 320:328], func=AF.Identity, scale=512.0 ** -0.5))
                        V([Rpa, R_sp], [R_out["ig"]], lambda e: e.tensor_tensor(out=igs[:, j, :], in0=pa[:, 328:332], in1=bigt[:], op=ALU.add))
                        V([Rpa, R_sp], [R_sml], lambda e: e.tensor_tensor(out=sml[:, 8:12], in0=pa[:, 332:336], in1=bfgt[:], op=ALU.add))
                        A([R_sml], [R_sml], lambda e: e.activation(out=sml[:, 8:12], in_=sml[:, 8:12], func=AF.Exp, scale=-1.0))
                        A([R_sml], [R_sml], lambda e: e.activation(out=sml[:, 8:12], in_=sml[:, 8:12], func=AF.Ln, bias=1.0))
                        V([R_sml], [R_out["lf"]], lambda e: e.tensor_scalar(out=lfs[:, j, :], in0=sml[:, 8:12], scalar1=-1.0, scalar2=None, op0=ALU.mult))
                        rope(pk[:], Rpk, 512, tix, rOs["k"][jb][:], [R_rOs["k"][jb]])
                        pv, Rpv = next_pf()
                        proj(tsl, pv, Rpv, 768, 1280, 0)
                        A([Rpv], [R_out["V"]], lambda e: e.activation(out=Vs[:, j, :], in_=pv[:], func=AF.Copy))
                        pv, Rpv = next_pf()
                        proj(tsl, pv, Rpv, 2376, 2888, 0)
                        A([Rpv], [R_out["Vm"]], lambda e: e.activation(out=Vms[:, j, :], in_=pv[:], func=AF.Copy))
                        pv, Rpv = next_pf()
                        proj(tsl, pv, Rpv, 2888, 3400, 0)
                        A([Rpv], [R_out["og"]], lambda e: e.activation(out=ogs[:, j, :], in_=pv[:], func=AF.Sigmoid))

                    def stage_Q(j):
                        tix = sq * NT + sti * 4 + j
                        jb = j % 2
                        pt, Rp = next_pt()
                        for kc in range(2):
                            T([R_cqn[jb]] + RC, [Rp], lambda e: e.transpose(out=pt[:, kc * 128:(kc + 1) * 128], in_=cqn[jb][:, kc * 128:(kc + 1) * 128], identity=identb[:]))
                        A([Rp], [R_cqT], lambda e: e.activation(out=cqT[:], in_=pt[:, 0:256].rearrange("p (k t) -> p k t", t=128), func=AF.Copy))
                        for which, key in ((0, "q"), (1, "qi")):
                            pq, Rpq = next_pf()
                            for kc in range(2):
                                T([R_cqT, *R_w], [Rpq], lambda e: e.matmul(pq[:], lhsT=cqT[:, kc, :], rhs=wq_sb[:, kc, which * 512:(which + 1) * 512], start=(kc == 0), stop=(kc == 1)))
                            rope(pq[:], Rpq, 512, tix, rOs[key][jb][:], [R_rOs[key][jb]])

                    def stage_R(j):
                        tsl = slice(j * 128, (j + 1) * 128)
                        jb = j % 2
                        pt, Rp = next_pt()
                        T([R_rOs["ki"][jb]] + RC, [Rp], lambda e: e.transpose(out=pt[0:64, 0:128], in_=rOs["ki"][jb][:, 0:64], identity=identb[:]))
                        A([Rp], [R_out["kiT"]], lambda e: e.activation(out=kiTs[:, tsl], in_=pt[0:64, 0:128], func=AF.Copy))
                        for key, dstT, okey in (("k", kTs, "kT"), ("q", qTs, "qT"), ("qi", qiTs, "qiT")):
                            pt, Rp = next_pt()
                            for pr in range(4):
                                T([R_rOs[key][jb]] + RC, [Rp], lambda e: e.transpose(out=pt[:, pr * 128:(pr + 1) * 128], in_=rOs[key][jb][:, pr * 128:(pr + 1) * 128], identity=identb[:]))
                            A([Rp], [R_out[okey]], lambda e: e.activation(out=dstT[:, :, tsl], in_=pt[:, 0:512].rearrange("p (k t) -> p k t", t=128), func=AF.Copy))

                    def stage_FM(ch):
                        pc, Rpc = next_pf()
                        c0 = 1352 + ch * 128
                        for kc in range(8):
                            T([R_hT, *R_w], [Rpc], lambda e: e.matmul(pc[:], lhsT=w_in_sb[:, kc, c0:c0 + 128], rhs=hT[:, kc, :], start=(kc == 0), stop=(kc == 7)))
                        A([Rpc], [R_pre], lambda e: e.activation(out=pre[:, ch, 3:515], in_=pc[:], func=AF.Copy))
                        V([R_pre, R_sp], [R_acc], lambda e: e.tensor_scalar(out=acc[:], in0=pre[:, ch, 0:512], scalar1=cw[:, ch, 0:1], scalar2=None, op0=ALU.mult))
                        for jj in range(1, 4):
                            V([R_pre, R_sp, R_acc], [R_acc], lambda e: e.scalar_tensor_tensor(out=acc[:], in0=pre[:, ch, jj:jj + 512], scalar=cw[:, ch, jj:jj + 1], in1=acc[:], op0=ALU.mult, op1=ALU.add))
                        A([R_acc, R_sp], [R_out["qmT"]], lambda e: e.activation(out=qmTs[:, ch, :], in_=acc[:], func=AF.Silu, bias=cb[:, ch:ch + 1]))

                    for j in range(4):
                        stage_P(j)
                        stage_FM(2 * j)
                        stage_FM(2 * j + 1)
                        stage_Q(j)
                        if j >= 1:
                            stage_R(j - 1)
                    stage_R(3)
                    V([R_pre], [R_pre], lambda e: e.tensor_copy(out=pre[:, :, 0:3], in_=pre[:, :, 512:515]))

                    tsl2 = slice(sti * 512, (sti + 1) * 512)
                    if sti == 0:
                        R_p1[sq] = []

                    def Rp1_new():
                        r_ = Region()
                        R_p1[sq].append(r_)
                        return [r_]
                    cx.dma("gpsimd", qT_d[sq, :, :, tsl2], qTs[:], [R_out["qT"]], Rp1_new())
                    cx.dma("gpsimd", qiT_d[sq, :, :, tsl2], qiTs[:], [R_out["qiT"]], Rp1_new())
                    cx.dma("gpsimd", kT_d[sq, :, :, tsl2], kTs[:], [R_out["kT"]], Rp1_new())
                    cx.dma("gpsimd", kiT_d[sq, :, tsl2], kiTs[:], [R_out["kiT"]], Rp1_new())
                    cx.dma("gpsimd", qmT_d[sq, :, :, tsl2], qmTs[:], [R_out["qmT"]], Rp1_new())
                    for (dd, sb_, key) in ((V_d, Vs, "V"), (Vm_d, Vms, "Vm"), (og_d, ogs, "og"), (wi_d, wis, "wi"), (ig_d, igs, "ig"), (lf_d, lfs, "lf")):
                        cx.dma("gpsimd", dd[sq, tsl2, :].rearrange("(j p) c -> p j c", p=128), sb_[:], [R_out[key]], Rp1_new())

                items = [(sq_, sti_) for sq_ in range(NSEQ) for sti_ in range(4)]
                p1_front(items[0][0], items[0][1], hTs[0], R_hTs[0])
                for k_, (sq_, sti_) in enumerate(items):
                    if k_ + 1 < len(items):
                        p1_front(items[k_ + 1][0], items[k_ + 1][1], hTs[(k_ + 1) % 2], R_hTs[(k_ + 1) % 2])
                    p1_back(sq_, sti_, hTs[k_ % 2], R_hTs[k_ % 2])
            cx.barrier()

            with ExitStack() as st:
                qT = sbuf(st, "qT_sb", [128, 4, S], BF16)
                qiT = sbuf(st, "qiT_sb", [128, 4, S], BF16)
                kT = sbuf(st, "kT_sb", [128, 4, S], BF16)
                kiT = sbuf(st, "kiT_sb", [128, S], BF16)
                Vx = sbuf(st, "Vx", [128, NT, 8, 65], BF16)
                wi = sbuf(st, "wi_sb", [128, NT, 8], F32)
                aon = sbuf(st, "aon", [128, 512], F32)
                R_in = Region()
                R_sp = Region()
                cx.dma("sync", aon[:], aon_d[l:l + 1, :].partition_broadcast(128), [], [R_sp])
                dg = [sbuf(st, "dg%d" % i, [128, 8, 128], BF16) for i in range(2)]
                R_dg = [Region() for _ in range(2)]
                NRB = 8
                rl = [sbuf(st, "rl%d" % i, [128, 512], BF16) for i in range(NRB)]
                R_rl = [Region() for _ in range(NRB)]
                sc = [sbuf(st, "sc%d" % i, [128, S], F32) for i in range(4)]
                R_sc = [Region() for _ in range(4)]
                junkA = sbuf(st, "junkA", [128, S], BF16)
                R_junkA = Region()
                bis = [dict(lo=sbuf(st, "b_lo%d" % i, [128, 4], F32), dk=sbuf(st, "b_dk%d" % i, [128, 32], F32),
                            ndk=sbuf(st, "b_ndk%d" % i, [128, 32], F32), S=sbuf(st, "b_S%d" % i, [128, 32], F32),
                            nmid=sbuf(st, "b_nm%d" % i, [128, 1], F32), g=sbuf(st, "b_g%d" % i, [128, 1], F32),
                            inc=sbuf(st, "b_inc%d" % i, [128, 1], F32)) for i in range(2)]
                R_bis = [dict(lo=Region(), dk=Region(), S=Region(), nmid=Region(), g=Region(), inc=Region()) for _ in range(2)]
                NRND = 26
                wk = sbuf(st, "wk", [128, S], F32)
                R_wk = Region()
                m8 = [sbuf(st, "m8_%d" % i, [128, 8], F32) for i in range(2)]
                R_m8 = [Region() for _ in range(2)]
                mb = [sbuf(st, "mb%d" % i, [128, S], BF16) for i in range(4)]
                R_mb = [Region() for _ in range(4)]
                PT = [sbuf(st, "PT%d" % i, [128, NT, 128], BF16) for i in range(2)]
                R_PT = [Region() for _ in range(2)]
                atf = [sbuf(st, "atf%d" % i, [128, 8, 64], F32) for i in range(2)]
                R_atf = [Region() for _ in range(2)]
                lnd = [sbuf(st, "lnd%d" % i, [128, 16], F32) for i in range(2)]
                R_ev = [Region() for _ in range(2)]
                rs = [sbuf(st, "rs%d" % i, [128, 8], F32) for i in range(2)]
                R_rs = [Region() for _ in range(2)]
                sqf = sbuf(st, "sqf", [128, 8, 64], F32)
                R_sqf = Region()
                att_st = [sbuf(st, "att_st%d" % i, [128, 512], BF16) for i in range(2)]
                R_ast = [Region() for _ in range(2)]
                sml = sbuf(st, "sml2", [128, 24], F32)
                R_sml = Region()
                pl = [psum(st, "pl%d" % i, [128, 512], F32) for i in range(3)]
                R_pl = [Region() for _ in range(3)]
                psc = psum(st, "psc", [128, 512], F32)
                R_psc = Region()
                plt = [psum(st, "plt%d" % i, [128, 512], F32) for i in range(2)]
                R_plt = [Region() for _ in range(2)]
                pav = [psum(st, "pav%d" % i, [128, 512], F32) for i in range(2)]
                R_pav = [Region() for _ in range(2)]
                cnts = {"pl": 0, "rl": 0, "lt": 0, "pt": 0}

                def emit_scores(sq, i):
                    ns = (i + 1) * 128
                    tq = slice(i * 128, (i + 1) * 128)
                    dgb, Rdg = dg[i % 2], R_dg[i % 2]
                    scb, Rsc = sc[i % 4], R_sc[i % 4]
                    for h in range(8):
                        V([R_in] + RC, [Rdg], lambda e: e.tensor_scalar(out=dgb[:, h, :], in0=identf[:], scalar1=wi[:, i, h:h + 1], scalar2=None, op0=ALU.mult))
                    for c0 in range(0, ns, 512):
                        cols = min(512, ns - c0)
                        rbuf = []
                        for h in range(8):
                            po = (h % 2) * 64
                            pb, Rpb = pl[cnts["pl"] % 3], R_pl[cnts["pl"] % 3]
                            cnts["pl"] += 1
                            T([R_in], [Rpb], lambda e: e.matmul(pb[:, 0:cols], lhsT=qiT[po:po + 64, h // 2, tq], rhs=kiT[po:po + 64, c0:c0 + cols], start=True, stop=True))
                            rb, Rrb = rl[cnts["rl"] % NRB], R_rl[cnts["rl"] % NRB]
                            cnts["rl"] += 1
                            if h not in (1, 3, 5):
                                A([Rpb], [Rrb], lambda e: e.activation(out=rb[:, 0:cols], in_=pb[:, 0:cols], func=AF.Relu))
                            else:
                                V([Rpb], [Rrb], lambda e: e.tensor_scalar(out=rb[:, 0:cols], in0=pb[:, 0:cols], scalar1=0.0, scalar2=None, op0=ALU.max))
                            rbuf.append((rb, Rrb))
                        for h in range(8):
                            rb, Rrb = rbuf[h]
                            T([Rrb, Rdg], [R_psc], lambda e: e.matmul(psc[:, 0:cols], lhsT=dgb[:, h, :], rhs=rb[:, 0:cols], start=(h == 0), stop=(h == 7)))
                        V([R_psc], [Rsc], lambda e: e.tensor_copy(out=scb[:, c0:c0 + cols], in_=psc[:, 0:cols]))
                    V([Rsc] + RC, [Rsc], lambda e: e.tensor_tensor(out=scb[:, tq], in0=scb[:, tq], in1=negtri[:], op=ALU.add))

                def emit_mask(i, thr, Rthr):
                    ns = (i + 1) * 128
                    scb, Rsc = sc[i % 4], R_sc[i % 4]
                    V([Rsc, Rthr], [R_mb[i % 4]], lambda e: e.tensor_scalar(out=mb[i % 4][:, 0:ns], in0=scb[:, 0:ns], scalar1=thr, scalar2=-30000.0, op0=ALU.is_lt, op1=ALU.mult))

                def emit_topk_dve(i):
                    ns = (i + 1) * 128
                    bb = i % 2
                    scb, Rsc = sc[i % 4], R_sc[i % 4]
                    if i >= 2:
                        nr = TOPK // 8
                        for r in range(nr):
                            src = scb if r == 0 else wk
                            Rs = Rsc if r == 0 else R_wk
                            V([Rs], [R_m8[bb]], lambda e: e.max(out=m8[bb][:], in_=src[:, 0:ns]))
                            if r < nr - 1:
                                V([Rs, R_m8[bb]], [R_wk], lambda e: e.match_replace(out=wk[:, 0:ns], in_to_replace=m8[bb][:], in_values=src[:, 0:ns], imm_value=NEG))
                        emit_mask(i, m8[bb][:, 7:8], R_m8[bb])
                    else:
                        emit_mask(i, thr0[:, 0:1], R_c6)

                def emit_topk_act(i, steps=()):
                    ns = (i + 1) * 128
                    scb, Rsc = sc[i % 4], R_sc[i % 4]
                    bt, Rb = bis[(i // 2) % 2], R_bis[(i // 2) % 2]
                    lo, dk, ndk, Sb, nmid, g, inc = bt["lo"], bt["dk"], bt["ndk"], bt["S"], bt["nmid"], bt["g"], bt["inc"]
                    V([Rsc], [Rb["lo"]], lambda e: e.tensor_reduce(out=lo[:, 0:1], in_=scb[:, 0:256], axis=AX.X, op=ALU.min))
                    V([Rsc], [Rb["lo"]], lambda e: e.tensor_reduce(out=lo[:, 1:2], in_=scb[:, 0:ns], axis=AX.X, op=ALU.max))
                    V([Rb["lo"]], [Rb["lo"]], lambda e: e.tensor_tensor(out=lo[:, 2:3], in0=lo[:, 1:2], in1=lo[:, 0:1], op=ALU.subtract))
                    V([Rb["lo"]] + RC, [Rb["dk"]], lambda e: e.tensor_scalar(out=dk[:], in0=pw2[:], scalar1=lo[:, 2:3], scalar2=None, op0=ALU.mult))
                    V([Rb["dk"]], [Rb["dk"]], lambda e: e.tensor_scalar(out=ndk[:], in0=dk[:], scalar1=-1.0, scalar2=None, op0=ALU.mult))
                    V([], [Rb["S"]], lambda e: e.memset(Sb[:], 0.0))
                    for k in range(NRND):
                        A([Rb["lo"], Rb["dk"]], [Rb["nmid"]], lambda e: e.activation(out=nmid[:], in_=lo[:, 0:1], func=AF.Identity, scale=-1.0, bias=ndk[:, k:k + 1]))
                        A([Rsc, Rb["nmid"], Rb["S"]], [R_junkA, Rb["S"]], lambda e: e.activation(out=junkA[:, 0:ns], in_=scb[:, 0:ns], func=AF.Sign, bias=nmid[:, 0:1], accum_out=Sb[:, k:k + 1]))
                        A([Rb["S"]], [Rb["g"]], lambda e: e.activation(out=g[:], in_=Sb[:, k:k + 1], func=AF.Sign, bias=float(ns - 511)))
                        A([Rb["g"], Rb["dk"]], [Rb["inc"]], lambda e: e.activation(out=inc[:], in_=g[:], func=AF.Relu, scale=dk[:, k:k + 1]))
                        A([Rb["inc"], Rb["lo"]], [Rb["lo"]], lambda e: e.activation(out=lo[:, 0:1], in_=inc[:], func=AF.Identity, bias=lo[:, 0:1]))
                        if k >= 2 and steps:
                            steps.pop(0)()
                    while steps:
                        steps.pop(0)()

                def emit_mask_act(i):
                    bt, Rb = bis[(i // 2) % 2], R_bis[(i // 2) % 2]
                    emit_mask(i, bt["lo"][:, 0:1], Rb["lo"])

                def attn_steps(sq, i):
                    tq = slice(i * 128, (i + 1) * 128)
                    bb = i % 2
                    mbb, Rmb = mb[i % 4], R_mb[i % 4]

                    def logits(h):
                        po = (h % 2) * 64
                        pr = h // 2
                        ptb_, Rptb = PT[h % 2], R_PT[h % 2]
                        for g0 in range(0, i + 1, 4):
                            g1 = min(g0 + 4, i + 1)
                            lt, Rlt = plt[cnts["lt"] % 2], R_plt[cnts["lt"] % 2]
                            cnts["lt"] += 1
                            for j in range(g0, g1):
                                o = (j - g0) * 128
                                ks = slice(j * 128, (j + 1) * 128)
                                T([R_in], [Rlt], lambda e: e.matmul(lt[:, o:o + 128], lhsT=kT[po:po + 64, pr, ks], rhs=qT[po:po + 64, pr, tq], start=True, stop=False))
                                T([Rmb] + RC, [Rlt], lambda e: e.matmul(lt[:, o:o + 128], lhsT=mbb[:, ks], rhs=identb[:], start=False, stop=True))
                            ncol = (g1 - g0) * 128
                            A([Rlt], [Rptb], lambda e: e.activation(out=ptb_[:, g0:g1, :], in_=lt[:, 0:ncol].rearrange("p (j t) -> p j t", t=128), func=AF.Exp, scale=0.125))

                    def pv(h):
                        ptb_, Rptb = PT[h % 2], R_PT[h % 2]
                        av = pav[h // 4]
                        Rav = R_pav[h // 4]
                        o = (h % 4) * 65
                        for j in range(i + 1):
                            T([Rptb, R_in], [Rav], lambda e: e.matmul(av[:, o:o + 65], lhsT=ptb_[:, j, :], rhs=Vx[:, j, h, :], start=(j == 0), stop=(j == i)))

                    def evac():
                        for hh in range(2):
                            av3 = pav[hh][:, 0:260].rearrange("p (h c) -> p h c", c=65)
                            A([R_pav[hh]], [R_ev[bb]], lambda e: e.activation(out=lnd[bb][:, hh * 4:hh * 4 + 4].unsqueeze(2), in_=av3[:, :, 64:65], func=AF.Ln))
                        A([R_ev[bb]], [R_ev[bb]], lambda e: e.activation(out=lnd[bb][:, 8:16], in_=lnd[bb][:, 0:8], func=AF.Exp, scale=-1.0))
                        for h in range(8):
                            av3 = pav[h // 4][:, 0:260].rearrange("p (h c) -> p h c", c=65)
                            A([R_pav[h // 4], R_ev[bb]], [R_atf[bb]], lambda e: e.activation(out=atf[bb][:, h, :], in_=av3[:, h % 4, 0:64], func=AF.Identity, scale=lnd[bb][:, 8 + h:9 + h]))

                    steps = [lambda: logits(0)]
                    for h in range(8):
                        def st_(h=h):
                            if h + 1 < 8:
                                logits(h + 1)
                            pv(h)
                        steps.append(st_)
                    steps.append(evac)
                    return steps

                def attn_norm(sq, i):
                    bb = i % 2
                    G([R_atf[bb]], [R_sqf], lambda e: e.tensor_tensor(out=sqf[:], in0=atf[bb][:], in1=atf[bb][:], op=ALU.mult))
                    V([R_sqf], [R_rs[bb]], lambda e: e.tensor_reduce(out=rs[bb][:], in_=sqf[:], axis=AX.X, op=ALU.add))
                    V([R_rs[bb]], [R_rs[bb]], lambda e: e.tensor_scalar(out=rs[bb][:], in0=rs[bb][:], scalar1=1.0 / 64, scalar2=EPS, op0=ALU.mult, op1=ALU.add))

                def emit_attn_fin(sq, i):
                    tq = slice(i * 128, (i + 1) * 128)
                    bb = i % 2
                    A([R_rs[bb]], [R_rs[bb]], lambda e: e.activation(out=rs[bb][:], in_=rs[bb][:], func=AF.Ln))
                    A([R_rs[bb]], [R_rs[bb]], lambda e: e.activation(out=rs[bb][:], in_=rs[bb][:], func=AF.Exp, scale=-0.5))
                    G([R_atf[bb], R_rs[bb]], [R_sqf], lambda e: e.tensor_tensor(out=sqf[:], in0=atf[bb][:], in1=bc_last(rs[bb][:], 64), op=ALU.mult))
                    G([R_sqf, R_sp], [R_ast[bb]], lambda e: e.tensor_tensor(out=att_st[bb][:], in0=sqf[:].rearrange("p h d -> p (h d)"), in1=aon[:], op=ALU.mult))
                    cx.dma("gpsimd", att_d[sq, tq, :], att_st[bb][:], [R_ast[bb]], [R_att[sq][i]])

                dummy = sbuf(st, "dummy2", [128, 1], F32)
                for sq in range(NSEQ):
                    if sq > 0:
                        cx.barrier()
                    R_ms = Region()
                    V([], [R_ms], lambda e: e.memset(Vx[:], 1.0))
                    rd = list(R_p1[sq]) + [R_ms]
                    Ls = []

                    def ld(out_, in__):
                        r_ = Region()
                        Ls.append(r_)
                        cx.dma("sync", out_, in__, rd, [r_])
                    ld(qT[:], qT_d[sq])
                    ld(qiT[:], qiT_d[sq])
                    ld(kT[:], kT_d[sq])
                    ld(kiT[0:64, :], kiT_d[sq])
                    ld(kiT[64:128, :], kiT_d[sq])
                    ld(wi[:], wi_d[sq].rearrange("(j p) c -> p j c", p=128))
                    for jt in range(NT):
                        ld(Vx[:, jt, :, 0:64], V_d[sq, jt * 128:(jt + 1) * 128, :].rearrange("p (h d) -> p h d", d=64))
                    V(Ls, [R_in], lambda e: e.memset(dummy[:], 0.0))
                    emit_scores(sq, 0)
                    emit_scores(sq, 1)
                    emit_topk_dve(0)
                    emit_topk_dve(1)
                    for p in range(NT // 2):
                        if p >= 1:
                            emit_attn_fin(sq, 2 * p - 2)
                            emit_attn_fin(sq, 2 * p - 1)
                        steps = attn_steps(sq, 2 * p) + attn_steps(sq, 2 * p + 1)
                        if p + 1 < NT // 2:
                            ia, ib = 2 * p + 2, 2 * p + 3
                            emit_scores(sq, ia)
                            emit_scores(sq, ib)
                            if ia >= 2:
                                emit_topk_act(ib, steps)
                                emit_topk_dve(ia)
                                emit_mask_act(ib)
                            else:
                                emit_topk_dve(ia)
                                emit_topk_dve(ib)
                        while steps:
                            steps.pop(0)()
                        attn_norm(sq, 2 * p)
                        attn_norm(sq, 2 * p + 1)
                    emit_attn_fin(sq, NT - 2)
                    emit_attn_fin(sq, NT - 1)
            cx.barrier()

            st_w = ExitStack()
            w_dn_sb = sbuf(st_w, "w_dn_sb", [128, NFC, D], BF16)
            R_wo = [Region() for _ in range(8)]
            R_wd = [Region() for _ in range(NFC)]
            for fc in range(NFC):
                cx.dma("gpsimd", w_dn_sb[:, fc, :], w_dn_d[l, fc * 128:(fc + 1) * 128, :], [], [R_wd[fc]])

            with ExitStack() as st:
                qmT = sbuf(st, "qmT_sb", [128, 8, S], BF16)
                Vmx = sbuf(st, "Vmx", [128, NT, 4, 129], BF16)
                og = sbuf(st, "og_sb", [128, NT, 512], BF16)
                ig = sbuf(st, "ig_sb", [128, NT, 4], F32)
                lf = sbuf(st, "lf_sb", [128, NT, 4], F32)
                mon = sbuf(st, "mon", [128, 512], F32)
                R_in = Region()
                R_sp = Region()
                cx.dma("sync", mon[:], mon_d[l:l + 1, :].partition_broadcast(128), [], [R_sp])
                Cf = sbuf(st, "Cf", [128, 4, 129], F32)
                Cb = sbuf(st, "Cb", [128, 4, 129], BF16)
                R_Cf = [Region() for _ in range(4)]
                R_Cb = [Region() for _ in range(4)]
                LU = sbuf(st, "LU", [128, 4, 128], F32)
                R_LU = [Region() for _ in range(4)]
                ebt = sbuf(st, "ebt", [128, 4, 128], F32)
                R_ebt = Region()
                DT = sbuf(st, "DT", [128, 4, 128], F32)
                R_DT = Region()
                STm = sbuf(st, "STm", [128, 4, 128], BF16)
                R_ST = [Region() for _ in range(4)]
                qtl = sbuf(st, "qtl", [128, 4, 128], BF16)
                R_qtl = [Region() for _ in range(4)]
                kw = sbuf(st, "kw", [128, 4, 128], BF16)
                R_kw = [Region() for _ in range(4)]
                sm = sbuf(st, "sm3", [128, 40], F32)
                R_sm = Region()
                hmf = sbuf(st, "hmf", [128, 4, 128], F32)
                hmq = sbuf(st, "hmq", [128, 4, 128], F32)
                R_hmf, R_hmq = Region(), Region()
                hm_st = sbuf(st, "hm_st", [128, 512], BF16)
                R_hst = Region()
                lnscale = math.log(128.0 ** -0.5)
                lnsc = sbuf(st, "lnsc", [128, 1], F32)
                V([], [R_sp], lambda e: e.memset(lnsc[:], lnscale))
                pB = psum(st, "pB", [128, 512], F32)
                pB2 = psum(st, "pB2", [128, 512], F32)
                pS = psum(st, "pS", [128, 512], F32)
                pH = [psum(st, "pH%d" % i, [128, 512], F32) for i in range(2)]
                pC = [psum(st, "pC%d" % i, [128, 512], F32) for i in range(2)]
                pK = psum(st, "pK", [128, 1024], BF16)
                R_pB, R_pB2, R_pS, R_pK = Region(), Region(), Region(), Region()
                R_pH = [Region() for _ in range(2)]
                R_pC = [Region() for _ in range(2)]
                R_bcol = Region()
                pB3 = pB[:].rearrange("p (h t) -> p h t", t=128)
                pB23 = pB2[:].rearrange("p (h t) -> p h t", t=128)
                pS3 = pS[:].rearrange("p (h t) -> p h t", t=128)
                dummy = sbuf(st, "dummy3", [128, 1], F32)
                for sq in range(NSEQ):
                    if sq > 0:
                        cx.barrier()
                    R_ms = Region()
                    V([], [R_ms], lambda e: e.memset(Vmx[:], 1.0))
                    rd = list(R_p1[sq]) + [R_ms]
                    Ls = []

                    def ld(out_, in__):
                        r_ = Region()
                        Ls.append(r_)
                        cx.dma("sync", out_, in__, rd, [r_])
                    ld(qmT[:], qmT_d[sq])
                    ld(ig[:], ig_d[sq].rearrange("(j p) c -> p j c", p=128))
                    ld(lf[:], lf_d[sq].rearrange("(j p) c -> p j c", p=128))
                    for jt in range(NT):
                        ld(Vmx[:, jt, :, 0:128], Vm_d[sq, jt * 128:(jt + 1) * 128, :].rearrange("p (h d) -> p h d", d=128))
                    ld(og[:], og_d[sq].rearrange("(j p) c -> p j c", p=128))
                    V(Ls, [R_in], lambda e: e.memset(dummy[:], 0.0))
                    for h in range(4):
                        V([], [R_Cf[h]], lambda e: e.memset(Cf[:, h, :], 0.0))
                        V([], [R_Cb[h]], lambda e: e.memset(Cb[:, h, :], 0.0))
                    for c in range(NT):
                        tq = slice(c * 128, (c + 1) * 128)
                        for h in range(4):
                            V([R_in] + RC, [R_LU[h]], lambda e: e.tensor_scalar(out=LU[:, h, :], in0=utri[:], scalar1=lf[:, c, h:h + 1], scalar2=None, op0=ALU.mult))
                            T([R_LU[h]] + RC, [R_pB], lambda e: e.matmul(pB3[:, h, :], lhsT=onesf[:], rhs=LU[:, h, :], start=True, stop=True))
                            T([R_LU[h]] + RC, [R_pB2], lambda e: e.matmul(pB23[:, h, :], lhsT=onesf[:], rhs=LU[:, h, :], start=True, stop=False))
                            T(RC, [R_pB2], lambda e: e.matmul(pB23[:, h, :], lhsT=identf[:], rhs=negm[:], start=False, stop=True))
                        T([R_in] + RC, [R_bcol], lambda e: e.matmul(pH[0][:, 300:304], lhsT=utri[:], rhs=lf[:, c, :], start=True, stop=True))
                        V([R_bcol, R_in], [R_sm], lambda e: e.tensor_tensor(out=sm[:, 0:4], in0=ig[:, c, :], in1=pH[0][:, 300:304], op=ALU.subtract))
                        V([R_sm], [R_sm], lambda e: e.tensor_scalar(out=sm[:, 4:8], in0=sm[:, 0:4], scalar1=lnscale, scalar2=None, op0=ALU.add))
                        V([R_sm, R_pB], [R_sm], lambda e: e.tensor_tensor(out=sm[:, 8:12].unsqueeze(2), in0=sm[:, 0:4].unsqueeze(2), in1=pB3[:, :, 127:128], op=ALU.add))
                        A([R_sm], [R_sm], lambda e: e.activation(out=sm[:, 12:16], in_=sm[:, 8:12], func=AF.Exp))
                        A([R_pB], [R_sm], lambda e: e.activation(out=sm[:, 16:20].unsqueeze(2), in_=pB3[:, :, 127:128], func=AF.Exp))
                        A([R_pB, R_sp], [R_ebt], lambda e: e.activation(out=ebt[:].rearrange("p h t -> p (h t)"), in_=pB[:], func=AF.Exp, bias=lnsc[:, 0:1]))
                        for h in range(4):
                            A([R_pB2, R_sm], [R_DT], lambda e: e.activation(out=DT[:, h, :], in_=pB23[:, h, :], func=AF.Exp, bias=sm[:, 4 + h:5 + h]))
                        for h in range(4):
                            T([R_in], [R_pS], lambda e: e.matmul(pS3[:, h, :], lhsT=qmT[:, 4 + h, tq], rhs=qmT[:, h, tq], start=True, stop=True))
                            T([R_in] + RC, [R_pK], lambda e: e.transpose(out=pK[:, h * 128:(h + 1) * 128], in_=qmT[:, 4 + h, tq], identity=identb[:]))
                        for h in range(4):
                            V([R_pS, R_DT], [R_ST[h]], lambda e: e.tensor_tensor(out=STm[:, h, :], in0=pS3[:, h, :], in1=DT[:, h, :], op=ALU.mult))
                            V([R_in, R_ebt], [R_qtl[h]], lambda e: e.tensor_tensor(out=qtl[:, h, :], in0=qmT[:, h, tq], in1=ebt[:, h, :], op=ALU.mult))
                            V([R_pK, R_sm], [R_kw[h]], lambda e: e.tensor_scalar(out=kw[:, h, :], in0=pK[:, h * 128:(h + 1) * 128], scalar1=sm[:, 12 + h:13 + h], scalar2=None, op0=ALU.mult))
                        for h in range(4):
                            ph = pH[h // 2]
                            o = (h % 2) * 129
                            T([R_ST[h], R_in], [R_pH[h // 2]], lambda e: e.matmul(ph[:, o:o + 129], lhsT=STm[:, h, :], rhs=Vmx[:, c, h, :], start=True, stop=False))
                            T([R_qtl[h], R_Cb[h]], [R_pH[h // 2]], lambda e: e.matmul(ph[:, o:o + 129], lhsT=qtl[:, h, :], rhs=Cb[:, h, :], start=False, stop=True))
                        for h in range(4):
                            pc = pC[h // 2]
                            o = (h % 2) * 129
                            T([R_kw[h], R_in], [R_pC[h // 2]], lambda e: e.matmul(pc[:, o:o + 129], lhsT=kw[:, h, :], rhs=Vmx[:, c, h, :], start=True, stop=True))
                            V([R_pC[h // 2], R_sm, R_Cf[h]], [R_Cf[h]], lambda e: e.scalar_tensor_tensor(out=Cf[:, h, :], in0=Cf[:, h, :], scalar=sm[:, 16 + h:17 + h], in1=pc[:, o:o + 129], op0=ALU.mult, op1=ALU.add))
                            A([R_Cf[h]], [R_Cb[h]], lambda e: e.activation(out=Cb[:, h, :], in_=Cf[:, h, :], func=AF.Copy))
                        for hh in range(2):
                            ph3 = pH[hh][:, 0:258].rearrange("p (h c) -> p h c", c=129)
                            V([R_pH[hh]], [R_sm], lambda e: e.tensor_copy(out=sm[:, 32 + hh * 2:34 + hh * 2].unsqueeze(2), in_=ph3[:, :, 128:129]))
                            V([R_sm], [R_sm], lambda e: e.scalar_tensor_tensor(out=sm[:, 20 + hh * 2:22 + hh * 2], in0=sm[:, 32 + hh * 2:34 + hh * 2], scalar=-1.0, in1=sm[:, 32 + hh * 2:34 + hh * 2], op0=ALU.mult, op1=ALU.max))
                            V([R_sm], [R_sm], lambda e: e.tensor_scalar(out=sm[:, 20 + hh * 2:22 + hh * 2], in0=sm[:, 20 + hh * 2:22 + hh * 2], scalar1=1.0, scalar2=None, op0=ALU.max))
                            V([R_sm], [R_sm], lambda e: e.reciprocal(out=sm[:, 24 + hh * 2:26 + hh * 2], in_=sm[:, 20 + hh * 2:22 + hh * 2]))
                            V([R_pH[hh], R_sm], [R_hmf], lambda e: e.tensor_tensor(out=hmf[:, hh * 2:hh * 2 + 2, :], in0=ph3[:, :, 0:128], in1=bc_last(sm[:, 24 + hh * 2:26 + hh * 2], 128), op=ALU.mult))
                        V([R_hmf], [R_hmq], lambda e: e.tensor_tensor(out=hmq[:], in0=hmf[:], in1=hmf[:], op=ALU.mult))
                        V([R_hmq], [R_sm], lambda e: e.tensor_reduce(out=sm[:, 28:32], in_=hmq[:], axis=AX.X, op=ALU.add))
                        V([R_sm], [R_sm], lambda e: e.tensor_scalar(out=sm[:, 28:32], in0=sm[:, 28:32], scalar1=1.0 / 128, scalar2=EPS, op0=ALU.mult, op1=ALU.add))
                        A([R_sm], [R_sm], lambda e: e.activation(out=sm[:, 28:32], in_=sm[:, 28:32], func=AF.Ln))
                        A([R_sm], [R_sm], lambda e: e.activation(out=sm[:, 28:32], in_=sm[:, 28:32], func=AF.Exp, scale=-0.5))
                        V([R_hmf, R_sm], [R_hmq], lambda e: e.tensor_tensor(out=hmq[:], in0=hmf[:], in1=bc_last(sm[:, 28:32], 128), op=ALU.mult))
                        V([R_hmq, R_sp], [R_hmq], lambda e: e.tensor_tensor(out=hmq[:].rearrange("p h d -> p (h d)"), in0=hmq[:].rearrange("p h d -> p (h d)"), in1=mon[:], op=ALU.mult))
                        V([R_hmq, R_in], [R_hst], lambda e: e.tensor_tensor(out=hm_st[:], in0=hmq[:].rearrange("p h d -> p (h d)"), in1=og[:, c, :], op=ALU.mult))
                        cx.dma("gpsimd", hm_d[sq, tq, :], hm_st[:], [R_hst], [R_hm[sq][c]])
            cx.barrier()

            w_gu_sb = sbuf(st_w, "w_gu_sb", [128, 8, 2 * FH], BF16)
            R_wg = [Region() for _ in range(16)]
            with ExitStack() as st:
                R_w = R_wo
                w_out_sb = sbuf(st, "w_out_sb", [128, 8, D], BF16)
                for kc in range(8):
                    cx.dma("gpsimd", w_out_sb[:, kc, :], w_out_d[l, kc * 128:(kc + 1) * 128, :], [], [R_wo[kc]])
                def load_wgu(ix):
                    kc, half = ix // 2, ix % 2
                    cx.dma("gpsimd", w_gu_sb[:, kc, half * FH:(half + 1) * FH], w_gu_d[l, kc * 128:(kc + 1) * 128, half * FH:(half + 1) * FH], [], [R_wg[ix]])
                gpost = sbuf(st, "gpost", [128, D], F32)
                R_sp = Region()
                cx.dma("sync", gpost[:], npost_d[l:l + 1, :].partition_broadcast(128), [], [R_sp])
                GM = sbuf(st, "GM", [128, D], F32)
                R_gm = Region()
                mx = [sbuf(st, "mx%d" % i, [128, D], BF16) for i in range(2)]
                R_mx = [Region() for _ in range(2)]
                mTs = [sbuf(st, "mT%d" % i, [128, 8, 128], BF16) for i in range(2)]
                R_mTs = [Region() for _ in range(2)]
                xt = [sbuf(st, "xt4_%d" % i, [128, D], F32) for i in range(2)]
                R_xt = [Region() for _ in range(2)]
                tmpf = sbuf(st, "tmp4", [128, D], F32)
                R_tmpf = Region()
                xo = [sbuf(st, "xo4_%d" % i, [128, D], F32) for i in range(2)]
                R_xo = [Region() for _ in range(2)]
                junk = sbuf(st, "junk4", [128, 512], BF16)
                R_junk = Region()
                sml = sbuf(st, "sml4", [128, 8], F32)
                R_sml = Region()
                py = [psum(st, "py%d" % i, [128, 512], F32) for i in range(4)]
                R_py = [Region() for _ in range(4)]
                ptb = [psum(st, "pt4_%d" % i, [128, 1024], BF16) for i in range(2)]
                R_pt = [Region() for _ in range(2)]
                def f4a_front(sq, i, b):
                    tq = slice(i * 128, (i + 1) * 128)
                    tg = slice(sq * S + i * 128, sq * S + (i + 1) * 128)
                    cx.dma("sync", mx[b][:, 0:512], att_d[sq, tq, :], [R_att[sq][i]], [R_mx[b]])
                    cx.dma("sync", mx[b][:, 512:1024], hm_d[sq, tq, :], [R_hm[sq][i]], [R_mx[b]])
                    rd = [] if R_xin is None else [R_xin[sq][i]]
                    cx.dma("sync", xt[b][:], xin_d[tg, :], rd, [R_xt[b]])
                    pt, Rp = ptb[b], R_pt[b]
                    for kc in range(8):
                        T([R_mx[b]] + RC, [Rp], lambda e: e.transpose(out=pt[:, kc * 128:(kc + 1) * 128], in_=mx[b][:, kc * 128:(kc + 1) * 128], identity=identb[:]))
                    A([Rp], [R_mTs[b]], lambda e: e.activation(out=mTs[b][:], in_=pt[:].rearrange("p (k t) -> p k t", t=128), func=AF.Copy))
                    for n in range(2):
                        pyb, Rpy = py[b * 2 + n], R_py[b * 2 + n]
                        for kc in range(8):
                            T([R_mTs[b], *R_w], [Rpy], lambda e: e.matmul(pyb[:], lhsT=mTs[b][:, kc, :], rhs=w_out_sb[:, kc, n * 512:(n + 1) * 512], start=(kc == 0), stop=(kc == 7)))

                def f4a_back(sq, i, b):
                    tg = slice(sq * S + i * 128, sq * S + (i + 1) * 128)
                    if i == 0:
                        cx.dma("sync", GM[:], modd[l, sq:sq + 1, 2 * D:3 * D].partition_broadcast(128), [R_modd], [R_gm])
                        V([R_gm, R_sp], [R_gm], lambda e: e.tensor_tensor(out=GM[:], in0=GM[:], in1=gpost[:], op=ALU.mult))
                    V([], [R_sml], lambda e: e.memset(sml[:, 0:2], 0.0))
                    for n in range(2):
                        pyb, Rpy = py[b * 2 + n], R_py[b * 2 + n]
                        A([Rpy, R_sml], [R_junk, R_sml], lambda e: e.activation(out=junk[:], in_=pyb[:], func=AF.Square, accum_out=sml[:, n:n + 1]))
                    V([R_sml], [R_sml], lambda e: e.tensor_tensor(out=sml[:, 2:3], in0=sml[:, 0:1], in1=sml[:, 1:2], op=ALU.add))
                    V([R_sml], [R_sml], lambda e: e.tensor_scalar(out=sml[:, 2:3], in0=sml[:, 2:3], scalar1=1.0 / D, scalar2=EPS, op0=ALU.mult, op1=ALU.add))
                    A([R_sml], [R_sml], lambda e: e.activation(out=sml[:, 2:3], in_=sml[:, 2:3], func=AF.Ln))
                    A([R_sml], [R_sml], lambda e: e.activation(out=sml[:, 2:3], in_=sml[:, 2:3], func=AF.Exp, scale=-0.5))
                    for n in range(2):
                        pyb, Rpy = py[b * 2 + n], R_py[b * 2 + n]
                        cs = slice(n * 512, (n + 1) * 512)
                        V([Rpy, R_sml, R_gm], [R_tmpf], lambda e: e.scalar_tensor_tensor(out=tmpf[:, cs], in0=pyb[:], scalar=sml[:, 2:3], in1=GM[:, cs], op0=ALU.mult, op1=ALU.mult))
                    V([R_tmpf, R_xt[b]], [R_xo[b]], lambda e: e.tensor_tensor(out=xo[b][:], in0=tmpf[:], in1=xt[b][:], op=ALU.add))
                    cx.dma("gpsimd", x1_d[tg, :], xo[b][:], [R_xo[b]], [R_x1[sq][i]])

                items4 = [(sq_, i_) for sq_ in range(NSEQ) for i_ in range(NT)]
                f4a_front(items4[0][0], items4[0][1], 0)
                for k_, (sq_, i_) in enumerate(items4):
                    if k_ + 1 < len(items4):
                        f4a_front(items4[k_ + 1][0], items4[k_ + 1][1], (k_ + 1) % 2)
                    if k_ % 2 == 0 and k_ // 2 < 16:
                        load_wgu(k_ // 2)
                    f4a_back(sq_, i_, k_ % 2)
            cx.barrier()

            with ExitStack() as st:
                R_w = R_wg + R_wd
                MODB = sbuf(st, "MODB", [128, 3, D], F32)
                R_modb = Region()
                x_st = [sbuf(st, "x5_%d" % i, [128, 2, D], F32) for i in range(2)]
                R_xst = [Region() for _ in range(2)]
                tmpf = sbuf(st, "tmp5", [128, D], F32)
                R_tmpf = Region()
                junk = tmpf
                R_junk = R_tmpf
                tmpF = sbuf(st, "tmpF5", [128, D], F32)
                R_tmpF = Region()
                junkF = tmpF
                R_junkF = R_tmpF
                smlF = sbuf(st, "smlF5", [128, 4], F32)
                R_smlF = Region()
                hb = sbuf(st, "hb5", [128, 2, D], BF16)
                R_hb = [Region() for _ in range(2)]
                hTs = [sbuf(st, "hT5_%d" % i, [128, 8, 256], BF16) for i in range(2)]
                R_hTs = [Region() for _ in range(2)]
                gua = sbuf(st, "gua", [128, NFC, 256], BF16)
                R_gua = Region()
                sg = [sbuf(st, "sg0", [128, 256], F32)] * 2
                R_sg = [Region()] * 2
                xo = [sbuf(st, "xo5_0", [128, D], F32)] * 2
                R_xo = [Region()] * 2
                gq = xo[0]
                R_gq = R_xo[0]
                sml = sbuf(st, "sml5", [128, 8], F32)
                R_sml = Region()
                pg = [psum(st, "pg%d" % i, [128, 512], F32) for i in range(2)]
                R_pg = [Region() for _ in range(2)]
                pyb_ = [psum(st, "py5_%d" % i, [128, 512], F32) for i in range(4)]
                R_pyb = [Region() for _ in range(4)]
                ptb = [psum(st, "pt5_%d" % i, [128, 1024], BF16) for i in range(2)]
                R_pt = [Region() for _ in range(2)]
                pgi = [0]
                pti = [0]
                xoi = [0]

                def load_modb(sq):
                    cx.dma("sync", MODB[:, 0, :], modd[l, sq:sq + 1, 3 * D:4 * D].partition_broadcast(128), [R_modd], [R_modb])
                    cx.dma("sync", MODB[:, 1, :], modd[l, sq:sq + 1, 4 * D:5 * D].partition_broadcast(128), [R_modd], [R_modb])
                    cx.dma("sync", MODB[:, 2, :], modd[l, sq:sq + 1, 5 * D:6 * D].partition_broadcast(128), [R_modd], [R_modb])
                    cx.dma("sync", gq[:], fpre_d[l:l + 1, :].partition_broadcast(128), [], [R_gq])
                    V([R_modb, R_gq], [R_modb], lambda e: e.scalar_tensor_tensor(out=MODB[:, 1, :], in0=MODB[:, 1, :], scalar=1.0, in1=gq[:], op0=ALU.add, op1=ALU.mult))
                    cx.dma("sync", gq[:], fpost_d[l:l + 1, :].partition_broadcast(128), [], [R_gq])
                    V([R_modb, R_gq], [R_modb], lambda e: e.tensor_tensor(out=MODB[:, 2, :], in0=MODB[:, 2, :], in1=gq[:], op=ALU.mult))

                def frontA(sq, ti, kb):
                    T0 = sq * S + ti * 256
                    xs = x_st[kb]
                    cx.dma("sync", xs[:], x1_d[T0:T0 + 256, :].rearrange("(j p) d -> p j d", p=128), [R_x1[sq][ti * 2], R_x1[sq][ti * 2 + 1]], [R_xst[kb]])
                    V([], [R_smlF], lambda e: e.memset(smlF[:, 0:2], 0.0))
                    for j in range(2):
                        A([R_xst[kb], R_smlF], [R_junkF, R_smlF], lambda e: e.activation(out=junkF[:], in_=xs[:, j, :], func=AF.Square, accum_out=smlF[:, j:j + 1]))
                    V([R_smlF], [R_smlF], lambda e: e.tensor_scalar(out=smlF[:, 0:2], in0=smlF[:, 0:2], scalar1=1.0 / D, scalar2=EPS, op0=ALU.mult, op1=ALU.add))
                    A([R_smlF], [R_smlF], lambda e: e.activation(out=smlF[:, 0:2], in_=smlF[:, 0:2], func=AF.Ln))
                    A([R_smlF], [R_smlF], lambda e: e.activation(out=smlF[:, 0:2], in_=smlF[:, 0:2], func=AF.Exp, scale=-0.5))
                    for j in range(2):
                        V([R_xst[kb], R_smlF, R_modb], [R_tmpF], lambda e: e.scalar_tensor_tensor(out=tmpF[:], in0=xs[:, j, :], scalar=smlF[:, j:j + 1], in1=MODB[:, 1, :], op0=ALU.mult, op1=ALU.mult))
                        V([R_tmpF, R_modb], [R_hb[j]], lambda e: e.tensor_tensor(out=hb[:, j, :], in0=tmpF[:], in1=MODB[:, 0, :], op=ALU.add))

                def frontB(kb):
                    for j in range(2):
                        pt, Rp = ptb[pti[0] % 2], R_pt[pti[0] % 2]
                        pti[0] += 1
                        for kc in range(8):
                            T([R_hb[j]] + RC, [Rp], lambda e: e.transpose(out=pt[:, kc * 128:(kc + 1) * 128], in_=hb[:, j, kc * 128:(kc + 1) * 128], identity=identb[:]))
                        A([Rp], [R_hTs[kb]], lambda e: e.activation(out=hTs[kb][:, :, j * 128:(j + 1) * 128], in_=pt[:].rearrange("p (k t) -> p k t", t=128), func=AF.Copy))

                def gate_up(kb):
                    hT, R_hT = hTs[kb], R_hTs[kb]
                    for fc in range(NFC):
                        pgb, Rpg = pg[pgi[0] % 2], R_pg[pgi[0] % 2]
                        pgi[0] += 1
                        for kc in range(8):
                            T([R_hT, *R_w], [Rpg], lambda e: e.matmul(pgb[:, 0:256], lhsT=w_gu_sb[:, kc, fc * 128:(fc + 1) * 128], rhs=hT[:, kc, :], start=(kc == 0), stop=(kc == 7)))
                        for kc in range(8):
                            T([R_hT, *R_w], [Rpg], lambda e: e.matmul(pgb[:, 256:512], lhsT=w_gu_sb[:, kc, FH + fc * 128:FH + (fc + 1) * 128], rhs=hT[:, kc, :], start=(kc == 0), stop=(kc == 7)))
                        sgb, Rsg = sg[fc % 2], R_sg[fc % 2]
                        A([Rpg], [Rsg], lambda e: e.activation(out=sgb[:], in_=pgb[:, 0:256], func=AF.Silu))
                        V([Rsg, Rpg], [R_gua], lambda e: e.tensor_tensor(out=gua[:, fc, :], in0=sgb[:], in1=pgb[:, 256:512], op=ALU.mult))

                def down_tail(sq, ti, kb):
                    T0 = sq * S + ti * 256
                    xs = x_st[kb]
                    for j in range(2):
                        V([], [R_sml], lambda e: e.memset(sml[:, 4:6], 0.0))
                        for n in range(2):
                            pyq, Rpyq = pyb_[j * 2 + n], R_pyb[j * 2 + n]
                            for fc in range(NFC):
                                T([R_gua, *R_w], [Rpyq], lambda e: e.matmul(pyq[:], lhsT=gua[:, fc, j * 128:(j + 1) * 128], rhs=w_dn_sb[:, fc, n * 512:(n + 1) * 512], start=(fc == 0), stop=(fc == NFC - 1)))
                            A([Rpyq, R_sml], [R_junk, R_sml], lambda e: e.activation(out=junk[:, 0:512], in_=pyq[:], func=AF.Square, accum_out=sml[:, 4 + n:5 + n]))
                        V([R_sml], [R_sml], lambda e: e.tensor_tensor(out=sml[:, 6:7], in0=sml[:, 4:5], in1=sml[:, 5:6], op=ALU.add))
                        V([R_sml], [R_sml], lambda e: e.tensor_scalar(out=sml[:, 6:7], in0=sml[:, 6:7], scalar1=1.0 / D, scalar2=EPS, op0=ALU.mult, op1=ALU.add))
                        A([R_sml], [R_sml], lambda e: e.activation(out=sml[:, 6:7], in_=sml[:, 6:7], func=AF.Ln))
                        A([R_sml], [R_sml], lambda e: e.activation(out=sml[:, 6:7], in_=sml[:, 6:7], func=AF.Exp, scale=-0.5))
                        for n in range(2):
                            pyq, Rpyq = pyb_[j * 2 + n], R_pyb[j * 2 + n]
                            cs = slice(n * 512, (n + 1) * 512)
                            V([Rpyq, R_sml, R_modb], [R_tmpf], lambda e: e.scalar_tensor_tensor(out=tmpf[:, cs], in0=pyq[:], scalar=sml[:, 6:7], in1=MODB[:, 2, cs], op0=ALU.mult, op1=ALU.mult))
                        ob = xoi[0] % 2
                        xoi[0] += 1
                        V([R_tmpf, R_xst[kb]], [R_xo[ob]], lambda e: e.tensor_tensor(out=xo[ob][:], in0=tmpf[:], in1=xs[:, j, :], op=ALU.add))
                        tg = slice(T0 + j * 128, T0 + (j + 1) * 128)
                        cx.dma("gpsimd", xout_d[tg, :], xo[ob][:], [R_xo[ob]], [R_xres[sq][ti * 2 + j]])

                items5 = [(sq_, ti_) for sq_ in range(NSEQ) for ti_ in range(8)]
                load_modb(0)
                frontA(0, 0, 0)
                frontB(0)
                for k_, (sq_, ti_) in enumerate(items5):
                    kb = k_ % 2
                    nxt = items5[k_ + 1] if k_ + 1 < len(items5) else None
                    if nxt is not None and nxt[0] == sq_:
                        frontA(nxt[0], nxt[1], 1 - kb)
                    gate_up(kb)
                    if nxt is not None and nxt[0] == sq_:
                        frontB(1 - kb)
                    down_tail(sq_, ti_, kb)
                    if nxt is not None and nxt[0] != sq_:
                        load_modb(nxt[0])
                        frontA(nxt[0], nxt[1], 1 - kb)
                        frontB(1 - kb)
            cx.barrier()
            st_w.close()
        cx.finish()
        stuck = cx.check_deadlock()
        if stuck:
            raise RuntimeError("static deadlock check failed: %r" % (stuck,))
    return nc


def _consts():
    k = np.arange(128)
    ident = np.eye(128, dtype=np.float32)
    utri = (k[:, None] <= k[None, :]).astype(np.float32)
    negtri = np.where(k[None, :] <= k[:, None], 0.0, NEG).astype(np.float32)
    negm = np.where(k[:, None] <= k[None, :], 0.0, -30000.0).astype(np.float32)
    invf = (10000.0 ** (-np.arange(0, 64, 2, dtype=np.float32) / 64)).astype(np.float32)[None, :]
    pw2 = np.ascontiguousarray(np.broadcast_to((2.0 ** -(np.arange(32, dtype=np.float32) + 1.0)).astype(np.float32)[None, :], (128, 32)))
    return dict(c_ident=ident, c_utri=utri, c_negtri=negtri, c_negm=negm, c_invf=invf, c_pw2=pw2)


def make_in_maps(inputs, n_cores=8):
    f32 = lambda a: np.ascontiguousarray(np.asarray(a, dtype=np.float32))
    shared = {}
    for k in ("w_mod", "b_mod", "mix_norm_pre", "mix_norm_post", "w_in", "q_latent_norm", "w_q_up", "w_qidx_up",
              "b_igate", "b_fgate", "attn_out_norm", "mlstm_out_norm", "w_out", "ffn_norm_pre", "ffn_norm_post",
              "w_gate_up", "w_down"):
        shared[k] = f32(inputs[k])
    cw = f32(inputs["conv_w"])
    shared["convw"] = np.ascontiguousarray(cw.reshape(2, 4, 8, 128).transpose(0, 3, 2, 1))
    shared["convb"] = np.ascontiguousarray(f32(inputs["conv_b"]).reshape(2, 8, 128).transpose(0, 2, 1))
    shared.update(_consts())
    x = f32(inputs["x"])
    c = f32(inputs["c"])
    pos = np.asarray(inputs["positions"]).astype(np.int32)
    maps = []
    for i in range(n_cores):
        m = dict(shared)
        m["x"] = np.ascontiguousarray(x[2 * i:2 * i + 2].reshape(NSEQ * S, D))
        m["cT"] = np.ascontiguousarray(c[2 * i:2 * i + 2].reshape(NSEQ, 8, 128).transpose(2, 1, 0))
        m["pos"] = np.ascontiguousarray(pos[2 * i:2 * i + 2].reshape(NSEQ, NT, 128).transpose(2, 0, 1))
        maps.append(m)
    return maps


_NC_CACHE = {}


def kernel(**inputs):
    if "nc" not in _NC_CACHE:
        _NC_CACHE["nc"] = build_nc()
    nc = _NC_CACHE["nc"]
    maps = make_in_maps(inputs)
    res = run_bass_kernel_spmd(nc, maps, core_ids=list(range(8)))
    outs = [np.asarray(r["out"]).reshape(NSEQ, S, D) for r in res.results]
    return np.concatenate(outs, axis=0).astype(np.float32)
```

```python
from contextlib import ExitStack
import math
import numpy as np
import concourse.bass as bass
import concourse.mybir as mybir
from concourse.bass_utils import run_bass_kernel_spmd

F32 = mybir.dt.float32
BF16 = mybir.dt.bfloat16
I32 = mybir.dt.int32
AF = mybir.ActivationFunctionType
ALU = mybir.AluOpType
AX = mybir.AxisListType

D = 1024
S = 2048
NT = 16
NSEQ = 2
FH = 2816
NFC = 22
INC = 3408
EPS = 1e-6
NEG = -1.0e30
TOPK = 256


class Region:
    __slots__ = ("w", "r")

    def __init__(self):
        self.w = None
        self.r = {}


class EngState:
    def __init__(self, name, eng, sem):
        self.name = name
        self.eng = eng
        self.sem = sem
        self.count = 0
        self.waited = {}


class Ctx:
    def __init__(self, nc, stack):
        self.nc = nc
        self.engs = {}
        for name in ("tensor", "vector", "scalar", "gpsimd", "sync"):
            sem = stack.enter_context(nc.semaphore("s_" + name))
            self.engs[name] = EngState(name, getattr(nc, name), sem)
        self.dma_pools = {}
        for q, n in (("sync", 24), ("gpsimd", 16), ("scalar", 4)):
            pool = []
            for i in range(n):
                sem = stack.enter_context(nc.semaphore("s_dma_%s%d" % (q, i)))
                pool.append([sem, 0])
            self.dma_pools[q] = [pool, 0]
        self.n_inst = 0
        self.trace = {n: [] for n in self.engs}

    def _wait(self, es, ev):
        sem, val = ev
        k = id(sem)
        if es.waited.get(k, 0) >= val:
            return
        es.eng.wait_ge(sem, val)
        es.waited[k] = val
        self.trace[es.name].append(("w", k, val))

    def check_deadlock(self):
        vals = {}
        pos = {n: 0 for n in self.trace}
        progress = True
        while progress:
            progress = False
            for n, tr in self.trace.items():
                while pos[n] < len(tr):
                    kind, k, v = tr[pos[n]]
                    if kind == "w":
                        if vals.get(k, 0) < v:
                            break
                    else:
                        vals[k] = vals.get(k, 0) + v
                    pos[n] += 1
                    progress = True
        stuck = {n: (pos[n], len(tr)) for n, tr in self.trace.items() if pos[n] < len(tr)}
        return stuck

    def _deps(self, es, reads, writes, skip_same=False):
        best = {}

        def add(ev):
            if ev is None:
                return
            k = id(ev[0])
            if k not in best or best[k][1] < ev[1]:
                best[k] = ev
        for r in reads:
            add(r.w)
        for w in writes:
            add(w.w)
            for ev in w.r.values():
                add(ev)
        for ev in best.values():
            if skip_same and ev[0] is es.sem:
                continue
            self._wait(es, ev)

    def _commit(self, ev, reads, writes):
        k = id(ev[0])
        for r in reads:
            r.r[k] = ev
        for w in writes:
            w.w = ev
            w.r = {}

    def op(self, engname, reads, writes, fn):
        es = self.engs[engname]
        self._deps(es, reads, writes, skip_same=(engname == "tensor"))
        inst = fn(es.eng)
        es.count += 1
        inst.then_inc(es.sem, 1)
        self.trace[engname].append(("i", id(es.sem), 1))
        self._commit((es.sem, es.count), reads, writes)
        self.n_inst += 1

    def dma(self, qname, out, in_, reads, writes, **kw):
        es = self.engs[qname]
        pr = self.dma_pools[qname]
        slot = pr[0][pr[1]]
        pr[1] = (pr[1] + 1) % len(pr[0])
        sem, tot = slot
        if tot > 0:
            self._wait(es, (sem, tot))
        self._deps(es, reads, writes)
        es.eng.dma_start(out=out, in_=in_, **kw).then_inc(sem, 16)
        self.trace[qname].append(("i", id(sem), 16))
        slot[1] = tot + 16
        self._commit((sem, tot + 16), reads, writes)
        self.n_inst += 1

    def barrier(self):
        snap = [(e.sem, e.count) for e in self.engs.values() if e.count > 0]
        for pr in self.dma_pools.values():
            snap += [(s_, t_) for s_, t_ in pr[0] if t_ > 0]
        for name in ("sync", "gpsimd", "tensor", "vector", "scalar"):
            es = self.engs[name]
            for ev in snap:
                if ev[0] is es.sem:
                    continue
                self._wait(es, ev)

    def finish(self):
        self.barrier()


def bc_mid(ap, n):
    p, f = ap.shape
    return ap.unsqueeze(1).to_broadcast([p, n, f])


def bc_last(ap, n):
    p, a = ap.shape
    return ap.unsqueeze(2).to_broadcast([p, a, n])


def build_nc(n_layers=2, dbg=False):
    nc = bass.Bass("TRN2", target_bir_lowering=False)

    def din(name, shape, dt=F32):
        return nc.dram_tensor(name, shape, dt, kind="ExternalInput").ap()

    def dscr(name, shape, dt):
        return nc.dram_tensor(name, shape, dt, kind=("ExternalOutput" if dbg else "Internal")).ap()

    x_d = din("x", [NSEQ * S, D])
    cT_d = din("cT", [128, 8, NSEQ])
    pos_d = din("pos", [128, NSEQ, NT], I32)
    w_mod_d = din("w_mod", [2, D, 6 * D])
    b_mod_d = din("b_mod", [2, 6 * D])
    npre_d = din("mix_norm_pre", [2, D])
    npost_d = din("mix_norm_post", [2, D])
    w_in_d = din("w_in", [2, D, INC])
    qln_d = din("q_latent_norm", [2, 256])
    wq_d = din("w_q_up", [2, 256, 512])
    wqi_d = din("w_qidx_up", [2, 256, 512])
    convw_d = din("convw", [2, 128, 8, 4])
    convb_d = din("convb", [2, 128, 8])
    big_d = din("b_igate", [2, 4])
    bfg_d = din("b_fgate", [2, 4])
    aon_d = din("attn_out_norm", [2, 512])
    mon_d = din("mlstm_out_norm", [2, 512])
    w_out_d = din("w_out", [2, D, D])
    fpre_d = din("ffn_norm_pre", [2, D])
    fpost_d = din("ffn_norm_post", [2, D])
    w_gu_d = din("w_gate_up", [2, D, 2 * FH])
    w_dn_d = din("w_down", [2, FH, D])
    ident_d = din("c_ident", [128, 128])
    utri_d = din("c_utri", [128, 128])
    negtri_d = din("c_negtri", [128, 128])
    negm_d = din("c_negm", [128, 128])
    invf_d = din("c_invf", [1, 32])
    pw2_d = din("c_pw2", [128, 32])

    out_d = nc.dram_tensor("out", [NSEQ * S, D], F32, kind="ExternalOutput").ap()

    xres_d = dscr("xres", [NSEQ * S, D], F32)
    x1_d = dscr("x1s", [NSEQ * S, D], F32)
    modd = dscr("modd", [2, NSEQ, 6 * D], F32)
    qT_d = dscr("qT", [NSEQ, 128, 4, S], BF16)
    qiT_d = dscr("qiT", [NSEQ, 128, 4, S], BF16)
    kT_d = dscr("kT", [NSEQ, 128, 4, S], BF16)
    kiT_d = dscr("kiT", [NSEQ, 64, S], BF16)
    V_d = dscr("Vs", [NSEQ, S, 512], BF16)
    wi_d = dscr("wi", [NSEQ, S, 8], F32)
    qmT_d = dscr("qmT", [NSEQ, 128, 8, S], BF16)
    Vm_d = dscr("Vm", [NSEQ, S, 512], BF16)
    og_d = dscr("og", [NSEQ, S, 512], BF16)
    ig_d = dscr("ig", [NSEQ, S, 4], F32)
    lf_d = dscr("lf", [NSEQ, S, 4], F32)
    att_d = dscr("att", [NSEQ, S, 512], BF16)
    hm_d = dscr("hm", [NSEQ, S, 512], BF16)

    R_xres = [[Region() for _ in range(NT)] for _ in range(NSEQ)]
    R_x1 = [[Region() for _ in range(NT)] for _ in range(NSEQ)]
    R_modd = Region()
    R_p1 = [[] for _ in range(NSEQ)]
    R_att = [[Region() for _ in range(NT)] for _ in range(NSEQ)]
    R_hm = [[Region() for _ in range(NT)] for _ in range(NSEQ)]

    with ExitStack() as st0:
        cx = Ctx(nc, st0)

        def V(r, w, fn):
            cx.op("vector", r, w, fn)

        def A(r, w, fn):
            cx.op("scalar", r, w, fn)

        def G(r, w, fn):
            cx.op("gpsimd", r, w, fn)

        def T(r, w, fn):
            cx.op("tensor", r, w, fn)

        uniq = [0]

        def sbuf(stk, name, shape, dt):
            uniq[0] += 1
            return stk.enter_context(nc.sbuf_tensor("%s_%d" % (name, uniq[0]), shape, dt))

        def psum(stk, name, shape, dt):
            uniq[0] += 1
            return stk.enter_context(nc.psum_tensor("%s_%d" % (name, uniq[0]), shape, dt))

        identf = sbuf(st0, "identf", [128, 128], F32)
        identb = sbuf(st0, "identb", [128, 128], BF16)
        utri = sbuf(st0, "utri", [128, 128], F32)
        negtri = sbuf(st0, "negtri", [128, 128], F32)
        negm = sbuf(st0, "negm", [128, 128], F32)
        onesf = sbuf(st0, "onesf", [128, 128], F32)
        invf = sbuf(st0, "invf", [128, 32], F32)
        posi = sbuf(st0, "posi", [128, NSEQ, NT], I32)
        posf = sbuf(st0, "posf", [128, NSEQ * NT], F32)
        COS = sbuf(st0, "COS", [128, NSEQ * NT, 32], F32)
        SIN = sbuf(st0, "SIN", [128, NSEQ * NT, 32], F32)
        cact = sbuf(st0, "cact", [128, 8, NSEQ], F32)
        thr0 = sbuf(st0, "thr0", [128, 1], F32)
        pw2 = sbuf(st0, "pw2", [128, 32], F32)
        R_c = Region()
        cx.dma("sync", identf[:], ident_d, [], [R_c])
        cx.dma("sync", utri[:], utri_d, [], [R_c])
        cx.dma("sync", negtri[:], negtri_d, [], [R_c])
        cx.dma("sync", negm[:], negm_d, [], [R_c])
        cx.dma("sync", invf[:], invf_d.partition_broadcast(128), [], [R_c])
        cx.dma("sync", posi[:], pos_d, [], [R_c])
        cx.dma("sync", pw2[:], pw2_d, [], [R_c])
        cx.dma("sync", cact[:], cT_d, [], [R_c])
        R_c2 = Region()
        R_c4, R_c5, R_c6 = Region(), Region(), Region()
        V([R_c], [R_c4], lambda e: e.tensor_copy(out=identb[:], in_=identf[:]))
        V([], [R_c5], lambda e: e.memset(onesf[:], 1.0))
        V([], [R_c6], lambda e: e.memset(thr0[:], -1.0e29))
        V([R_c], [R_c2], lambda e: e.tensor_copy(out=posf[:], in_=posi[:].rearrange("p a b -> p (a b)")))
        R_cs = Region()
        R_c3 = Region()
        A([R_c], [R_c3], lambda e: e.activation(out=cact[:], in_=cact[:], func=AF.Silu))
        RC = [R_c, R_c2, R_c3, R_c4, R_c5, R_c6, R_cs]

        with ExitStack() as st:
            R_ang = Region()
            ANG = sbuf(st, "ANG", [128, NSEQ * NT, 32], F32)
            ang2 = sbuf(st, "ang2", [128, NSEQ * NT, 32], F32)
            angk = sbuf(st, "angk", [128, NSEQ * NT, 32], F32)
            angi = sbuf(st, "angi", [128, NSEQ * NT, 32], I32)
            V([R_c, R_c2], [R_ang], lambda e: e.tensor_tensor(out=ANG[:], in0=bc_last(posf[:], 32), in1=bc_mid(invf[:], NSEQ * NT), op=ALU.mult))
            C1 = 6.28125
            C2 = 2.0 * math.pi - 6.28125
            for dst, off in ((SIN, 0.0), (COS, 0.5 * math.pi)):
                V([R_ang], [R_ang], lambda e: e.tensor_scalar(out=ang2[:], in0=ANG[:], scalar1=off, scalar2=None, op0=ALU.add))
                V([R_ang], [R_ang], lambda e: e.tensor_scalar(out=angk[:], in0=ang2[:], scalar1=1.0 / (2.0 * math.pi), scalar2=None, op0=ALU.mult))
                V([R_ang], [R_ang], lambda e: e.tensor_copy(out=angi[:], in_=angk[:]))
                V([R_ang], [R_ang], lambda e: e.tensor_copy(out=angk[:], in_=angi[:]))
                V([R_ang], [R_ang], lambda e: e.scalar_tensor_tensor(out=ang2[:], in0=angk[:], scalar=-C1, in1=ang2[:], op0=ALU.mult, op1=ALU.add))
                V([R_ang], [R_ang], lambda e: e.scalar_tensor_tensor(out=ang2[:], in0=angk[:], scalar=-C2, in1=ang2[:], op0=ALU.mult, op1=ALU.add))
                V([R_ang], [R_ang], lambda e: e.tensor_scalar(out=angk[:], in0=ang2[:], scalar1=math.pi, scalar2=-2.0 * math.pi, op0=ALU.is_gt, op1=ALU.mult))
                V([R_ang], [R_ang], lambda e: e.tensor_tensor(out=ang2[:], in0=ang2[:], in1=angk[:], op=ALU.add))
                V([R_ang], [R_ang], lambda e: e.tensor_scalar(out=angk[:], in0=ang2[:], scalar1=-math.pi, scalar2=2.0 * math.pi, op0=ALU.is_lt, op1=ALU.mult))
                V([R_ang], [R_ang], lambda e: e.tensor_tensor(out=ang2[:], in0=ang2[:], in1=angk[:], op=ALU.add))
                V([R_ang], [R_ang], lambda e: e.tensor_scalar(out=ang2[:], in0=ang2[:], scalar1=-3.1415925, scalar2=3.1415925, op0=ALU.max, op1=ALU.min))
                A([R_ang], [R_cs, R_ang], lambda e: e.activation(out=dst[:], in_=ang2[:], func=AF.Sin))
            wst = [sbuf(st, "wmst%d" % i, [128, 3072], BF16) for i in range(3)]
            cactb = sbuf(st, "cactb", [128, 8, NSEQ], BF16)
            R_cb = Region()
            V(RC, [R_cb], lambda e: e.tensor_copy(out=cactb[:], in_=cact[:]))
            R_wst = [Region() for _ in range(3)]
            bmod = sbuf(st, "bmod", [NSEQ, 6 * D], F32)
            modsb = sbuf(st, "modsb", [NSEQ, 6 * D], F32)
            R_bm, R_ms = Region(), Region()
            pm = [psum(st, "pm%d" % i, [128, 512], F32) for i in range(6)]
            R_pm = [Region() for _ in range(6)]
            cnt = 0
            for l in range(n_layers):
                cx.dma("sync", bmod[:], b_mod_d[l:l + 1, :].partition_broadcast(NSEQ), [], [R_bm])
                for half in range(2):
                    for kc in range(8):
                        b = cnt % 3
                        cnt += 1
                        cx.dma("gpsimd", wst[b][:], w_mod_d[l, kc * 128:(kc + 1) * 128, half * 3072:(half + 1) * 3072], [], [R_wst[b]])
                        for n in range(6):
                            T([R_wst[b], R_cb], [R_pm[n]], lambda e: e.matmul(pm[n][0:NSEQ, :], lhsT=cactb[:, kc, :], rhs=wst[b][:, n * 512:(n + 1) * 512], start=(kc == 0), stop=(kc == 7)))
                    for n in range(6):
                        c0 = half * 3072 + n * 512
                        V([R_pm[n], R_bm], [R_ms], lambda e: e.tensor_tensor(out=modsb[:, c0:c0 + 512], in0=pm[n][0:NSEQ, :], in1=bmod[:, c0:c0 + 512], op=ALU.add))
                cx.dma("sync", modd[l], modsb[:], [R_ms], [R_modd])
        cx.barrier()

        for l in range(n_layers):
            xin_d = x_d if l == 0 else xres_d
            xout_d = xres_d if l < n_layers - 1 else out_d
            R_xin = None if l == 0 else R_xres

            with ExitStack() as st:
                w_in_sb = sbuf(st, "w_in_sb", [128, 8, INC], BF16)
                wq_sb = sbuf(st, "wq_sb", [128, 2, 1024], BF16)
                R_w = [Region() for _ in range(40)]
                for kc in range(8):
                    cx.dma("gpsimd", w_in_sb[:, kc, :], w_in_d[l, kc * 128:(kc + 1) * 128, :], [], [R_w[kc]])
                for kc in range(2):
                    cx.dma("gpsimd", wq_sb[:, kc, 0:512], wq_d[l, kc * 128:(kc + 1) * 128, :], [], [R_w[8 + kc]])
                    cx.dma("gpsimd", wq_sb[:, kc, 512:1024], wqi_d[l, kc * 128:(kc + 1) * 128, :], [], [R_w[10 + kc]])
                gpre = sbuf(st, "gpre", [128, D], F32)
                qln = sbuf(st, "qln", [128, 256], F32)
                cw = sbuf(st, "cw", [128, 8, 4], F32)
                cb = sbuf(st, "cb", [128, 8], F32)
                bigt = sbuf(st, "bigt", [128, 4], F32)
                bfgt = sbuf(st, "bfgt", [128, 4], F32)
                R_sp = Region()
                cx.dma("sync", gpre[:], npre_d[l:l + 1, :].partition_broadcast(128), [], [R_sp])
                cx.dma("sync", qln[:], qln_d[l:l + 1, :].partition_broadcast(128), [], [R_sp])
                cx.dma("sync", cw[:], convw_d[l], [], [R_sp])
                cx.dma("sync", cb[:], convb_d[l], [], [R_sp])
                cx.dma("sync", bigt[:], big_d[l:l + 1, :].partition_broadcast(128), [], [R_sp])
                cx.dma("sync", bfgt[:], bfg_d[l:l + 1, :].partition_broadcast(128), [], [R_sp])

                MODA = sbuf(st, "MODA", [128, 2, D], F32)
                R_moda = Region()
                x_st = [sbuf(st, "x_st0", [128, 4, D], F32)] * 2
                R_xst = [Region()] * 2
                junk = sbuf(st, "junk", [128, D], BF16)
                R_junk = Region()
                ss = sbuf(st, "ss", [128, 8], F32)
                R_ss = Region()
                tmpf = sbuf(st, "tmpf", [128, D], F32)
                R_tmpf = Region()
                hb = sbuf(st, "hb", [128, 4, D], BF16)
                R_hb = [Region() for _ in range(4)]
                hTs = [sbuf(st, "hT%d" % i, [128, 8, 512], BF16) for i in range(2)]
                R_hTs = [Region() for _ in range(2)]
                smlF = sbuf(st, "smlF", [128, 8], F32)
                R_smlF = Region()
                junk2 = sbuf(st, "junk2", [128, 256], BF16)
                R_junk2 = Region()
                qTs = sbuf(st, "qTs", [128, 4, 512], BF16)
                qiTs = sbuf(st, "qiTs", [128, 4, 512], BF16)
                kTs = sbuf(st, "kTs", [128, 4, 512], BF16)
                kiTs = sbuf(st, "kiTs", [64, 512], BF16)
                Vs = sbuf(st, "Vs_sb", [128, 4, 512], BF16)
                Vms = sbuf(st, "Vms", [128, 4, 512], BF16)
                ogs = sbuf(st, "ogs", [128, 4, 512], BF16)
                wis = sbuf(st, "wis", [128, 4, 8], F32)
                igs = sbuf(st, "igs", [128, 4, 4], F32)
                lfs = sbuf(st, "lfs", [128, 4, 4], F32)
                qmTs = sbuf(st, "qmTs", [128, 8, 512], BF16)
                R_out = {k: Region() for k in ("qT", "qiT", "kT", "kiT", "V", "Vm", "og", "wi", "ig", "lf", "qmT")}
                pre = sbuf(st, "pre", [128, 8, 515], F32)
                R_pre = Region()
                acc = sbuf(st, "acc", [128, 512], F32)
                R_acc = Region()
                cqn = [sbuf(st, "cqn%d" % i, [128, 256], BF16) for i in range(2)]
                R_cqn = [Region() for _ in range(2)]
                rOs = {k_: [sbuf(st, "rO_%s%d" % (k_, i), [128, 512 if k_ != "ki" else 64], BF16) for i in range(2)] for k_ in ("k", "q", "qi", "ki")}
                R_rOs = {k_: [Region() for _ in range(2)] for k_ in ("k", "q", "qi", "ki")}
                cqT = sbuf(st, "cqT", [128, 2, 128], BF16)
                R_cqT = Region()
                rAs = [sbuf(st, "rA%d" % i, [128, 512], F32) for i in range(2)]
                rBs = [sbuf(st, "rB%d" % i, [128, 512], F32) for i in range(2)]
                R_rAs = [Region() for _ in range(2)]
                R_rBs = [Region() for _ in range(2)]
                ropei = [0]
                sml = sbuf(st, "sml", [128, 16], F32)
                R_sml = Region()
                pf = [psum(st, "pf%d" % i, [128, 512], F32) for i in range(6)]
                R_pf = [Region() for _ in range(6)]
                ptb = [psum(st, "ptb%d" % i, [128, 1024], BF16) for i in range(2)]
                R_pt = [Region() for _ in range(2)]
                pfi = [0]
                pti = [0]

                def next_pf():
                    i = pfi[0] % 6
                    pfi[0] += 1
                    return pf[i], R_pf[i]

                def next_pt():
                    i = pti[0] % 2
                    pti[0] += 1
                    return ptb[i], R_pt[i]

                def rope(src, R_src, ncols, tix, dst, R_dst_w):
                    nh = ncols // 64
                    rb_ = ropei[0] % 2
                    ropei[0] += 1
                    rA, rB, R_rA, R_rB = rAs[rb_], rBs[rb_], R_rAs[rb_], R_rBs[rb_]
                    cos = COS[:, tix, :]
                    sin = SIN[:, tix, :]
                    s3 = src.rearrange("p (g f) -> p g f", f=32)
                    a3 = rA[:, 0:ncols].rearrange("p (g f) -> p g f", f=32)
                    V([R_src] + RC, [R_rA], lambda e: e.tensor_tensor(out=a3, in0=s3, in1=bc_mid(cos, 2 * nh), op=ALU.mult))
                    s4 = src.rearrange("p (h t f) -> p h t f", t=2, f=32)
                    b4 = rB[:, 0:ncols].rearrange("p (h t f) -> p h t f", t=2, f=32)
                    a4 = rA[:, 0:ncols].rearrange("p (h t f) -> p h t f", t=2, f=32)
                    d4 = dst.rearrange("p (h t f) -> p h t f", t=2, f=32)
                    V([R_src] + RC, [R_rB], lambda e: e.tensor_tensor(out=b4[:, :, 0, :], in0=s4[:, :, 1, :], in1=bc_mid(sin, nh), op=ALU.mult))
                    V([R_src] + RC, [R_rB], lambda e: e.tensor_tensor(out=b4[:, :, 1, :], in0=s4[:, :, 0, :], in1=bc_mid(sin, nh), op=ALU.mult))
                    G([R_rA, R_rB], R_dst_w, lambda e: e.tensor_tensor(out=d4[:, :, 0, :], in0=a4[:, :, 0, :], in1=b4[:, :, 0, :], op=ALU.subtract))
                    G([R_rA, R_rB], R_dst_w, lambda e: e.tensor_tensor(out=d4[:, :, 1, :], in0=a4[:, :, 1, :], in1=b4[:, :, 1, :], op=ALU.add))

                def rstd_from_ss(col0, n, inv_n, tl=None, Rt=None):
                    tl = sml if tl is None else tl
                    Rt = R_sml if Rt is None else Rt
                    sl = tl[:, col0:col0 + n]
                    V([Rt], [Rt], lambda e: e.tensor_scalar(out=sl, in0=sl, scalar1=inv_n, scalar2=EPS, op0=ALU.mult, op1=ALU.add))
                    A([Rt], [Rt], lambda e: e.activation(out=sl, in_=sl, func=AF.Ln))
                    A([Rt], [Rt], lambda e: e.activation(out=sl, in_=sl, func=AF.Exp, scale=-0.5))

                xcnt = 0

                def p1_front(sq, sti, hT, R_hT):
                    if sti == 0:
                        cx.dma("sync", MODA[:, 0, :], modd[l, sq:sq + 1, 0:D].partition_broadcast(128), [R_modd], [R_moda])
                        cx.dma("sync", MODA[:, 1, :], modd[l, sq:sq + 1, D:2 * D].partition_broadcast(128), [R_modd], [R_moda])
                        V([R_moda, R_sp], [R_moda], lambda e: e.scalar_tensor_tensor(out=MODA[:, 1, :], in0=MODA[:, 1, :], scalar=1.0, in1=gpre[:], op0=ALU.add, op1=ALU.mult))
                    T0 = sq * S + sti * 512
                    xb = 0
                    xs = x_st[xb]
                    rd = [] if R_xin is None else [R_xin[sq][sti * 4 + j] for j in range(4)]
                    cx.dma("sync", xs[:], xin_d[T0:T0 + 512, :].rearrange("(j p) d -> p j d", p=128), rd, [R_xst[xb]])
                    V([], [R_smlF], lambda e: e.memset(smlF[:, 0:4], 0.0))
                    for j in range(4):
                        A([R_xst[xb], R_smlF], [R_junk, R_smlF], lambda e: e.activation(out=junk[:], in_=xs[:, j, :], func=AF.Square, accum_out=smlF[:, j:j + 1]))
                    rstd_from_ss(0, 4, 1.0 / D, smlF, R_smlF)
                    for j in range(4):
                        V([R_xst[xb], R_smlF, R_moda], [R_tmpf], lambda e: e.scalar_tensor_tensor(out=tmpf[:], in0=xs[:, j, :], scalar=smlF[:, j:j + 1], in1=MODA[:, 1, :], op0=ALU.mult, op1=ALU.mult))
                        G([R_tmpf, R_moda], [R_hb[j]], lambda e: e.tensor_tensor(out=hb[:, j, :], in0=tmpf[:], in1=MODA[:, 0, :], op=ALU.add))
                    for j in range(4):
                        pt, Rp = next_pt()
                        for kc in range(8):
                            T([R_hb[j]] + RC, [Rp], lambda e: e.transpose(out=pt[:, kc * 128:(kc + 1) * 128], in_=hb[:, j, kc * 128:(kc + 1) * 128], identity=identb[:]))
                        A([Rp], [R_hT], lambda e: e.activation(out=hT[:, :, j * 128:(j + 1) * 128], in_=pt[:].rearrange("p (k t) -> p k t", t=128), func=AF.Copy))


                def p1_back(sq, sti, hT, R_hT):
                    T0 = sq * S + sti * 512
                    if sti == 0:
                        V([], [R_pre], lambda e: e.memset(pre[:, :, 0:3], 0.0))
                    def proj(tsl, pb, Rpb, c0, c1, o0):
                        for kc in range(8):
                            T([R_hT, *R_w], [Rpb], lambda e: e.matmul(pb[:, o0:o0 + (c1 - c0)], lhsT=hT[:, kc, tsl], rhs=w_in_sb[:, kc, c0:c1], start=(kc == 0), stop=(kc == 7)))

                    def stage_P(j):
                        tix = sq * NT + sti * 4 + j
                        tsl = slice(j * 128, (j + 1) * 128)
                        jb = j % 2
                        pa, Rpa = next_pf()
                        proj(tsl, pa, Rpa, 0, 256, 0)
                        proj(tsl, pa, Rpa, 1280, 1352, 256)
                        proj(tsl, pa, Rpa, 3400, 3408, 328)
                        pk, Rpk = next_pf()
                        proj(tsl, pk, Rpk, 256, 768, 0)
                        V([], [R_sml], lambda e: e.memset(sml[:, 4:5], 0.0))
                        A([Rpa, R_sml], [R_junk2, R_sml], lambda e: e.activation(out=junk2[:], in_=pa[:, 0:256], func=AF.Square, accum_out=sml[:, 4:5]))
                        rstd_from_ss(4, 1, 1.0 / 256)
                        V([Rpa, R_sml, R_sp], [R_cqn[jb]], lambda e: e.scalar_tensor_tensor(out=cqn[jb][:], in0=pa[:, 0:256], scalar=sml[:, 4:5], in1=qln[:], op0=ALU.mult, op1=ALU.mult))
                        rope(pa[:, 256:320], Rpa, 64, tix, rOs["ki"][jb][:, 0:64], [R_rOs["ki"][jb]])
                        A([Rpa], [R_out["wi"]], lambda e: e.activation(out=wis[:, j, :], in_=pa[:, 320:328], func=AF.Identity, scale=512.0 ** -0.5))
                        V([Rpa, R_sp], [R_out["ig"]], lambda e: e.tensor_tensor(out=igs[:, j, :], in0=pa[:, 328:332], in1=bigt[:], op=ALU.add))
                        V([Rpa, R_sp], [R_sml], lambda e: e.tensor_tensor(out=sml[:, 8:12], in0=pa[:, 332:336], in1=bfgt[:], op=ALU.add))
                        A([R_sml], [R_sml], lambda e: e.activation(out=sml[:, 8:12], in_=sml[:, 8:12], func=AF.Exp, scale=-1.0))
                        A([R_sml], [R_sml], lambda e: e.activation(out=sml[:, 8:12], in_=sml[:, 8:12], func=AF.Ln, bias=1.0))
                        V([R_sml], [R_out["lf"]], lambda e: e.tensor_scalar(out=lfs[:, j, :], in0=sml[:, 8:12], scalar1=-1.0, scalar2=None, op0=ALU.mult))
                        rope(pk[:], Rpk, 512, tix, rOs["k"][jb][:], [R_rOs["k"][jb]])
                        pv, Rpv = next_pf()
                        proj(tsl, pv, Rpv, 768, 1280, 0)
                        A([Rpv], [R_out["V"]], lambda e: e.activation(out=Vs[:, j, :], in_=pv[:], func=AF.Copy))
                        pv, Rpv = next_pf()
                        proj(tsl, pv, Rpv, 2376, 2888, 0)
                        A([Rpv], [R_out["Vm"]], lambda e: e.activation(out=Vms[:, j, :], in_=pv[:], func=AF.Copy))
                        pv, Rpv = next_pf()
                        proj(tsl, pv, Rpv, 2888, 3400, 0)
                        A([Rpv], [R_out["og"]], lambda e: e.activation(out=ogs[:, j, :], in_=pv[:], func=AF.Sigmoid))

                    def stage_Q(j):
                        tix = sq * NT + sti * 4 + j
                        jb = j % 2
                        pt, Rp = next_pt()
                        for kc in range(2):
                            T([R_cqn[jb]] + RC, [Rp], lambda e: e.transpose(out=pt[:, kc * 128:(kc + 1) * 128], in_=cqn[jb][:, kc * 128:(kc + 1) * 128], identity=identb[:]))
                        A([Rp], [R_cqT], lambda e: e.activation(out=cqT[:], in_=pt[:, 0:256].rearrange("p (k t) -> p k t", t=128), func=AF.Copy))
                        for which, key in ((0, "q"), (1, "qi")):
                            pq, Rpq = next_pf()
                            for kc in range(2):
                                T([R_cqT, *R_w], [Rpq], lambda e: e.matmul(pq[:], lhsT=cqT[:, kc, :], rhs=wq_sb[:, kc, which * 512:(which + 1) * 512], start=(kc == 0), stop=(kc == 1)))
                            rope(pq[:], Rpq, 512, tix, rOs[key][jb][:], [R_rOs[key][jb]])

                    def stage_R(j):
                        tsl = slice(j * 128, (j + 1) * 128)
                        jb = j % 2
                        pt, Rp = next_pt()
                        T([R_rOs["ki"][jb]] + RC, [Rp], lambda e: e.transpose(out=pt[0:64, 0:128], in_=rOs["ki"][jb][:, 0:64], identity=identb[:]))
                        A([Rp], [R_out["kiT"]], lambda e: e.activation(out=kiTs[:, tsl], in_=pt[0:64, 0:128], func=AF.Copy))
                        for key, dstT, okey in (("k", kTs, "kT"), ("q", qTs, "qT"), ("qi", qiTs, "qiT")):
                            pt, Rp = next_pt()
                            for pr in range(4):
                                T([R_rOs[key][jb]] + RC, [Rp], lambda e: e.transpose(out=pt[:, pr * 128:(pr + 1) * 128], in_=rOs[key][jb][:, pr * 128:(pr + 1) * 128], identity=identb[:]))
                            A([Rp], [R_out[okey]], lambda e: e.activation(out=dstT[:, :, tsl], in_=pt[:, 0:512].rearrange("p (k t) -> p k t", t=128), func=AF.Copy))

                    def stage_FM(ch):
                        pc, Rpc = next_pf()
                        c0 = 1352 + ch * 128
                        for kc in range(8):
                            T([R_hT, *R_w], [Rpc], lambda e: e.matmul(pc[:], lhsT=w_in_sb[:, kc, c0:c0 + 128], rhs=hT[:, kc, :], start=(kc == 0), stop=(kc == 7)))
                        A([Rpc], [R_pre], lambda e: e.activation(out=pre[:, ch, 3:515], in_=pc[:], func=AF.Copy))
                        V([R_pre, R_sp], [R_acc], lambda e: e.tensor_scalar(out=acc[:], in0=pre[:, ch, 0:512], scalar1=cw[:, ch, 0:1], scalar2=None, op0=ALU.mult))
                        for jj in range(1, 4):
                            V([R_pre, R_sp, R_acc], [R_acc], lambda e: e.scalar_tensor_tensor(out=acc[:], in0=pre[:, ch, jj:jj + 512], scalar=cw[:, ch, jj:jj + 1], in1=acc[:], op0=ALU.mult, op1=ALU.add))
                        A([R_acc, R_sp], [R_out["qmT"]], lambda e: e.activation(out=qmTs[:, ch, :], in_=acc[:], func=AF.Silu, bias=cb[:, ch:ch + 1]))

                    for j in range(4):
                        stage_P(j)
                        stage_FM(2 * j)
                        stage_FM(2 * j + 1)
                        stage_Q(j)
                        if j >= 1:
                            stage_R(j - 1)
                    stage_R(3)
                    V([R_pre], [R_pre], lambda e: e.tensor_copy(out=pre[:, :, 0:3], in_=pre[:, :, 512:515]))

                    tsl2 = slice(sti * 512, (sti + 1) * 512)
                    if sti == 0:
                        R_p1[sq] = []

                    def Rp1_new():
                        r_ = Region()
                        R_p1[sq].append(r_)
                        return [r_]
                    cx.dma("gpsimd", qT_d[sq, :, :, tsl2], qTs[:], [R_out["qT"]], Rp1_new())
                    cx.dma("gpsimd", qiT_d[sq, :, :, tsl2], qiTs[:], [R_out["qiT"]], Rp1_new())
                    cx.dma("gpsimd", kT_d[sq, :, :, tsl2], kTs[:], [R_out["kT"]], Rp1_new())
                    cx.dma("gpsimd", kiT_d[sq, :, tsl2], kiTs[:], [R_out["kiT"]], Rp1_new())
                    cx.dma("gpsimd", qmT_d[sq, :, :, tsl2], qmTs[:], [R_out["qmT"]], Rp1_new())
                    for (dd, sb_, key) in ((V_d, Vs, "V"), (Vm_d, Vms, "Vm"), (og_d, ogs, "og"), (wi_d, wis, "wi"), (ig_d, igs, "ig"), (lf_d, lfs, "lf")):
                        cx.dma("gpsimd", dd[sq, tsl2, :].rearrange("(j p) c -> p j c", p=128), sb_[:], [R_out[key]], Rp1_new())

                items = [(sq_, sti_) for sq_ in range(NSEQ) for sti_ in range(4)]
                p1_front(items[0][0], items[0][1], hTs[0], R_hTs[0])
                for k_, (sq_, sti_) in enumerate(items):
                    if k_ + 1 < len(items):
                        p1_front(items[k_ + 1][0], items[k_ + 1][1], hTs[(k_ + 1) % 2], R_hTs[(k_ + 1) % 2])
                    p1_back(sq_, sti_, hTs[k_ % 2], R_hTs[k_ % 2])
            cx.barrier()

            with ExitStack() as st:
                qT = sbuf(st, "qT_sb", [128, 4, S], BF16)
                qiT = sbuf(st, "qiT_sb", [128, 4, S], BF16)
                kT = sbuf(st, "kT_sb", [128, 4, S], BF16)
                kiT = sbuf(st, "kiT_sb", [128, S], BF16)
                Vx = sbuf(st, "Vx", [128, NT, 8, 65], BF16)
                wi = sbuf(st, "wi_sb", [128, NT, 8], F32)
                aon = sbuf(st, "aon", [128, 512], F32)
                R_in = Region()
                R_sp = Region()
                cx.dma("sync", aon[:], aon_d[l:l + 1, :].partition_broadcast(128), [], [R_sp])
                dg = [sbuf(st, "dg%d" % i, [128, 8, 128], BF16) for i in range(2)]
                R_dg = [Region() for _ in range(2)]
                NRB = 8
                rl = [sbuf(st, "rl%d" % i, [128, 512], BF16) for i in range(NRB)]
                R_rl = [Region() for _ in range(NRB)]
                sc = [sbuf(st, "sc%d" % i, [128, S], F32) for i in range(4)]
                R_sc = [Region() for _ in range(4)]
                junkA = sbuf(st, "junkA", [128, S], BF16)
                R_junkA = Region()
                bis = [dict(lo=sbuf(st, "b_lo%d" % i, [128, 4], F32), dk=sbuf(st, "b_dk%d" % i, [128, 32], F32),
                            ndk=sbuf(st, "b_ndk%d" % i, [128, 32], F32), S=sbuf(st, "b_S%d" % i, [128, 32], F32),
                            nmid=sbuf(st, "b_nm%d" % i, [128, 1], F32), g=sbuf(st, "b_g%d" % i, [128, 1], F32),
                            inc=sbuf(st, "b_inc%d" % i, [128, 1], F32)) for i in range(2)]
                R_bis = [dict(lo=Region(), dk=Region(), S=Region(), nmid=Region(), g=Region(), inc=Region()) for _ in range(2)]
                NRND = 26
                wk = sbuf(st, "wk", [128, S], F32)
                R_wk = Region()
                m8 = [sbuf(st, "m8_%d" % i, [128, 8], F32) for i in range(2)]
                R_m8 = [Region() for _ in range(2)]
                mb = [sbuf(st, "mb%d" % i, [128, S], BF16) for i in range(4)]
                R_mb = [Region() for _ in range(4)]
                PT = [sbuf(st, "PT%d" % i, [128, NT, 128], BF16) for i in range(2)]
                R_PT = [Region() for _ in range(2)]
                atf = [sbuf(st, "atf%d" % i, [128, 8, 64], F32) for i in range(2)]
                R_atf = [Region() for _ in range(2)]
                lnd = [sbuf(st, "lnd%d" % i, [128, 16], F32) for i in range(2)]
                R_ev = [Region() for _ in range(2)]
                rs = [sbuf(st, "rs%d" % i, [128, 8], F32) for i in range(2)]
                R_rs = [Region() for _ in range(2)]
                sqf = sbuf(st, "sqf", [128, 8, 64], F32)
                R_sqf = Region()
                att_st = [sbuf(st, "att_st%d" % i, [128, 512], BF16) for i in range(2)]
                R_ast = [Region() for _ in range(2)]
                sml = sbuf(st, "sml2", [128, 24], F32)
                R_sml = Region()
                pl = [psum(st, "pl%d" % i, [128, 512], F32) for i in range(3)]
                R_pl = [Region() for _ in range(3)]
                psc = psum(st, "psc", [128, 512], F32)
                R_psc = Region()
                plt = [psum(st, "plt%d" % i, [128, 512], F32) for i in range(2)]
                R_plt = [Region() for _ in range(2)]
                pav = [psum(st, "pav%d" % i, [128, 512], F32) for i in range(2)]
                R_pav = [Region() for _ in range(2)]
                cnts = {"pl": 0, "rl": 0, "lt": 0, "pt": 0}

                def emit_scores(sq, i):
                    ns = (i + 1) * 128
                    tq = slice(i * 128, (i + 1) * 128)
                    dgb, Rdg = dg[i % 2], R_dg[i % 2]
                    scb, Rsc = sc[i % 4], R_sc[i % 4]
                    for h in range(8):
                        V([R_in] + RC, [Rdg], lambda e: e.tensor_scalar(out=dgb[:, h, :], in0=identf[:], scalar1=wi[:, i, h:h + 1], scalar2=None, op0=ALU.mult))
                    for c0 in range(0, ns, 512):
                        cols = min(512, ns - c0)
                        rbuf = []
                        for h in range(8):
                            po = (h % 2) * 64
                            pb, Rpb = pl[cnts["pl"] % 3], R_pl[cnts["pl"] % 3]
                            cnts["pl"] += 1
                            T([R_in], [Rpb], lambda e: e.matmul(pb[:, 0:cols], lhsT=qiT[po:po + 64, h // 2, tq], rhs=kiT[po:po + 64, c0:c0 + cols], start=True, stop=True))
                            rb, Rrb = rl[cnts["rl"] % NRB], R_rl[cnts["rl"] % NRB]
                            cnts["rl"] += 1
                            if h not in (1, 3, 5):
                                A([Rpb], [Rrb], lambda e: e.activation(out=rb[:, 0:cols], in_=pb[:, 0:cols], func=AF.Relu))
                            else:
                                V([Rpb], [Rrb], lambda e: e.tensor_scalar(out=rb[:, 0:cols], in0=pb[:, 0:cols], scalar1=0.0, scalar2=None, op0=ALU.max))
                            rbuf.append((rb, Rrb))
                        for h in range(8):
                            rb, Rrb = rbuf[h]
                            T([Rrb, Rdg], [R_psc], lambda e: e.matmul(psc[:, 0:cols], lhsT=dgb[:, h, :], rhs=rb[:, 0:cols], start=(h == 0), stop=(h == 7)))
                        V([R_psc], [Rsc], lambda e: e.tensor_copy(out=scb[:, c0:c0 + cols], in_=psc[:, 0:cols]))
                    V([Rsc] + RC, [Rsc], lambda e: e.tensor_tensor(out=scb[:, tq], in0=scb[:, tq], in1=negtri[:], op=ALU.add))

                def emit_mask(i, thr, Rthr):
                    ns = (i + 1) * 128
                    scb, Rsc = sc[i % 4], R_sc[i % 4]
                    V([Rsc, Rthr], [R_mb[i % 4]], lambda e: e.tensor_scalar(out=mb[i % 4][:, 0:ns], in0=scb[:, 0:ns], scalar1=thr, scalar2=-30000.0, op0=ALU.is_lt, op1=ALU.mult))

                def emit_topk_dve(i):
                    ns = (i + 1) * 128
                    bb = i % 2
                    scb, Rsc = sc[i % 4], R_sc[i % 4]
                    if i >= 2:
                        nr = TOPK // 8
                        for r in range(nr):
                            src = scb if r == 0 else wk
                            Rs = Rsc if r == 0 else R_wk
                            V([Rs], [R_m8[bb]], lambda e: e.max(out=m8[bb][:], in_=src[:, 0:ns]))
                            if r < nr - 1:
                                V([Rs, R_m8[bb]], [R_wk], lambda e: e.match_replace(out=wk[:, 0:ns], in_to_replace=m8[bb][:], in_values=src[:, 0:ns], imm_value=NEG))
                        emit_mask(i, m8[bb][:, 7:8], R_m8[bb])
                    else:
                        emit_mask(i, thr0[:, 0:1], R_c6)

                def emit_topk_act(i, steps=()):
                    ns = (i + 1) * 128
                    scb, Rsc = sc[i % 4], R_sc[i % 4]
                    bt, Rb = bis[(i // 2) % 2], R_bis[(i // 2) % 2]
                    lo, dk, ndk, Sb, nmid, g, inc = bt["lo"], bt["dk"], bt["ndk"], bt["S"], bt["nmid"], bt["g"], bt["inc"]
                    V([Rsc], [Rb["lo"]], lambda e: e.tensor_reduce(out=lo[:, 0:1], in_=scb[:, 0:256], axis=AX.X, op=ALU.min))
                    V([Rsc], [Rb["lo"]], lambda e: e.tensor_reduce(out=lo[:, 1:2], in_=scb[:, 0:ns], axis=AX.X, op=ALU.max))
                    V([Rb["lo"]], [Rb["lo"]], lambda e: e.tensor_tensor(out=lo[:, 2:3], in0=lo[:, 1:2], in1=lo[:, 0:1], op=ALU.subtract))
                    V([Rb["lo"]] + RC, [Rb["dk"]], lambda e: e.tensor_scalar(out=dk[:], in0=pw2[:], scalar1=lo[:, 2:3], scalar2=None, op0=ALU.mult))
                    V([Rb["dk"]], [Rb["dk"]], lambda e: e.tensor_scalar(out=ndk[:], in0=dk[:], scalar1=-1.0, scalar2=None, op0=ALU.mult))
                    V([], [Rb["S"]], lambda e: e.memset(Sb[:], 0.0))
                    for k in range(NRND):
                        A([Rb["lo"], Rb["dk"]], [Rb["nmid"]], lambda e: e.activation(out=nmid[:], in_=lo[:, 0:1], func=AF.Identity, scale=-1.0, bias=ndk[:, k:k + 1]))
                        A([Rsc, Rb["nmid"], Rb["S"]], [R_junkA, Rb["S"]], lambda e: e.activation(out=junkA[:, 0:ns], in_=scb[:, 0:ns], func=AF.Sign, bias=nmid[:, 0:1], accum_out=Sb[:, k:k + 1]))
                        A([Rb["S"]], [Rb["g"]], lambda e: e.activation(out=g[:], in_=Sb[:, k:k + 1], func=AF.Sign, bias=float(ns - 511)))
                        A([Rb["g"], Rb["dk"]], [Rb["inc"]], lambda e: e.activation(out=inc[:], in_=g[:], func=AF.Relu, scale=dk[:, k:k + 1]))
                        A([Rb["inc"], Rb["lo"]], [Rb["lo"]], lambda e: e.activation(out=lo[:, 0:1], in_=inc[:], func=AF.Identity, bias=lo[:, 0:1]))
                        if k >= 2 and steps:
                            steps.pop(0)()
                    while steps:
                        steps.pop(0)()

                def emit_mask_act(i):
                    bt, Rb = bis[(i // 2) % 2], R_bis[(i // 2) % 2]
                    emit_mask(i, bt["lo"][:, 0:1], Rb["lo"])

                def attn_steps(sq, i):
                    tq = slice(i * 128, (i + 1) * 128)
                    bb = i % 2
                    mbb, Rmb = mb[i % 4], R_mb[i % 4]

                    def logits(h):
                        po = (h % 2) * 64
                        pr = h // 2
                        ptb_, Rptb = PT[h % 2], R_PT[h % 2]
                        for g0 in range(0, i + 1, 4):
                            g1 = min(g0 + 4, i + 1)
                            lt, Rlt = plt[cnts["lt"] % 2], R_plt[cnts["lt"] % 2]
                            cnts["lt"] += 1
                            for j in range(g0, g1):
                                o = (j - g0) * 128
                                ks = slice(j * 128, (j + 1) * 128)
                                T([R_in], [Rlt], lambda e: e.matmul(lt[:, o:o + 128], lhsT=kT[po:po + 64, pr, ks], rhs=qT[po:po + 64, pr, tq], start=True, stop=False))
                                T([Rmb] + RC, [Rlt], lambda e: e.matmul(lt[:, o:o + 128], lhsT=mbb[:, ks], rhs=identb[:], start=False, stop=True))
                            ncol = (g1 - g0) * 128
                            A([Rlt], [Rptb], lambda e: e.activation(out=ptb_[:, g0:g1, :], in_=lt[:, 0:ncol].rearrange("p (j t) -> p j t", t=128), func=AF.Exp, scale=0.125))

                    def pv(h):
                        ptb_, Rptb = PT[h % 2], R_PT[h % 2]
                        av = pav[h // 4]
                        Rav = R_pav[h // 4]
                        o = (h % 4) * 65
                        for j in range(i + 1):
                            T([Rptb, R_in], [Rav], lambda e: e.matmul(av[:, o:o + 65], lhsT=ptb_[:, j, :], rhs=Vx[:, j, h, :], start=(j == 0), stop=(j == i)))

                    def evac():
                        for hh in range(2):
                            av3 = pav[hh][:, 0:260].rearrange("p (h c) -> p h c", c=65)
                            A([R_pav[hh]], [R_ev[bb]], lambda e: e.activation(out=lnd[bb][:, hh * 4:hh * 4 + 4].unsqueeze(2), in_=av3[:, :, 64:65], func=AF.Ln))
                        A([R_ev[bb]], [R_ev[bb]], lambda e: e.activation(out=lnd[bb][:, 8:16], in_=lnd[bb][:, 0:8], func=AF.Exp, scale=-1.0))
                        for h in range(8):
                            av3 = pav[h // 4][:, 0:260].rearrange("p (h c) -> p h c", c=65)
                            A([R_pav[h // 4], R_ev[bb]], [R_atf[bb]], lambda e: e.activation(out=atf[bb][:, h, :], in_=av3[:, h % 4, 0:64], func=AF.Identity, scale=lnd[bb][:, 8 + h:9 + h]))

                    steps = [lambda: logits(0)]
                    for h in range(8):
                        def st_(h=h):
                            if h + 1 < 8:
                                logits(h + 1)
                            pv(h)
                        steps.append(st_)
                    steps.append(evac)
                    return steps

                def attn_norm(sq, i):
                    bb = i % 2
                    G([R_atf[bb]], [R_sqf], lambda e: e.tensor_tensor(out=sqf[:], in0=atf[bb][:], in1=atf[bb][:], op=ALU.mult))
                    V([R_sqf], [R_rs[bb]], lambda e: e.tensor_reduce(out=rs[bb][:], in_=sqf[:], axis=AX.X, op=ALU.add))
                    V([R_rs[bb]], [R_rs[bb]], lambda e: e.tensor_scalar(out=rs[bb][:], in0=rs[bb][:], scalar1=1.0 / 64, scalar2=EPS, op0=ALU.mult, op1=ALU.add))

                def emit_attn_fin(sq, i):
                    tq = slice(i * 128, (i + 1) * 128)
                    bb = i % 2
                    A([R_rs[bb]], [R_rs[bb]], lambda e: e.activation(out=rs[bb][:], in_=rs[bb][:], func=AF.Ln))
                    A([R_rs[bb]], [R_rs[bb]], lambda e: e.activation(out=rs[bb][:], in_=rs[bb][:], func=AF.Exp, scale=-0.5))
                    G([R_atf[bb], R_rs[bb]], [R_sqf], lambda e: e.tensor_tensor(out=sqf[:], in0=atf[bb][:], in1=bc_last(rs[bb][:], 64), op=ALU.mult))
                    G([R_sqf, R_sp], [R_ast[bb]], lambda e: e.tensor_tensor(out=att_st[bb][:], in0=sqf[:].rearrange("p h d -> p (h d)"), in1=aon[:], op=ALU.mult))
                    cx.dma("gpsimd", att_d[sq, tq, :], att_st[bb][:], [R_ast[bb]], [R_att[sq][i]])

                dummy = sbuf(st, "dummy2", [128, 1], F32)
                for sq in range(NSEQ):
                    if sq > 0:
                        cx.barrier()
                    R_ms = Region()
                    V([], [R_ms], lambda e: e.memset(Vx[:], 1.0))
                    rd = list(R_p1[sq]) + [R_ms]
                    Ls = []

                    def ld(out_, in__):
                        r_ = Region()
                        Ls.append(r_)
                        cx.dma("sync", out_, in__, rd, [r_])
                    ld(qT[:], qT_d[sq])
                    ld(qiT[:], qiT_d[sq])
                    ld(kT[:], kT_d[sq])
                    ld(kiT[0:64, :], kiT_d[sq])
                    ld(kiT[64:128, :], kiT_d[sq])
                    ld(wi[:], wi_d[sq].rearrange("(j p) c -> p j c", p=128))
                    for jt in range(NT):
                        ld(Vx[:, jt, :, 0:64], V_d[sq, jt * 128:(jt + 1) * 128, :].rearrange("p (h d) -> p h d", d=64))
                    V(Ls, [R_in], lambda e: e.memset(dummy[:], 0.0))
                    emit_scores(sq, 0)
                    emit_scores(sq, 1)
                    emit_topk_dve(0)
                    emit_topk_dve(1)
                    for p in range(NT // 2):
                        if p >= 1:
                            emit_attn_fin(sq, 2 * p - 2)
                            emit_attn_fin(sq, 2 * p - 1)
                        steps = attn_steps(sq, 2 * p) + attn_steps(sq, 2 * p + 1)
                        if p + 1 < NT // 2:
                            ia, ib = 2 * p + 2, 2 * p + 3
                            emit_scores(sq, ia)
                            emit_scores(sq, ib)
                            if ia >= 2:
                                emit_topk_act(ib, steps)
                                emit_topk_dve(ia)
                                emit_mask_act(ib)
                            else:
                                emit_topk_dve(ia)
                                emit_topk_dve(ib)
                        while steps:
                            steps.pop(0)()
                        attn_norm(sq, 2 * p)
                        attn_norm(sq, 2 * p + 1)
                    emit_attn_fin(sq, NT - 2)
                    emit_attn_fin(sq, NT - 1)
            cx.barrier()

            st_w = ExitStack()
            w_dn_sb = sbuf(st_w, "w_dn_sb", [128, NFC, D], BF16)
            R_wo = [Region() for _ in range(8)]
            R_wd = [Region() for _ in range(NFC)]
            for fc in range(NFC):
                cx.dma("gpsimd", w_dn_sb[:, fc, :], w_dn_d[l, fc * 128:(fc + 1) * 128, :], [], [R_wd[fc]])

            with ExitStack() as st:
                qmT = sbuf(st, "qmT_sb", [128, 8, S], BF16)
                Vmx = sbuf(st, "Vmx", [128, NT, 4, 129], BF16)
                og = sbuf(st, "og_sb", [128, NT, 512], BF16)
                ig = sbuf(st, "ig_sb", [128, NT, 4], F32)
                lf = sbuf(st, "lf_sb", [128, NT, 4], F32)
                mon = sbuf(st, "mon", [128, 512], F32)
                R_in = Region()
                R_sp = Region()
                cx.dma("sync", mon[:], mon_d[l:l + 1, :].partition_broadcast(128), [], [R_sp])
                Cf = sbuf(st, "Cf", [128, 4, 129], F32)
                Cb = sbuf(st, "Cb", [128, 4, 129], BF16)
                R_Cf = [Region() for _ in range(4)]
                R_Cb = [Region() for _ in range(4)]
                LU = sbuf(st, "LU", [128, 4, 128], F32)
                R_LU = [Region() for _ in range(4)]
                ebt = sbuf(st, "ebt", [128, 4, 128], F32)
                R_ebt = Region()
                DT = sbuf(st, "DT", [128, 4, 128], F32)
                R_DT = Region()
                STm = sbuf(st, "STm", [128, 4, 128], BF16)
                R_ST = [Region() for _ in range(4)]
                qtl = sbuf(st, "qtl", [128, 4, 128], BF16)
                R_qtl = [Region() for _ in range(4)]
                kw = sbuf(st, "kw", [128, 4, 128], BF16)
                R_kw = [Region() for _ in range(4)]
                sm = sbuf(st, "sm3", [128, 40], F32)
                R_sm = Region()
                hmf = sbuf(st, "hmf", [128, 4, 128], F32)
                hmq = sbuf(st, "hmq", [128, 4, 128], F32)
                R_hmf, R_hmq = Region(), Region()
                hm_st = sbuf(st, "hm_st", [128, 512], BF16)
                R_hst = Region()
                lnscale = math.log(128.0 ** -0.5)
                lnsc = sbuf(st, "lnsc", [128, 1], F32)
                V([], [R_sp], lambda e: e.memset(lnsc[:], lnscale))
                pB = psum(st, "pB", [128, 512], F32)
                pB2 = psum(st, "pB2", [128, 512], F32)
                pS = psum(st, "pS", [128, 512], F32)
                pH = [psum(st, "pH%d" % i, [128, 512], F32) for i in range(2)]
                pC = [psum(st, "pC%d" % i, [128, 512], F32) for i in range(2)]
                pK = psum(st, "pK", [128, 1024], BF16)
                R_pB, R_pB2, R_pS, R_pK = Region(), Region(), Region(), Region()
                R_pH = [Region() for _ in range(2)]
                R_pC = [Region() for _ in range(2)]
                R_bcol = Region()
                pB3 = pB[:].rearrange("p (h t) -> p h t", t=128)
                pB23 = pB2[:].rearrange("p (h t) -> p h t", t=128)
                pS3 = pS[:].rearrange("p (h t) -> p h t", t=128)
                dummy = sbuf(st, "dummy3", [128, 1], F32)
                for sq in range(NSEQ):
                    if sq > 0:
                        cx.barrier()
                    R_ms = Region()
                    V([], [R_ms], lambda e: e.memset(Vmx[:], 1.0))
                    rd = list(R_p1[sq]) + [R_ms]
                    Ls = []

                    def ld(out_, in__):
                        r_ = Region()
                        Ls.append(r_)
                        cx.dma("sync", out_, in__, rd, [r_])
                    ld(qmT[:], qmT_d[sq])
                    ld(ig[:], ig_d[sq].rearrange("(j p) c -> p j c", p=128))
                    ld(lf[:], lf_d[sq].rearrange("(j p) c -> p j c", p=128))
                    for jt in range(NT):
                        ld(Vmx[:, jt, :, 0:128], Vm_d[sq, jt * 128:(jt + 1) * 128, :].rearrange("p (h d) -> p h d", d=128))
                    ld(og[:], og_d[sq].rearrange("(j p) c -> p j c", p=128))
                    V(Ls, [R_in], lambda e: e.memset(dummy[:], 0.0))
                    for h in range(4):
                        V([], [R_Cf[h]], lambda e: e.memset(Cf[:, h, :], 0.0))
                        V([], [R_Cb[h]], lambda e: e.memset(Cb[:, h, :], 0.0))
                    for c in range(NT):
                        tq = slice(c * 128, (c + 1) * 128)
                        for h in range(4):
                            V([R_in] + RC, [R_LU[h]], lambda e: e.tensor_scalar(out=LU[:, h, :], in0=utri[:], scalar1=lf[:, c, h:h + 1], scalar2=None, op0=ALU.mult))
                            T([R_LU[h]] + RC, [R_pB], lambda e: e.matmul(pB3[:, h, :], lhsT=onesf[:], rhs=LU[:, h, :], start=True, stop=True))
                            T([R_LU[h]] + RC, [R_pB2], lambda e: e.matmul(pB23[:, h, :], lhsT=onesf[:], rhs=LU[:, h, :], start=True, stop=False))
                            T(RC, [R_pB2], lambda e: e.matmul(pB23[:, h, :], lhsT=identf[:], rhs=negm[:], start=False, stop=True))
                        T([R_in] + RC, [R_bcol], lambda e: e.matmul(pH[0][:, 300:304], lhsT=utri[:], rhs=lf[:, c, :], start=True, stop=True))
                        V([R_bcol, R_in], [R_sm], lambda e: e.tensor_tensor(out=sm[:, 0:4], in0=ig[:, c, :], in1=pH[0][:, 300:304], op=ALU.subtract))
                        V([R_sm], [R_sm], lambda e: e.tensor_scalar(out=sm[:, 4:8], in0=sm[:, 0:4], scalar1=lnscale, scalar2=None, op0=ALU.add))
                        V([R_sm, R_pB], [R_sm], lambda e: e.tensor_tensor(out=sm[:, 8:12].unsqueeze(2), in0=sm[:, 0:4].unsqueeze(2), in1=pB3[:, :, 127:128], op=ALU.add))
                        A([R_sm], [R_sm], lambda e: e.activation(out=sm[:, 12:16], in_=sm[:, 8:12], func=AF.Exp))
                        A([R_pB], [R_sm], lambda e: e.activation(out=sm[:, 16:20].unsqueeze(2), in_=pB3[:, :, 127:128], func=AF.Exp))
                        A([R_pB, R_sp], [R_ebt], lambda e: e.activation(out=ebt[:].rearrange("p h t -> p (h t)"), in_=pB[:], func=AF.Exp, bias=lnsc[:, 0:1]))
                        for h in range(4):
                            A([R_pB2, R_sm], [R_DT], lambda e: e.activation(out=DT[:, h, :], in_=pB23[:, h, :], func=AF.Exp, bias=sm[:, 4 + h:5 + h]))
                        for h in range(4):
                            T([R_in], [R_pS], lambda e: e.matmul(pS3[:, h, :], lhsT=qmT[:, 4 + h, tq], rhs=qmT[:, h, tq], start=True, stop=True))
                            T([R_in] + RC, [R_pK], lambda e: e.transpose(out=pK[:, h * 128:(h + 1) * 128], in_=qmT[:, 4 + h, tq], identity=identb[:]))
                        for h in range(4):
                            V([R_pS, R_DT], [R_ST[h]], lambda e: e.tensor_tensor(out=STm[:, h, :], in0=pS3[:, h, :], in1=DT[:, h, :], op=ALU.mult))
                            V([R_in, R_ebt], [R_qtl[h]], lambda e: e.tensor_tensor(out=qtl[:, h, :], in0=qmT[:, h, tq], in1=ebt[:, h, :], op=ALU.mult))
                            A([R_pK, R_sm], [R_kw[h]], lambda e: e.activation(out=kw[:, h, :], in_=pK[:, h * 128:(h + 1) * 128], func=AF.Identity, scale=sm[:, 12 + h:13 + h]))
                        for h in range(4):
                            ph = pH[h // 2]
                            o = (h % 2) * 129
                            T([R_ST[h], R_in], [R_pH[h // 2]], lambda e: e.matmul(ph[:, o:o + 129], lhsT=STm[:, h, :], rhs=Vmx[:, c, h, :], start=True, stop=False))
                            T([R_qtl[h], R_Cb[h]], [R_pH[h // 2]], lambda e: e.matmul(ph[:, o:o + 129], lhsT=qtl[:, h, :], rhs=Cb[:, h, :], start=False, stop=True))
                        for h in range(4):
                            pc = pC[h // 2]
                            o = (h % 2) * 129
                            T([R_kw[h], R_in], [R_pC[h // 2]], lambda e: e.matmul(pc[:, o:o + 129], lhsT=kw[:, h, :], rhs=Vmx[:, c, h, :], start=True, stop=True))
                            V([R_pC[h // 2], R_sm, R_Cf[h]], [R_Cf[h]], lambda e: e.scalar_tensor_tensor(out=Cf[:, h, :], in0=Cf[:, h, :], scalar=sm[:, 16 + h:17 + h], in1=pc[:, o:o + 129], op0=ALU.mult, op1=ALU.add))
                            A([R_Cf[h]], [R_Cb[h]], lambda e: e.activation(out=Cb[:, h, :], in_=Cf[:, h, :], func=AF.Copy))
                        for hh in range(2):
                            ph3 = pH[hh][:, 0:258].rearrange("p (h c) -> p h c", c=129)
                            V([R_pH[hh]], [R_sm], lambda e: e.tensor_copy(out=sm[:, 32 + hh * 2:34 + hh * 2].unsqueeze(2), in_=ph3[:, :, 128:129]))
                            V([R_sm], [R_sm], lambda e: e.scalar_tensor_tensor(out=sm[:, 20 + hh * 2:22 + hh * 2], in0=sm[:, 32 + hh * 2:34 + hh * 2], scalar=-1.0, in1=sm[:, 32 + hh * 2:34 + hh * 2], op0=ALU.mult, op1=ALU.max))
                            V([R_sm], [R_sm], lambda e: e.tensor_scalar(out=sm[:, 20 + hh * 2:22 + hh * 2], in0=sm[:, 20 + hh * 2:22 + hh * 2], scalar1=1.0, scalar2=None, op0=ALU.max))
                            V([R_sm], [R_sm], lambda e: e.reciprocal(out=sm[:, 24 + hh * 2:26 + hh * 2], in_=sm[:, 20 + hh * 2:22 + hh * 2]))
                            V([R_pH[hh], R_sm], [R_hmf], lambda e: e.tensor_tensor(out=hmf[:, hh * 2:hh * 2 + 2, :], in0=ph3[:, :, 0:128], in1=bc_last(sm[:, 24 + hh * 2:26 + hh * 2], 128), op=ALU.mult))
                        V([R_hmf], [R_hmq], lambda e: e.tensor_tensor(out=hmq[:], in0=hmf[:], in1=hmf[:], op=ALU.mult))
                        V([R_hmq], [R_sm], lambda e: e.tensor_reduce(out=sm[:, 28:32], in_=hmq[:], axis=AX.X, op=ALU.add))
                        V([R_sm], [R_sm], lambda e: e.tensor_scalar(out=sm[:, 28:32], in0=sm[:, 28:32], scalar1=1.0 / 128, scalar2=EPS, op0=ALU.mult, op1=ALU.add))
                        A([R_sm], [R_sm], lambda e: e.activation(out=sm[:, 28:32], in_=sm[:, 28:32], func=AF.Ln))
                        A([R_sm], [R_sm], lambda e: e.activation(out=sm[:, 28:32], in_=sm[:, 28:32], func=AF.Exp, scale=-0.5))
                        V([R_hmf, R_sm], [R_hmq], lambda e: e.tensor_tensor(out=hmq[:], in0=hmf[:], in1=bc_last(sm[:, 28:32], 128), op=ALU.mult))
                        V([R_hmq, R_sp], [R_hmq], lambda e: e.tensor_tensor(out=hmq[:].rearrange("p h d -> p (h d)"), in0=hmq[:].rearrange("p h d -> p (h d)"), in1=mon[:], op=ALU.mult))
                        V([R_hmq, R_in], [R_hst], lambda e: e.tensor_tensor(out=hm_st[:], in0=hmq[:].rearrange("p h d -> p (h d)"), in1=og[:, c, :], op=ALU.mult))
                        cx.dma("gpsimd", hm_d[sq, tq, :], hm_st[:], [R_hst], [R_hm[sq][c]])
            cx.barrier()

            w_gu_sb = sbuf(st_w, "w_gu_sb", [128, 8, 2 * FH], BF16)
            R_wg = [Region() for _ in range(16)]
            with ExitStack() as st:
                R_w = R_wo
                w_out_sb = sbuf(st, "w_out_sb", [128, 8, D], BF16)
                for kc in range(8):
                    cx.dma("gpsimd", w_out_sb[:, kc, :], w_out_d[l, kc * 128:(kc + 1) * 128, :], [], [R_wo[kc]])
                def load_wgu(ix):
                    kc, half = ix // 2, ix % 2
                    cx.dma("gpsimd", w_gu_sb[:, kc, half * FH:(half + 1) * FH], w_gu_d[l, kc * 128:(kc + 1) * 128, half * FH:(half + 1) * FH], [], [R_wg[ix]])
                gpost = sbuf(st, "gpost", [128, D], F32)
                R_sp = Region()
                cx.dma("sync", gpost[:], npost_d[l:l + 1, :].partition_broadcast(128), [], [R_sp])
                GM = sbuf(st, "GM", [128, D], F32)
                R_gm = Region()
                mx = [sbuf(st, "mx%d" % i, [128, D], BF16) for i in range(2)]
                R_mx = [Region() for _ in range(2)]
                mTs = [sbuf(st, "mT%d" % i, [128, 8, 128], BF16) for i in range(2)]
                R_mTs = [Region() for _ in range(2)]
                xt = [sbuf(st, "xt4_%d" % i, [128, D], F32) for i in range(2)]
                R_xt = [Region() for _ in range(2)]
                tmpf = sbuf(st, "tmp4", [128, D], F32)
                R_tmpf = Region()
                xo = [sbuf(st, "xo4_%d" % i, [128, D], F32) for i in range(2)]
                R_xo = [Region() for _ in range(2)]
                junk = sbuf(st, "junk4", [128, 512], BF16)
                R_junk = Region()
                sml = sbuf(st, "sml4", [128, 8], F32)
                R_sml = Region()
                py = [psum(st, "py%d" % i, [128, 512], F32) for i in range(4)]
                R_py = [Region() for _ in range(4)]
                ptb = [psum(st, "pt4_%d" % i, [128, 1024], BF16) for i in range(2)]
                R_pt = [Region() for _ in range(2)]
                def f4a_front(sq, i, b):
                    tq = slice(i * 128, (i + 1) * 128)
                    tg = slice(sq * S + i * 128, sq * S + (i + 1) * 128)
                    cx.dma("sync", mx[b][:, 0:512], att_d[sq, tq, :], [R_att[sq][i]], [R_mx[b]])
                    cx.dma("sync", mx[b][:, 512:1024], hm_d[sq, tq, :], [R_hm[sq][i]], [R_mx[b]])
                    rd = [] if R_xin is None else [R_xin[sq][i]]
                    cx.dma("sync", xt[b][:], xin_d[tg, :], rd, [R_xt[b]])
                    pt, Rp = ptb[b], R_pt[b]
                    for kc in range(8):
                        T([R_mx[b]] + RC, [Rp], lambda e: e.transpose(out=pt[:, kc * 128:(kc + 1) * 128], in_=mx[b][:, kc * 128:(kc + 1) * 128], identity=identb[:]))
                    A([Rp], [R_mTs[b]], lambda e: e.activation(out=mTs[b][:], in_=pt[:].rearrange("p (k t) -> p k t", t=128), func=AF.Copy))
                    for n in range(2):
                        pyb, Rpy = py[b * 2 + n], R_py[b * 2 + n]
                        for kc in range(8):
                            T([R_mTs[b], *R_w], [Rpy], lambda e: e.matmul(pyb[:], lhsT=mTs[b][:, kc, :], rhs=w_out_sb[:, kc, n * 512:(n + 1) * 512], start=(kc == 0), stop=(kc == 7)))

                def f4a_back(sq, i, b):
                    tg = slice(sq * S + i * 128, sq * S + (i + 1) * 128)
                    if i == 0:
                        cx.dma("sync", GM[:], modd[l, sq:sq + 1, 2 * D:3 * D].partition_broadcast(128), [R_modd], [R_gm])
                        V([R_gm, R_sp], [R_gm], lambda e: e.tensor_tensor(out=GM[:], in0=GM[:], in1=gpost[:], op=ALU.mult))
                    V([], [R_sml], lambda e: e.memset(sml[:, 0:2], 0.0))
                    for n in range(2):
                        pyb, Rpy = py[b * 2 + n], R_py[b * 2 + n]
                        A([Rpy, R_sml], [R_junk, R_sml], lambda e: e.activation(out=junk[:], in_=pyb[:], func=AF.Square, accum_out=sml[:, n:n + 1]))
                    V([R_sml], [R_sml], lambda e: e.tensor_tensor(out=sml[:, 2:3], in0=sml[:, 0:1], in1=sml[:, 1:2], op=ALU.add))
                    V([R_sml], [R_sml], lambda e: e.tensor_scalar(out=sml[:, 2:3], in0=sml[:, 2:3], scalar1=1.0 / D, scalar2=EPS, op0=ALU.mult, op1=ALU.add))
                    A([R_sml], [R_sml], lambda e: e.activation(out=sml[:, 2:3], in_=sml[:, 2:3], func=AF.Ln))
                    A([R_sml], [R_sml], lambda e: e.activation(out=sml[:, 2:3], in_=sml[:, 2:3], func=AF.Exp, scale=-0.5))
                    for n in range(2):
                        pyb, Rpy = py[b * 2 + n], R_py[b * 2 + n]
                        cs = slice(n * 512, (n + 1) * 512)
                        V([Rpy, R_sml, R_gm], [R_tmpf], lambda e: e.scalar_tensor_tensor(out=tmpf[:, cs], in0=pyb[:], scalar=sml[:, 2:3], in1=GM[:, cs], op0=ALU.mult, op1=ALU.mult))
                    V([R_tmpf, R_xt[b]], [R_xo[b]], lambda e: e.tensor_tensor(out=xo[b][:], in0=tmpf[:], in1=xt[b][:], op=ALU.add))
                    cx.dma("gpsimd", x1_d[tg, :], xo[b][:], [R_xo[b]], [R_x1[sq][i]])

                items4 = [(sq_, i_) for sq_ in range(NSEQ) for i_ in range(NT)]
                f4a_front(items4[0][0], items4[0][1], 0)
                for k_, (sq_, i_) in enumerate(items4):
                    if k_ + 1 < len(items4):
                        f4a_front(items4[k_ + 1][0], items4[k_ + 1][1], (k_ + 1) % 2)
                    if k_ % 2 == 0 and k_ // 2 < 16:
                        load_wgu(k_ // 2)
                    f4a_back(sq_, i_, k_ % 2)
            cx.barrier()

            with ExitStack() as st:
                R_w = R_wg + R_wd
                MODB = sbuf(st, "MODB", [128, 3, D], F32)
                R_modb = Region()
                x_st = [sbuf(st, "x5_%d" % i, [128, 2, D], F32) for i in range(2)]
                R_xst = [Region() for _ in range(2)]
                tmpf = sbuf(st, "tmp5", [128, D], F32)
                R_tmpf = Region()
                junk = tmpf
                R_junk = R_tmpf
                tmpF = sbuf(st, "tmpF5", [128, D], F32)
                R_tmpF = Region()
                junkF = tmpF
                R_junkF = R_tmpF
                smlF = sbuf(st, "smlF5", [128, 4], F32)
                R_smlF = Region()
                hb = sbuf(st, "hb5", [128, 2, D], BF16)
                R_hb = [Region() for _ in range(2)]
                hTs = [sbuf(st, "hT5_%d" % i, [128, 8, 256], BF16) for i in range(2)]
                R_hTs = [Region() for _ in range(2)]
                gua = sbuf(st, "gua", [128, NFC, 256], BF16)
                R_gua = Region()
                sg = [sbuf(st, "sg0", [128, 256], F32)] * 2
                R_sg = [Region()] * 2
                xo = [sbuf(st, "xo5_0", [128, D], F32)] * 2
                R_xo = [Region()] * 2
                gq = xo[0]
                R_gq = R_xo[0]
                sml = sbuf(st, "sml5", [128, 8], F32)
                R_sml = Region()
                pg = [psum(st, "pg%d" % i, [128, 512], F32) for i in range(2)]
                R_pg = [Region() for _ in range(2)]
                pyb_ = [psum(st, "py5_%d" % i, [128, 512], F32) for i in range(4)]
                R_pyb = [Region() for _ in range(4)]
                ptb = [psum(st, "pt5_%d" % i, [128, 1024], BF16) for i in range(2)]
                R_pt = [Region() for _ in range(2)]
                pgi = [0]
                pti = [0]
                xoi = [0]

                def load_modb(sq):
                    cx.dma("sync", MODB[:, 0, :], modd[l, sq:sq + 1, 3 * D:4 * D].partition_broadcast(128), [R_modd], [R_modb])
                    cx.dma("sync", MODB[:, 1, :], modd[l, sq:sq + 1, 4 * D:5 * D].partition_broadcast(128), [R_modd], [R_modb])
                    cx.dma("sync", MODB[:, 2, :], modd[l, sq:sq + 1, 5 * D:6 * D].partition_broadcast(128), [R_modd], [R_modb])
                    cx.dma("sync", gq[:], fpre_d[l:l + 1, :].partition_broadcast(128), [], [R_gq])
                    V([R_modb, R_gq], [R_modb], lambda e: e.scalar_tensor_tensor(out=MODB[:, 1, :], in0=MODB[:, 1, :], scalar=1.0, in1=gq[:], op0=ALU.add, op1=ALU.mult))
                    cx.dma("sync", gq[:], fpost_d[l:l + 1, :].partition_broadcast(128), [], [R_gq])
                    V([R_modb, R_gq], [R_modb], lambda e: e.tensor_tensor(out=MODB[:, 2, :], in0=MODB[:, 2, :], in1=gq[:], op=ALU.mult))

                def frontA(sq, ti, kb):
                    T0 = sq * S + ti * 256
                    xs = x_st[kb]
                    cx.dma("sync", xs[:], x1_d[T0:T0 + 256, :].rearrange("(j p) d -> p j d", p=128), [R_x1[sq][ti * 2], R_x1[sq][ti * 2 + 1]], [R_xst[kb]])
                    V([], [R_smlF], lambda e: e.memset(smlF[:, 0:2], 0.0))
                    for j in range(2):
                        A([R_xst[kb], R_smlF], [R_junkF, R_smlF], lambda e: e.activation(out=junkF[:], in_=xs[:, j, :], func=AF.Square, accum_out=smlF[:, j:j + 1]))
                    V([R_smlF], [R_smlF], lambda e: e.tensor_scalar(out=smlF[:, 0:2], in0=smlF[:, 0:2], scalar1=1.0 / D, scalar2=EPS, op0=ALU.mult, op1=ALU.add))
                    A([R_smlF], [R_smlF], lambda e: e.activation(out=smlF[:, 0:2], in_=smlF[:, 0:2], func=AF.Ln))
                    A([R_smlF], [R_smlF], lambda e: e.activation(out=smlF[:, 0:2], in_=smlF[:, 0:2], func=AF.Exp, scale=-0.5))
                    for j in range(2):
                        V([R_xst[kb], R_smlF, R_modb], [R_tmpF], lambda e: e.scalar_tensor_tensor(out=tmpF[:], in0=xs[:, j, :], scalar=smlF[:, j:j + 1], in1=MODB[:, 1, :], op0=ALU.mult, op1=ALU.mult))
                        V([R_tmpF, R_modb], [R_hb[j]], lambda e: e.tensor_tensor(out=hb[:, j, :], in0=tmpF[:], in1=MODB[:, 0, :], op=ALU.add))

                def frontB(kb):
                    for j in range(2):
                        pt, Rp = ptb[pti[0] % 2], R_pt[pti[0] % 2]
                        pti[0] += 1
                        for kc in range(8):
                            T([R_hb[j]] + RC, [Rp], lambda e: e.transpose(out=pt[:, kc * 128:(kc + 1) * 128], in_=hb[:, j, kc * 128:(kc + 1) * 128], identity=identb[:]))
                        A([Rp], [R_hTs[kb]], lambda e: e.activation(out=hTs[kb][:, :, j * 128:(j + 1) * 128], in_=pt[:].rearrange("p (k t) -> p k t", t=128), func=AF.Copy))

                def gate_up(kb):
                    hT, R_hT = hTs[kb], R_hTs[kb]
                    for fc in range(NFC):
                        pgb, Rpg = pg[pgi[0] % 2], R_pg[pgi[0] % 2]
                        pgi[0] += 1
                        for kc in range(8):
                            T([R_hT, *R_w], [Rpg], lambda e: e.matmul(pgb[:, 0:256], lhsT=w_gu_sb[:, kc, fc * 128:(fc + 1) * 128], rhs=hT[:, kc, :], start=(kc == 0), stop=(kc == 7)))
                        for kc in range(8):
                            T([R_hT, *R_w], [Rpg], lambda e: e.matmul(pgb[:, 256:512], lhsT=w_gu_sb[:, kc, FH + fc * 128:FH + (fc + 1) * 128], rhs=hT[:, kc, :], start=(kc == 0), stop=(kc == 7)))
                        sgb, Rsg = sg[fc % 2], R_sg[fc % 2]
                        A([Rpg], [Rsg], lambda e: e.activation(out=sgb[:], in_=pgb[:, 0:256], func=AF.Silu))
                        V([Rsg, Rpg], [R_gua], lambda e: e.tensor_tensor(out=gua[:, fc, :], in0=sgb[:], in1=pgb[:, 256:512], op=ALU.mult))

                def down_tail(sq, ti, kb):
                    T0 = sq * S + ti * 256
                    xs = x_st[kb]
                    for j in range(2):
                        V([], [R_sml], lambda e: e.memset(sml[:, 4:6], 0.0))
                        for n in range(2):
                            pyq, Rpyq = pyb_[j * 2 + n], R_pyb[j * 2 + n]
                            for fc in range(NFC):
                                T([R_gua, *R_w], [Rpyq], lambda e: e.matmul(pyq[:], lhsT=gua[:, fc, j * 128:(j + 1) * 128], rhs=w_dn_sb[:, fc, n * 512:(n + 1) * 512], start=(fc == 0), stop=(fc == NFC - 1)))
                            A([Rpyq, R_sml], [R_junk, R_sml], lambda e: e.activation(out=junk[:, 0:512], in_=pyq[:], func=AF.Square, accum_out=sml[:, 4 + n:5 + n]))
                        V([R_sml], [R_sml], lambda e: e.tensor_tensor(out=sml[:, 6:7], in0=sml[:, 4:5], in1=sml[:, 5:6], op=ALU.add))
                        V([R_sml], [R_sml], lambda e: e.tensor_scalar(out=sml[:, 6:7], in0=sml[:, 6:7], scalar1=1.0 / D, scalar2=EPS, op0=ALU.mult, op1=ALU.add))
                        A([R_sml], [R_sml], lambda e: e.activation(out=sml[:, 6:7], in_=sml[:, 6:7], func=AF.Ln))
                        A([R_sml], [R_sml], lambda e: e.activation(out=sml[:, 6:7], in_=sml[:, 6:7], func=AF.Exp, scale=-0.5))
                        for n in range(2):
                            pyq, Rpyq = pyb_[j * 2 + n], R_pyb[j * 2 + n]
                            cs = slice(n * 512, (n + 1) * 512)
                            V([Rpyq, R_sml, R_modb], [R_tmpf], lambda e: e.scalar_tensor_tensor(out=tmpf[:, cs], in0=pyq[:], scalar=sml[:, 6:7], in1=MODB[:, 2, cs], op0=ALU.mult, op1=ALU.mult))
                        ob = xoi[0] % 2
                        xoi[0] += 1
                        V([R_tmpf, R_xst[kb]], [R_xo[ob]], lambda e: e.tensor_tensor(out=xo[ob][:], in0=tmpf[:], in1=xs[:, j, :], op=ALU.add))
                        tg = slice(T0 + j * 128, T0 + (j + 1) * 128)
                        cx.dma("gpsimd", xout_d[tg, :], xo[ob][:], [R_xo[ob]], [R_xres[sq][ti * 2 + j]])

                items5 = [(sq_, ti_) for sq_ in range(NSEQ) for ti_ in range(8)]
                load_modb(0)
                frontA(0, 0, 0)
                frontB(0)
                for k_, (sq_, ti_) in enumerate(items5):
                    kb = k_ % 2
                    nxt = items5[k_ + 1] if k_ + 1 < len(items5) else None
                    if nxt is not None and nxt[0] == sq_:
                        frontA(nxt[0], nxt[1], 1 - kb)
                    gate_up(kb)
                    if nxt is not None and nxt[0] == sq_:
                        frontB(1 - kb)
                    down_tail(sq_, ti_, kb)
                    if nxt is not None and nxt[0] != sq_:
                        load_modb(nxt[0])
                        frontA(nxt[0], nxt[1], 1 - kb)
                        frontB(1 - kb)
            cx.barrier()
            st_w.close()
        cx.finish()
        stuck = cx.check_deadlock()
        if stuck:
            raise RuntimeError("static deadlock check failed: %r" % (stuck,))
    return nc


def _consts():
    k = np.arange(128)
    ident = np.eye(128, dtype=np.float32)
    utri = (k[:, None] <= k[None, :]).astype(np.float32)
    negtri = np.where(k[None, :] <= k[:, None], 0.0, NEG).astype(np.float32)
    negm = np.where(k[:, None] <= k[None, :], 0.0, -30000.0).astype(np.float32)
    invf = (10000.0 ** (-np.arange(0, 64, 2, dtype=np.float32) / 64)).astype(np.float32)[None, :]
    pw2 = np.ascontiguousarray(np.broadcast_to((2.0 ** -(np.arange(32, dtype=np.float32) + 1.0)).astype(np.float32)[None, :], (128, 32)))
    return dict(c_ident=ident, c_utri=utri, c_negtri=negtri, c_negm=negm, c_invf=invf, c_pw2=pw2)


def make_in_maps(inputs, n_cores=8):
    f32 = lambda a: np.ascontiguousarray(np.asarray(a, dtype=np.float32))
    shared = {}
    for k in ("w_mod", "b_mod", "mix_norm_pre", "mix_norm_post", "w_in", "q_latent_norm", "w_q_up", "w_qidx_up",
              "b_igate", "b_fgate", "attn_out_norm", "mlstm_out_norm", "w_out", "ffn_norm_pre", "ffn_norm_post",
              "w_gate_up", "w_down"):
        shared[k] = f32(inputs[k])
    cw = f32(inputs["conv_w"])
    shared["convw"] = np.ascontiguousarray(cw.reshape(2, 4, 8, 128).transpose(0, 3, 2, 1))
    shared["convb"] = np.ascontiguousarray(f32(inputs["conv_b"]).reshape(2, 8, 128).transpose(0, 2, 1))
    shared.update(_consts())
    x = f32(inputs["x"])
    c = f32(inputs["c"])
    pos = np.asarray(inputs["positions"]).astype(np.int32)
    maps = []
    for i in range(n_cores):
        m = dict(shared)
        m["x"] = np.ascontiguousarray(x[2 * i:2 * i + 2].reshape(NSEQ * S, D))
        m["cT"] = np.ascontiguousarray(c[2 * i:2 * i + 2].reshape(NSEQ, 8, 128).transpose(2, 1, 0))
        m["pos"] = np.ascontiguousarray(pos[2 * i:2 * i + 2].reshape(NSEQ, NT, 128).transpose(2, 0, 1))
        maps.append(m)
    return maps


_NC_CACHE = {}


def kernel(**inputs):
    if "nc" not in _NC_CACHE:
        _NC_CACHE["nc"] = build_nc()
    nc = _NC_CACHE["nc"]
    maps = make_in_maps(inputs)
    res = run_bass_kernel_spmd(nc, maps, core_ids=list(range(8)))
    outs = [np.asarray(r["out"]).reshape(NSEQ, S, D) for r in res.results]
    return np.concatenate(outs, axis=0).astype(np.float32)
```
